# Optimizing a Trainium2 kernel written in Bass

```python
import math
import jax
import jax.numpy as jnp
from jax import lax
import numpy as np

D_MODEL = 1024
BATCH = 4
SEQ = 8192
DEPTH = 4

A_HEADS = 8
A_HEAD_DIM = 64
A_WIDTH = A_HEADS * A_HEAD_DIM
A_KV_RANK = 128
IDX_HEADS = 8
IDX_DIM = 64
TOPK_MAX = 256
Q_BLOCK = 128
POOL_WINDOWS = (2, 4, 8, 16)
POOL_WIDTH = D_MODEL - A_WIDTH
POOL_GROUP = POOL_WIDTH // len(POOL_WINDOWS)
EVEN_SPLITS = (A_WIDTH, A_WIDTH + A_KV_RANK, A_WIDTH + A_KV_RANK + IDX_HEADS * IDX_DIM, A_WIDTH + A_KV_RANK + IDX_HEADS * IDX_DIM + IDX_DIM, A_WIDTH + A_KV_RANK + IDX_HEADS * IDX_DIM + IDX_DIM + IDX_HEADS)
EVEN_IN = EVEN_SPLITS[-1] + POOL_WIDTH
C_HEADS = 8
C_HEAD_DIM = 128
C_WIDTH = C_HEADS * C_HEAD_DIM
CONV_WIDTH = 4
CHUNK = 64
ODD_SPLITS = (3 * C_WIDTH, 4 * C_WIDTH, 4 * C_WIDTH + C_HEADS)
ODD_IN = 4 * C_WIDTH + 2 * C_HEADS
D_FF = 4 * D_MODEL
N_EVEN = (DEPTH + 1) // 2
N_ODD = DEPTH // 2
ROPE_THETA = 10000.0
NORM_EPS = 1e-6
NEG_SCORE = -1e30

kernel_name = 'hybrid_dsa_pool_gdn_trunk'


def rmsnorm(x, g):
    xf = x.astype(jnp.float32)
    y = xf * lax.rsqrt(jnp.mean(xf * xf, axis=-1, keepdims=True) + NORM_EPS)
    return (y * g.astype(jnp.float32)).astype(x.dtype)


def l2norm(x):
    return x * lax.rsqrt(jnp.sum(x * x, axis=-1, keepdims=True) + NORM_EPS)


def rope_tables(L, dim, dtype):
    inv = ROPE_THETA ** (-jnp.arange(0, dim, 2, dtype=jnp.float32) / dim)
    ang = jnp.arange(L, dtype=jnp.float32)[:, None] * inv[None, :]
    return jnp.cos(ang).astype(dtype), jnp.sin(ang).astype(dtype)


def apply_rope(x, cos, sin):
    x1, x2 = jnp.split(x, 2, axis=-1)
    return jnp.concatenate([x1 * cos - x2 * sin, x1 * sin + x2 * cos], axis=-1)


def dsa_attention(q, k, v, qi, ki, wi):
    B, L, H, Dh = q.shape
    topk = min(TOPK_MAX, L // 4)
    nb = L // Q_BLOCK
    key_pos = jnp.arange(L)
    gather = jax.vmap(lambda t, i: t[i])

    def blocks(t):
        return jnp.moveaxis(t.reshape(B, nb, Q_BLOCK, *t.shape[2:]), 1, 0)

    def one_block(args):
        q_b, qi_b, wi_b, start = args
        q_pos = start + jnp.arange(Q_BLOCK)
        rel = jax.nn.relu(jnp.einsum('bqhd,bsd->bqhs', qi_b, ki).astype(jnp.float32))
        score = jnp.einsum('bqhs,bqh->bqs', rel, wi_b.astype(jnp.float32)) * (IDX_DIM ** -0.5)
        causal = key_pos[None, :] <= q_pos[:, None]
        score = jnp.where(causal[None], score, NEG_SCORE)
        _, idx = lax.top_k(score, topk)
        k_sel = gather(k, idx)
        v_sel = gather(v, idx)
        valid = idx <= q_pos[None, :, None]
        logits = jnp.einsum('bqhd,bqkd->bhqk', q_b, k_sel).astype(jnp.float32) * (Dh ** -0.5)
        logits = jnp.where(valid[:, None], logits, -jnp.inf)
        prob = jax.nn.softmax(logits, axis=-1).astype(v.dtype)
        return jnp.einsum('bhqk,bqkd->bqhd', prob, v_sel)

    out = lax.map(one_block, (blocks(q), blocks(qi), blocks(wi), jnp.arange(nb) * Q_BLOCK))
    return jnp.moveaxis(out, 0, 1).reshape(B, L, H * Dh)


def multiscale_pool(u, pool_w, pool_scale):
    B, L, _ = u.shape
    uf = u.astype(jnp.float32)
    cs = jnp.pad(jnp.cumsum(uf, axis=1), ((0, 0), (1, 0), (0, 0)))
    t = jnp.arange(L)
    outs = []
    for gi, w in enumerate(POOL_WINDOWS):
        lo, hi = gi * POOL_GROUP, (gi + 1) * POOL_GROUP
        csg = cs[..., lo:hi]
        lower = jnp.pad(csg[:, :L - w + 1], ((0, 0), (w - 1, 0), (0, 0)))
        count = jnp.minimum(t + 1, w).astype(jnp.float32)[:, None]
        outs.append((csg[:, 1:] - lower) / count - uf[..., lo:hi])
    pooled = jnp.stack(outs, axis=2).astype(u.dtype)
    y = jnp.einsum('blgc,gcd->blgd', pooled, pool_w).reshape(B, L, POOL_WIDTH)
    return y * pool_scale


def sparse_pool_mixer(h, w_in, kv_norm, w_uk, w_uv, pool_w, pool_scale, w_out):
    B, L, _ = h.shape
    p = h @ w_in
    q, c_kv, qi, ki, wi, u = jnp.split(p, list(EVEN_SPLITS), axis=-1)
    cos, sin = rope_tables(L, A_HEAD_DIM, h.dtype)
    cos_i, sin_i = rope_tables(L, IDX_DIM, h.dtype)
    q = apply_rope(q.reshape(B, L, A_HEADS, A_HEAD_DIM), cos[:, None], sin[:, None])
    c_kv = rmsnorm(c_kv, kv_norm)
    k = apply_rope(c_kv @ w_uk, cos, sin)
    v = c_kv @ w_uv
    qi = apply_rope(qi.reshape(B, L, IDX_HEADS, IDX_DIM), cos_i[:, None], sin_i[:, None])
    ki = apply_rope(ki, cos_i, sin_i)
    wi = wi * (IDX_HEADS ** -0.5)
    ya = dsa_attention(q, k, v, qi, ki, wi)
    yb = multiscale_pool(u, pool_w, pool_scale)
    return jnp.concatenate([ya, yb], axis=-1) @ w_out


def causal_depthwise_conv(x, w):
    K = w.shape[0]
    return lax.conv_general_dilated(x, w[:, None, :], window_strides=(1,), padding=[(K - 1, 0)], dimension_numbers=('NWC', 'WIO', 'NWC'), feature_group_count=x.shape[-1])


def chunk_gated_delta_rule(q, k, v, beta, g):
    B, L, H, Dk = q.shape
    Dv = v.shape[-1]
    N = L // CHUNK

    def to_chunks(t):
        t = jnp.moveaxis(t, 2, 1)
        return t.reshape(B, H, N, CHUNK, *t.shape[3:])

    q, k, v, beta, g = to_chunks(q), to_chunks(k), to_chunks(v), to_chunks(beta), to_chunks(g)
    g = jnp.cumsum(g, axis=-1)
    idx = jnp.arange(CHUNK)
    tril = idx[:, None] >= idx[None, :]
    strict = idx[:, None] > idx[None, :]
    decay = jnp.exp(jnp.where(tril, g[..., :, None] - g[..., None, :], -jnp.inf))
    kk = jnp.einsum('bhnid,bhnjd->bhnij', k, k)
    lmat = jnp.where(strict, beta[..., :, None] * kk * decay, 0.0)
    eye = jnp.eye(CHUNK, dtype=jnp.float32)
    tmat = lax.linalg.triangular_solve(eye + lmat, jnp.broadcast_to(eye, lmat.shape), left_side=True, lower=True, unit_diagonal=True)
    value = tmat @ (v * beta[..., None])
    k_cumdecay = tmat @ (k * (beta * jnp.exp(g))[..., None])
    attn = jnp.einsum('bhnid,bhnjd->bhnij', q, k) * decay
    q_dec = q * jnp.exp(g)[..., None]
    k_dec = k * jnp.exp(g[..., -1:] - g)[..., None]
    g_last = jnp.exp(g[..., -1])

    def step(state, xs):
        qd, kd, val, kcd, at, gl = xs
        v_new = val - kcd @ state
        o = qd @ state + at @ v_new
        state = state * gl[..., None, None] + jnp.swapaxes(kd, -1, -2) @ v_new
        return state, o

    xs = (jnp.moveaxis(q_dec, 2, 0), jnp.moveaxis(k_dec, 2, 0), jnp.moveaxis(value, 2, 0), jnp.moveaxis(k_cumdecay, 2, 0), jnp.moveaxis(attn, 2, 0), jnp.moveaxis(g_last, 2, 0))
    s0 = jnp.zeros((B, H, Dk, Dv), jnp.float32)
    _, o = lax.scan(step, s0, xs)
    o = jnp.moveaxis(o, 0, 2).reshape(B, H, L, Dv)
    return jnp.moveaxis(o, 1, 2)


def gated_deltanet(h, w_in, conv_w, a_log, dt_bias, o_norm, w_out):
    B, L, _ = h.shape
    f32 = jnp.float32
    p = h @ w_in
    qkv, z, b, a = jnp.split(p, list(ODD_SPLITS), axis=-1)
    qkv = jax.nn.silu(causal_depthwise_conv(qkv, conv_w)).astype(f32)
    q, k, v = [t.reshape(B, L, C_HEADS, C_HEAD_DIM) for t in jnp.split(qkv, 3, axis=-1)]
    q = l2norm(q) * (C_HEAD_DIM ** -0.5)
    k = l2norm(k)
    beta = jax.nn.sigmoid(b.astype(f32))
    g = -jnp.exp(a_log.astype(f32)) * jax.nn.softplus(a.astype(f32) + dt_bias.astype(f32))
    o = chunk_gated_delta_rule(q, k, v, beta, g)
    o = rmsnorm(o, o_norm) * jax.nn.silu(z.reshape(B, L, C_HEADS, C_HEAD_DIM).astype(f32))
    return o.reshape(B, L, C_WIDTH).astype(h.dtype) @ w_out


def squared_relu_mlp(h, w1, w2):
    return jnp.square(jax.nn.relu(h @ w1)) @ w2


def setup_inputs(seed: int = 0):
    key = jax.random.key(seed)
    ks = jax.random.split(key, 20)
    f32 = jnp.float32

    def nrm(k, shape, fan_in):
        return jax.random.normal(k, shape, f32) * (fan_in ** -0.5)

    def gain(k, shape):
        return 1.0 + 0.02 * jax.random.normal(k, shape, f32)

    x = jax.random.normal(ks[0], (BATCH, SEQ, D_MODEL), f32)
    mix_norm = gain(ks[1], (DEPTH, D_MODEL))
    mlp_norm = gain(ks[2], (DEPTH, D_MODEL))
    w_ff1 = nrm(ks[3], (DEPTH, D_MODEL, D_FF), D_MODEL)
    w_ff2 = nrm(ks[4], (DEPTH, D_FF, D_MODEL), D_FF)
    ev_w_in = nrm(ks[5], (N_EVEN, D_MODEL, EVEN_IN), D_MODEL)
    ev_kv_norm = gain(ks[6], (N_EVEN, A_KV_RANK))
    ev_w_uk = nrm(ks[7], (N_EVEN, A_KV_RANK, A_HEAD_DIM), A_KV_RANK)
    ev_w_uv = nrm(ks[8], (N_EVEN, A_KV_RANK, A_HEAD_DIM), A_KV_RANK)
    ev_pool_w = nrm(ks[9], (N_EVEN, len(POOL_WINDOWS), POOL_GROUP, POOL_GROUP), POOL_GROUP)
    ev_pool_scale = 1.0 + 0.1 * jax.random.normal(ks[10], (N_EVEN, POOL_WIDTH), f32)
    ev_w_out = nrm(ks[11], (N_EVEN, D_MODEL, D_MODEL), D_MODEL)
    od_w_in = nrm(ks[12], (N_ODD, D_MODEL, ODD_IN), D_MODEL)
    od_conv_w = nrm(ks[13], (N_ODD, CONV_WIDTH, 3 * C_WIDTH), CONV_WIDTH)
    od_a_log = jnp.log(jax.random.uniform(ks[14], (N_ODD, C_HEADS), f32, 1.0, 16.0))
    dt = jnp.exp(jax.random.uniform(ks[15], (N_ODD, C_HEADS), f32, math.log(1e-3), math.log(1e-1)))
    od_dt_bias = dt + jnp.log(-jnp.expm1(-dt))
    od_o_norm = gain(ks[16], (N_ODD, C_HEAD_DIM))
    od_w_out = nrm(ks[17], (N_ODD, C_WIDTH, D_MODEL), C_WIDTH)
    final_norm = gain(ks[18], (D_MODEL,))
    return {'x': x, 'mix_norm': mix_norm, 'mlp_norm': mlp_norm, 'w_ff1': w_ff1, 'w_ff2': w_ff2,
            'ev_w_in': ev_w_in, 'ev_kv_norm': ev_kv_norm, 'ev_w_uk': ev_w_uk, 'ev_w_uv': ev_w_uv,
            'ev_pool_w': ev_pool_w, 'ev_pool_scale': ev_pool_scale, 'ev_w_out': ev_w_out,
            'od_w_in': od_w_in, 'od_conv_w': od_conv_w, 'od_a_log': od_a_log, 'od_dt_bias': od_dt_bias,
            'od_o_norm': od_o_norm, 'od_w_out': od_w_out, 'final_norm': final_norm}


def reference(x, mix_norm, mlp_norm, w_ff1, w_ff2, ev_w_in, ev_kv_norm, ev_w_uk, ev_w_uv, ev_pool_w, ev_pool_scale, ev_w_out, od_w_in, od_conv_w, od_a_log, od_dt_bias, od_o_norm, od_w_out, final_norm):
    h = x
    for layer in range(DEPTH):
        hn = rmsnorm(h, mix_norm[layer])
        j = layer // 2
        if layer % 2 == 0:
            h = h + sparse_pool_mixer(hn, ev_w_in[j], ev_kv_norm[j], ev_w_uk[j], ev_w_uv[j], ev_pool_w[j], ev_pool_scale[j], ev_w_out[j])
        else:
            h = h + gated_deltanet(hn, od_w_in[j], od_conv_w[j], od_a_log[j], od_dt_bias[j], od_o_norm[j], od_w_out[j])
        h = h + squared_relu_mlp(rmsnorm(h, mlp_norm[layer]), w_ff1[layer], w_ff2[layer])
    return rmsnorm(h, final_norm)
```

```python
import numpy as np
import ml_dtypes
from contextlib import ExitStack
import concourse.bass as bass
import concourse.mybir as mybir
from concourse.bass_utils import run_bass_kernel_spmd

F32 = mybir.dt.float32
BF16 = mybir.dt.bfloat16
I32 = mybir.dt.int32
AF = mybir.ActivationFunctionType
ALU = mybir.AluOpType
AX = mybir.AxisListType

NCORES = 8
D = 1024
DFF = 4096
EPS = 1e-6


class Trk:
    __slots__ = ("w", "r")

    def __init__(self):
        self.w = None
        self.r = {}


class SemObj:
    __slots__ = ("h", "val")

    def __init__(self, h):
        self.h = h
        self.val = 0


class Eng:
    def __init__(self, kb, name, h):
        self.kb = kb
        self.name = name
        self.h = h
        self.sem = SemObj(kb.es.enter_context(kb.nc.semaphore("s_" + name)))
        self.seen = {}


class KB:
    NDMASEM = 24

    def __init__(self):
        self.nc = bass.Bass("TRN2", target_bir_lowering=False)
        self.es = ExitStack()
        self.E = {n: Eng(self, n, getattr(self.nc, n)) for n in ("tensor", "vector", "scalar", "gpsimd", "sync")}
        self.dsems = [SemObj(self.es.enter_context(self.nc.semaphore("s_dma%d" % i))) for i in range(self.NDMASEM)]
        self.dma_i = 0
        self.out_toks = []
        self.ninstr = 0

    def sbuf(self, name, shape, dt):
        return self.es.enter_context(self.nc.sbuf_tensor(name, list(shape), dt))

    def psum(self, name, shape, dt):
        return self.es.enter_context(self.nc.psum_tensor(name, list(shape), dt))

    def dram(self, name, shape, dt, kind):
        return self.nc.dram_tensor(name, list(shape), dt, kind=kind).ap()

    def _waits(self, e, reads, writes, acc=False):
        need = {}

        def req(tok):
            if tok is None:
                return
            s, v = tok
            if need.get(s, (None, 0))[1] < v:
                need[s] = (s, v)

        for t in reads:
            req(t.w)
        for t in writes:
            if not (acc and t.w is not None and t.w[0] is e.sem):
                req(t.w)
            for r in t.r.items():
                req(r)
        for s, v in need.values():
            if e.seen.get(s, 0) < v:
                e.h.wait_ge(s.h, v)
                e.seen[s] = v
                self.ninstr += 1

    def _post(self, tok, reads, writes):
        for t in reads:
            if t.r.get(tok[0], 0) < tok[1]:
                t.r[tok[0]] = tok[1]
        for t in writes:
            t.w = tok
            t.r = {}

    def op(self, eng, fn, reads=(), writes=(), acc=False):
        e = self.E[eng]
        self._waits(e, reads, writes, acc or eng == "tensor")
        ins = fn(e.h)
        e.sem.val += 1
        ins.then_inc(e.sem.h, 1)
        self.ninstr += 1
        tok = (e.sem, e.sem.val)
        self._post(tok, reads, writes)
        return tok

    def dma(self, eng, out, in_, reads=(), writes=(), is_output=False, **kw):
        e = self.E[eng]
        s = self.dsems[self.dma_i % self.NDMASEM]
        self.dma_i += 1
        if s.val > 0 and e.seen.get(s, 0) < s.val:
            e.h.wait_ge(s.h, s.val)
            e.seen[s] = s.val
        self._waits(e, reads, writes)
        ins = e.h.dma_start(out=out, in_=in_, **kw)
        s.val += 16
        ins.then_inc(s.h, 16)
        self.ninstr += 1
        tok = (s, s.val)
        self._post(tok, reads, writes)
        if is_output:
            self.out_toks.append(tok)
        return tok

    def finish(self):
        e = self.E["sync"]
        for s, v in self.out_toks:
            if e.seen.get(s, 0) < v:
                e.h.wait_ge(s.h, v)
                e.seen[s] = v
        for n, en in self.E.items():
            if en.sem.val > 0 and n != "sync":
                e.h.wait_ge(en.sem.h, en.sem.val)
        self.es.close()
        return self.nc


def load_weight_bf16(kb, name, w_ap, K, N, eng="gpsimd"):
    kc = K // 128
    t = kb.sbuf(name, [128, kc, N], BF16)
    trk = Trk()
    src = w_ap.rearrange("(kc p) n -> p kc n", p=128)
    step = 2048
    for c in range(kc):
        for n0 in range(0, N, step):
            n1 = min(N, n0 + step)
            kb.dma(eng, t[:, c, n0:n1], src[:, c, n0:n1], writes=[trk])
    return t, trk


def load_vec_col(kb, name, v_ap, n):
    c = n // 128
    t = kb.sbuf(name, [128, c], F32)
    trk = Trk()
    with kb.nc.allow_non_contiguous_dma(reason="tiny per-feature vector"):
        kb.dma("sync", t[:, :], v_ap.rearrange("(c p) -> p c", p=128), writes=[trk])
    return t, trk


class PsumPool:
    def __init__(self, kb, n=8):
        self.kb = kb
        self.t = [kb.psum("ps%d" % i, [128, 512], F32) for i in range(n)]
        self.trk = [Trk() for _ in range(n)]
        self.i = 0
        self.n = n

    def get(self):
        i = self.i % self.n
        self.i += 1
        return self.t[i], self.trk[i]


def rmsnorm_fm(kb, pp, x_tiles, x_trks, g_t, g_trk, ones_t, ones_trk, out_tiles, out_trks, scr, nd, TT, Dn):
    ps, ps_trk = pp.get()
    for c in range(nd):
        sq, sq_trk = scr["sq"][c]
        kb.op("scalar", lambda h, c=c, sq=sq: h.activation(out=sq, in_=x_tiles[c], func=AF.Square),
              reads=[x_trks[c]], writes=[sq_trk])
    for c in range(nd):
        sq, sq_trk = scr["sq"][c]
        kb.op("tensor", lambda h, c=c, sq=sq: h.matmul(ps[:, :TT], lhsT=ones_t, rhs=sq, start=(c == 0), stop=(c == nd - 1)),
              reads=[sq_trk, ones_trk], writes=[ps_trk])
    rstd, rstd_trk = scr["rstd"]
    kb.op("scalar", lambda h: h.activation(out=rstd, in_=ps[:, :TT], func=AF.Sqrt, bias=scr["eps"][0], scale=1.0 / Dn),
          reads=[ps_trk, scr["eps"][1]], writes=[rstd_trk])
    kb.op("vector", lambda h: h.reciprocal(out=rstd, in_=rstd), reads=[rstd_trk], writes=[rstd_trk])
    for c in range(nd):
        kb.op("vector", lambda h, c=c: h.scalar_tensor_tensor(out=out_tiles[c], in0=x_tiles[c], scalar=g_t[:, c:c + 1], in1=rstd,
                                                               op0=ALU.mult, op1=ALU.mult),
              reads=[x_trks[c], rstd_trk, g_trk], writes=[out_trks[c]])


def build_post(T, final):
    kb = KB()
    nc = kb.nc
    TT = 256
    NT = T // TT
    hT = kb.dram("hT", [D, T], F32, "ExternalInput")
    yT = kb.dram("yT", [D, T], BF16, "ExternalInput")
    w_out = kb.dram("w_out", [D, D], F32, "ExternalInput")
    w1 = kb.dram("w1", [D, DFF], F32, "ExternalInput")
    w2 = kb.dram("w2", [DFF, D], F32, "ExternalInput")
    g_mlp = kb.dram("g_mlp", [D], F32, "ExternalInput")
    g_next = kb.dram("g_next", [D], F32, "ExternalInput")
    hT_out = kb.dram("hT_out", [D, T], F32, "ExternalOutput")
    if final:
        hn_out = kb.dram("hn_out", [D, T], F32, "ExternalOutput")
    else:
        hn_out = kb.dram("hn_out", [D, T], BF16, "ExternalOutput")

    wo_t, wo_trk = load_weight_bf16(kb, "wo", w_out, D, D)
    gm_t, gm_trk = load_vec_col(kb, "gm", g_mlp, D)
    gn_t, gn_trk = load_vec_col(kb, "gn", g_next, D)
    w1_t, w1_trk = load_weight_bf16(kb, "w1s", w1, D, DFF)
    w2_t, w2_trk = load_weight_bf16(kb, "w2s", w2, DFF, D)

    ones = kb.sbuf("ones", [128, 128], BF16)
    ones_trk = Trk()
    kb.op("vector", lambda h: h.memset(ones[:], 1.0), writes=[ones_trk])
    eps_t = kb.sbuf("eps", [128, 1], F32)
    eps_trk = Trk()
    kb.op("vector", lambda h: h.memset(eps_t[:], EPS), writes=[eps_trk])

    pp = PsumPool(kb)
    NB = 2
    y_s = [kb.sbuf("y%d" % b, [128, 8, TT], BF16) for b in range(NB)]
    y_k = [Trk() for _ in range(NB)]
    h_s = [kb.sbuf("h%d" % b, [128, 8, TT], F32) for b in range(NB)]
    h_k = [[Trk() for _ in range(8)] for _ in range(NB)]
    hload_k = [Trk() for _ in range(NB)]
    xn_s = kb.sbuf("xn", [128, 8, TT], BF16)
    xn_k = [Trk() for _ in range(8)]
    sq_s = kb.sbuf("sq", [128, 8, TT], BF16)
    sq_k = [Trk() for _ in range(8)]
    rstd_s = kb.sbuf("rstd", [128, TT], F32)
    rstd_k = Trk()
    a_s = kb.sbuf("a", [128, 32, TT], BF16)
    a_k = [Trk() for _ in range(32)]
    r_s = [kb.sbuf("r%d" % b, [128, TT], F32) for b in range(4)]
    r_k = [Trk() for _ in range(4)]
    if final:
        hn_s = kb.sbuf("hn", [128, 8, TT], F32)
    else:
        hn_s = kb.sbuf("hn", [128, 8, TT], BF16)
    hn_k = [Trk() for _ in range(8)]

    hT_v = hT.rearrange("(c p) t -> p c t", p=128)
    yT_v = yT.rearrange("(c p) t -> p c t", p=128)
    hTo_v = hT_out.rearrange("(c p) t -> p c t", p=128)
    hno_v = hn_out.rearrange("(c p) t -> p c t", p=128)

    scr = {"sq": [(sq_s[:, c, :], sq_k[c]) for c in range(8)], "rstd": (rstd_s[:, :], rstd_k), "eps": (eps_t[:, 0:1], eps_trk)}

    def load(i):
        b = i % NB
        t0 = i * TT
        kb.dma("sync", y_s[b][:, :, :], yT_v[:, :, t0:t0 + TT], writes=[y_k[b]])
        kb.dma("sync", h_s[b][:, :, :], hT_v[:, :, t0:t0 + TT], writes=h_k[b])

    load(0)
    for i in range(NT):
        b = i % NB
        t0 = i * TT
        if i + 1 < NT:
            load(i + 1)
        for oc in range(8):
            ps, pk = pp.get()
            for kc in range(8):
                kb.op("tensor", lambda h, kc=kc, oc=oc, ps=ps: h.matmul(ps[:, :TT], lhsT=wo_t[:, kc, oc * 128:(oc + 1) * 128], rhs=y_s[b][:, kc, :],
                                                                        start=(kc == 0), stop=(kc == 7)),
                      reads=[wo_trk, y_k[b]], writes=[pk])
            kb.op("vector", lambda h, oc=oc, ps=ps: h.tensor_tensor(out=h_s[b][:, oc, :], in0=h_s[b][:, oc, :], in1=ps[:, :TT], op=ALU.add),
                  reads=[pk], writes=[h_k[b][oc]])
        rmsnorm_fm(kb, pp, [h_s[b][:, c, :] for c in range(8)], h_k[b], gm_t, gm_trk, ones[:, :], ones_trk,
                   [xn_s[:, c, :] for c in range(8)], xn_k, scr, 8, TT, D)
        for fc in range(32):
            ps, pk = pp.get()
            for kc in range(8):
                kb.op("tensor", lambda h, kc=kc, fc=fc, ps=ps: h.matmul(ps[:, :TT], lhsT=w1_t[:, kc, fc * 128:(fc + 1) * 128], rhs=xn_s[:, kc, :],
                                                                        start=(kc == 0), stop=(kc == 7)),
                      reads=[w1_trk, xn_k[kc]], writes=[pk])
            rb = fc % 4
            kb.op("scalar", lambda h, ps=ps, rb=rb: h.activation(out=r_s[rb][:, :], in_=ps[:, :TT], func=AF.Relu),
                  reads=[pk], writes=[r_k[rb]])
            kb.op("gpsimd", lambda h, fc=fc, rb=rb: h.tensor_tensor(out=a_s[:, fc, :], in0=r_s[rb][:, :], in1=r_s[rb][:, :], op=ALU.mult),
                  reads=[r_k[rb]], writes=[a_k[fc]])
        for oc in range(8):
            ps, pk = pp.get()
            for fc in range(32):
                kb.op("tensor", lambda h, fc=fc, oc=oc, ps=ps: h.matmul(ps[:, :TT], lhsT=w2_t[:, fc, oc * 128:(oc + 1) * 128], rhs=a_s[:, fc, :],
                                                                        start=(fc == 0), stop=(fc == 31)),
                      reads=[w2_trk, a_k[fc]], writes=[pk])
            kb.op("vector", lambda h, oc=oc, ps=ps: h.tensor_tensor(out=h_s[b][:, oc, :], in0=h_s[b][:, oc, :], in1=ps[:, :TT], op=ALU.add),
                  reads=[pk], writes=[h_k[b][oc]])
        kb.dma("sync", hTo_v[:, :, t0:t0 + TT], h_s[b][:, :, :], reads=h_k[b], is_output=True)
        rmsnorm_fm(kb, pp, [h_s[b][:, c, :] for c in range(8)], h_k[b], gn_t, gn_trk, ones[:, :], ones_trk,
                   [hn_s[:, c, :] for c in range(8)], hn_k, scr, 8, TT, D)
        kb.dma("sync", hno_v[:, :, t0:t0 + TT], hn_s[:, :, :], reads=hn_k, is_output=True)
    return kb.finish(), kb


EV_IN = 1736
C_Q, C_KV, C_QI, C_KI, C_WI, C_U = 0, 512, 640, 1152, 1216, 1224
HALO = 16


def build_e1(T):
    kb = KB()
    nc = kb.nc
    TT = 512
    NT = T // TT
    W = TT + HALO
    hnT = kb.dram("hnT", [D, HALO + T], BF16, "ExternalInput")
    w_in = kb.dram("w_in", [D, EV_IN], F32, "ExternalInput")
    w_rot = kb.dram("w_rot", [D, 1088], F32, "ExternalInput")
    kvn = kb.dram("kvn", [128], F32, "ExternalInput")
    w_uk = kb.dram("w_uk", [128, 64], F32, "ExternalInput")
    w_ukr = kb.dram("w_ukr", [128, 64], F32, "ExternalInput")
    w_uv = kb.dram("w_uv", [128, 64], F32, "ExternalInput")
    pool_w = kb.dram("pool_w", [4, 128, 128], F32, "ExternalInput")
    pool_sc = kb.dram("pool_sc", [512], F32, "ExternalInput")
    cosT = kb.dram("cosT", [128, T], F32, "ExternalInput")
    sinT = kb.dram("sinT", [128, T], F32, "ExternalInput")
    invc0 = kb.dram("invc0", [128, 4, TT], F32, "ExternalInput")
    qT_o = kb.dram("qT", [512, T], BF16, "ExternalOutput")
    qiT_o = kb.dram("qiT", [512, T], BF16, "ExternalOutput")
    kiT_o = kb.dram("kiT", [64, T], BF16, "ExternalOutput")
    kT_o = kb.dram("kT", [64, T], BF16, "ExternalOutput")
    v_o = kb.dram("v", [T, 64], BF16, "ExternalOutput")
    wiT_o = kb.dram("wiT", [8, T], F32, "ExternalOutput")
    ybT_o = kb.dram("ybT", [512, T], BF16, "ExternalOutput")

    win_t, win_k = load_weight_bf16(kb, "win", w_in, D, EV_IN)
    wrot_t, wrot_k = load_weight_bf16(kb, "wrot", w_rot, D, 1088)
    wuk_t, wuk_k = load_weight_bf16(kb, "wuk", w_uk, 128, 64)
    wukr_t, wukr_k = load_weight_bf16(kb, "wukr", w_ukr, 128, 64)
    wuv_t, wuv_k = load_weight_bf16(kb, "wuv", w_uv, 128, 64)
    pw_t = kb.sbuf("pw", [128, 4, 128], BF16)
    pw_k = Trk()
    kb.dma("gpsimd", pw_t[:, :, :], pool_w.rearrange("g c d -> c g d"), writes=[pw_k])
    psc_t, psc_k = load_vec_col(kb, "psc", pool_sc, 512)
    kvn_t, kvn_k = load_vec_col(kb, "kvn_s", kvn, 128)
    invc0_t = kb.sbuf("invc0_s", [128, 4, TT], F32)
    invc0_k = Trk()
    kb.dma("sync", invc0_t[:, :, :], invc0[:, :, :], writes=[invc0_k])
    invc_t = kb.sbuf("invc_s", [128, 4, TT], F32)
    invc_k = Trk()
    for g in range(4):
        kb.op("vector", lambda h, g=g: h.memset(invc_t[:, g, :], 1.0 / (2 ** (g + 1))), writes=[invc_k])
    ones = kb.sbuf("ones", [128, 128], BF16)
    ones_trk = Trk()
    kb.op("vector", lambda h: h.memset(ones[:], 1.0), writes=[ones_trk])
    eps_t = kb.sbuf("eps", [128, 1], F32)
    eps_trk = Trk()
    kb.op("vector", lambda h: h.memset(eps_t[:], EPS), writes=[eps_trk])

    pp = PsumPool(kb)
    NB = 2
    hn_s = [kb.sbuf("hn%d" % b, [128, 8, TT], BF16) for b in range(NB)]
    hn_k = [Trk() for _ in range(NB)]
    halo_s = kb.sbuf("halo", [128, 8, HALO], BF16)
    halo_k = Trk()
    cs_s = [kb.sbuf("cs%d" % b, [128, 2, TT], F32) for b in range(NB)]
    cs_k = [Trk() for _ in range(NB)]
    t1_s = [kb.sbuf("t1_%d" % b, [128, TT], F32) for b in range(2)]
    t1_k = [Trk() for _ in range(2)]
    t2_s = [kb.sbuf("t2_%d" % b, [128, TT], F32) for b in range(2)]
    t2_k = [Trk() for _ in range(2)]
    q_s = kb.sbuf("q_s", [128, 4, TT], BF16)
    q_k = Trk()
    qi_s = kb.sbuf("qi_s", [128, 4, TT], BF16)
    qi_k = Trk()
    ki_s = kb.sbuf("ki_s", [64, TT], BF16)
    ki_k = Trk()
    k_s = kb.sbuf("k_s", [64, TT], BF16)
    k_k = Trk()
    v_s = kb.sbuf("v_s", [128, 4, 64], BF16)
    v_k = Trk()
    wi_s = kb.sbuf("wi_s", [8, TT], F32)
    wi_k = Trk()
    ckv_s = kb.sbuf("ckv_s", [128, TT], F32)
    ckv_k = Trk()
    ckvn_s = kb.sbuf("ckvn_s", [128, TT], BF16)
    ckvn_k = Trk()
    sq_s = kb.sbuf("sq", [128, TT], BF16)
    sq_k = Trk()
    rstd_s = kb.sbuf("rstd", [128, TT], F32)
    rstd_k = Trk()
    u_s = [kb.sbuf("u%d" % g, [128, W], F32) for g in range(4)]
    u_k = [Trk() for _ in range(4)]
    sA = kb.sbuf("sA", [128, W], F32)
    sA_k = Trk()
    sB = kb.sbuf("sB", [128, W], F32)
    sB_k = Trk()
    pl_s = [kb.sbuf("pl%d" % g, [128, TT], BF16) for g in range(4)]
    pl_k = [Trk() for _ in range(4)]
    yb_s = kb.sbuf("yb_s", [128, 4, TT], BF16)
    yb_k = Trk()
    scr = {"sq": [(sq_s[:, :], sq_k)], "rstd": (rstd_s[:, :], rstd_k), "eps": (eps_t[:, 0:1], eps_trk)}

    hn_v = hnT.rearrange("(c p) t -> p c t", p=128)
    rope_i = [0]

    def load(i):
        b = i % NB
        t0 = i * TT
        kb.dma("sync", hn_s[b][:, :, :], hn_v[:, :, HALO + t0:HALO + t0 + TT], writes=[hn_k[b]])
        kb.dma("sync", cs_s[b][:, 0, :], cosT[:, t0:t0 + TT], writes=[cs_k[b]])
        kb.dma("sync", cs_s[b][:, 1, :], sinT[:, t0:t0 + TT], writes=[cs_k[b]])

    def proj(ps, pk, wt, wk, c0, c1, rhs_fn, rk, N):
        M = c1 - c0
        for kc in range(8):
            kb.op("tensor", lambda h, kc=kc: h.matmul(ps[:M, :N], lhsT=wt[:, kc, c0:c1], rhs=rhs_fn(kc), start=(kc == 0), stop=(kc == 7)),
                  reads=[wk, rk], writes=[pk])

    def rope(b, psa, pka, psb, pkb, M, out_ap, out_k):
        j = rope_i[0] % 2
        rope_i[0] += 1
        kb.op("vector", lambda h: h.tensor_tensor(out=t1_s[j][:M, :], in0=psa[:M, :TT], in1=cs_s[b][:M, 0, :], op=ALU.mult),
              reads=[pka, cs_k[b]], writes=[t1_k[j]])
        kb.op("vector", lambda h: h.tensor_tensor(out=t2_s[j][:M, :], in0=psb[:M, :TT], in1=cs_s[b][:M, 1, :], op=ALU.mult),
              reads=[pkb, cs_k[b]], writes=[t2_k[j]])
        kb.op("gpsimd", lambda h: h.tensor_tensor(out=out_ap, in0=t1_s[j][:M, :], in1=t2_s[j][:M, :], op=ALU.add),
              reads=[t1_k[j], t2_k[j]], writes=[out_k])

    kb.dma("sync", halo_s[:, :, :], hn_v[:, :, 0:HALO], writes=[halo_k])
    load(0)
    for i in range(NT):
        b = i % NB
        t0 = i * TT
        if i + 1 < NT:
            load(i + 1)
        rhs_fn = lambda kc, b=b: hn_s[b][:, kc, :]
        for (cbase, rbase, dst, dk) in ((C_Q, 0, q_s, q_k), (C_QI, 512, qi_s, qi_k)):
            for c in range(4):
                psa, pka = pp.get()
                proj(psa, pka, win_t, win_k, cbase + c * 128, cbase + (c + 1) * 128, rhs_fn, hn_k[b], TT)
                psb, pkb = pp.get()
                proj(psb, pkb, wrot_t, wrot_k, rbase + c * 128, rbase + (c + 1) * 128, rhs_fn, hn_k[b], TT)
                rope(b, psa, pka, psb, pkb, 128, dst[:, c, :], dk)
        kb.dma("sync", qT_o.rearrange("(c p) t -> p c t", p=128)[:, :, t0:t0 + TT], q_s[:, :, :], reads=[q_k], is_output=True)
        kb.dma("sync", qiT_o.rearrange("(c p) t -> p c t", p=128)[:, :, t0:t0 + TT], qi_s[:, :, :], reads=[qi_k], is_output=True)
        psa, pka = pp.get()
        proj(psa, pka, win_t, win_k, C_KI, C_KI + 64, rhs_fn, hn_k[b], TT)
        psb, pkb = pp.get()
        proj(psb, pkb, wrot_t, wrot_k, 1024, 1088, rhs_fn, hn_k[b], TT)
        rope(b, psa, pka, psb, pkb, 64, ki_s[:, :], ki_k)
        kb.dma("sync", kiT_o[:, t0:t0 + TT], ki_s[:, :], reads=[ki_k], is_output=True)
        ps, pk = pp.get()
        proj(ps, pk, win_t, win_k, C_WI, C_WI + 8, rhs_fn, hn_k[b], TT)
        kb.op("scalar", lambda h, ps=ps: h.activation(out=wi_s[:, :], in_=ps[:8, :TT], func=AF.Copy, scale=float(8 ** -0.5 * 64 ** -0.5)),
              reads=[pk], writes=[wi_k])
        kb.dma("sync", wiT_o[:, t0:t0 + TT], wi_s[:, :], reads=[wi_k], is_output=True)
        ps, pk = pp.get()
        proj(ps, pk, win_t, win_k, C_KV, C_KV + 128, rhs_fn, hn_k[b], TT)
        kb.op("scalar", lambda h, ps=ps: h.activation(out=ckv_s[:, :], in_=ps[:, :TT], func=AF.Copy), reads=[pk], writes=[ckv_k])
        rmsnorm_fm(kb, pp, [ckv_s[:, :]], [ckv_k], kvn_t, kvn_k, ones[:, :], ones_trk, [ckvn_s[:, :]], [ckvn_k], scr, 1, TT, 128)
        psa, pka = pp.get()
        kb.op("tensor", lambda h, psa=psa: h.matmul(psa[:64, :TT], lhsT=wuk_t[:, 0, :], rhs=ckvn_s[:, :], start=True, stop=True),
              reads=[wuk_k, ckvn_k], writes=[pka])
        psb, pkb = pp.get()
        kb.op("tensor", lambda h, psb=psb: h.matmul(psb[:64, :TT], lhsT=wukr_t[:, 0, :], rhs=ckvn_s[:, :], start=True, stop=True),
              reads=[wukr_k, ckvn_k], writes=[pkb])
        rope(b, psa, pka, psb, pkb, 64, k_s[:, :], k_k)
        kb.dma("sync", kT_o[:, t0:t0 + TT], k_s[:, :], reads=[k_k], is_output=True)
        ps, pk = pp.get()
        for j in range(4):
            kb.op("tensor", lambda h, ps=ps, j=j: h.matmul(ps[:, j * 64:(j + 1) * 64], lhsT=ckvn_s[:, j * 128:(j + 1) * 128], rhs=wuv_t[:, 0, :],
                                                           start=True, stop=True),
                  reads=[wuv_k, ckvn_k], writes=[pk])
        kb.op("scalar", lambda h, ps=ps: h.activation(out=v_s[:, :, :], in_=ps[:, 0:256].rearrange("p (j d) -> p j d", d=64), func=AF.Copy),
              reads=[pk], writes=[v_k])
        kb.dma("sync", v_o[t0:t0 + TT, :].rearrange("(j p) d -> p j d", p=128), v_s[:, :, :], reads=[v_k], is_output=True)
        for g in range(4):
            w = 2 ** (g + 1)
            if i == 0:
                ps, pk = pp.get()
                proj(ps, pk, win_t, win_k, C_U + g * 128, C_U + (g + 1) * 128, lambda kc: halo_s[:, kc, :], halo_k, HALO)
                kb.op("scalar", lambda h, ps=ps, g=g: h.activation(out=u_s[g][:, 0:HALO], in_=ps[:, :HALO], func=AF.Copy),
                      reads=[pk], writes=[u_k[g]])
            else:
                kb.op("gpsimd", lambda h, g=g: h.tensor_copy(out=u_s[g][:, 0:HALO], in_=u_s[g][:, TT:TT + HALO]),
                      reads=[u_k[g]], writes=[u_k[g]])
            ps, pk = pp.get()
            proj(ps, pk, win_t, win_k, C_U + g * 128, C_U + (g + 1) * 128, rhs_fn, hn_k[b], TT)
            kb.op("scalar", lambda h, ps=ps, g=g: h.activation(out=u_s[g][:, HALO:W], in_=ps[:, :TT], func=AF.Copy),
                  reads=[pk], writes=[u_k[g]])
            src, srck = u_s[g], u_k[g]
            bufs = [(sA, sA_k), (sB, sB_k)]
            sh = 1
            lo = 0
            for step in range(g + 1):
                dst, dstk = bufs[step % 2]
                lo = lo + sh
                kb.op("gpsimd", lambda h, src=src, dst=dst, lo=lo, sh=sh: h.tensor_tensor(out=dst[:, lo:W], in0=src[:, lo:W], in1=src[:, lo - sh:W - sh], op=ALU.add),
                      reads=[srck], writes=[dstk])
                src, srck = dst, dstk
                sh *= 2
            tab, tabk = (invc0_t, invc0_k) if i == 0 else (invc_t, invc_k)
            j = rope_i[0] % 2
            rope_i[0] += 1
            kb.op("gpsimd", lambda h, src=src, tab=tab, g=g, j=j: h.tensor_tensor(out=t1_s[j][:, :], in0=src[:, HALO:W], in1=tab[:, g, :], op=ALU.mult),
                  reads=[srck, tabk], writes=[t1_k[j]])
            kb.op("gpsimd", lambda h, g=g, j=j: h.tensor_tensor(out=pl_s[g][:, :], in0=t1_s[j][:, :], in1=u_s[g][:, HALO:W], op=ALU.subtract),
                  reads=[t1_k[j], u_k[g]], writes=[pl_k[g]])
            ps, pk = pp.get()
            kb.op("tensor", lambda h, ps=ps, g=g: h.matmul(ps[:, :TT], lhsT=pw_t[:, g, :], rhs=pl_s[g][:, :], start=True, stop=True),
                  reads=[pw_k, pl_k[g]], writes=[pk])
            kb.op("scalar", lambda h, ps=ps, g=g: h.activation(out=yb_s[:, g, :], in_=ps[:, :TT], func=AF.Copy, scale=psc_t[:, g:g + 1]),
                  reads=[pk, psc_k], writes=[yb_k])
        kb.dma("sync", ybT_o.rearrange("(c p) t -> p c t", p=128)[:, :, t0:t0 + TT], yb_s[:, :, :], reads=[yb_k], is_output=True)
    return kb.finish(), kb


TOPK = 256
NEG = -1.0e30


def build_e2(NQB, NIT=20, blk_list=None):
    kb = KB()
    nc = kb.nc
    NQ = NQB * 128
    nmax = 2 * NQB
    L = nmax * 128
    qh = kb.dram("qh", [64, 8, NQ], BF16, "ExternalInput")
    qih = kb.dram("qih", [64, 8, NQ], BF16, "ExternalInput")
    wi_tok = kb.dram("wi_tok", [128, NQB, 8], F32, "ExternalInput")
    qrel = kb.dram("qrel", [128, NQB], F32, "ExternalInput")
    kiT = kb.dram("kiT", [64, L], BF16, "ExternalInput")
    kT = kb.dram("kT", [64, L], BF16, "ExternalInput")
    vv = kb.dram("v", [128, nmax, 64], BF16, "ExternalInput")
    iota_in = kb.dram("iota", [128, 256], F32, "ExternalInput")
    ident_in = kb.dram("ident", [128, 128], BF16, "ExternalInput")
    ya_o = kb.dram("ya", [128, NQB, 512], BF16, "ExternalOutput")

    def ld(name, ap, shape, dt):
        t = kb.sbuf(name, shape, dt)
        k = Trk()
        kb.dma("sync", t[tuple(slice(None) for _ in shape)], ap, writes=[k])
        return t, k

    ki_t, ki_k = ld("ki_t", kiT[:, :], [64, L], BF16)
    k_t, k_k = ld("k_t", kT[:, :], [64, L], BF16)
    v_t, v_k = ld("v_t", vv[:, :, :], [128, nmax, 64], BF16)
    wi_t, wi_k = ld("wi_t", wi_tok[:, :, :], [128, NQB, 8], F32)
    qr_t, qr_k = ld("qr_t", qrel[:, :], [128, NQB], F32)
    io_t, io_k = ld("io_t", iota_in[:, :], [128, 256], F32)
    id_t, id_k = ld("id_t", ident_in[:, :], [128, 128], BF16)

    ps_f = [kb.psum("psf%d" % i, [128, 512], F32) for i in range(5)]
    ps_fk = [Trk() for _ in range(5)]
    ps_i = [0]

    def getps():
        j = ps_i[0] % 5
        ps_i[0] += 1
        return ps_f[j], ps_fk[j]

    ps_t = [kb.psum("pst%d" % i, [128, 1024], BF16) for i in range(2)]
    ps_tk = [Trk() for _ in range(2)]
    ps_o = kb.psum("pso", [128, 512], F32)
    ps_ok = Trk()

    q_s = [kb.sbuf("q_s%d" % b, [64, 8, 128], BF16) for b in range(2)]
    q_k = [Trk() for _ in range(2)]
    qi_s = [kb.sbuf("qi_s%d" % b, [64, 8, 128], BF16) for b in range(2)]
    qi_k = [Trk() for _ in range(2)]
    score = kb.sbuf("score", [128, L], F32)
    score_k = Trk()
    lg = kb.sbuf("lg", [128, L], F32)
    lg_k = Trk()
    P_s = kb.sbuf("P_s", [128, L], BF16)
    P_k = Trk()
    PT_s = kb.sbuf("PT_s", [128, nmax, 128], BF16)
    PT_k = Trk()
    junk = kb.sbuf("junk", [128, L], BF16)
    junk_k = Trk()
    r_s = [kb.sbuf("r%d" % b, [128, 512], F32) for b in range(4)]
    r_k = [Trk() for _ in range(4)]
    mb_s = kb.sbuf("mb", [128, 256], F32)
    mb_k = Trk()
    sm = kb.sbuf("sm", [128, 16], F32)
    lo_k, w0_k, mid_k, cnt_k, tmp_k, hi_k, negm_k, rinv_k = (Trk() for _ in range(8))
    LO, W0, MID, CNT, TMP, HI, NEGM, RINV = (sm[:, j:j + 1] for j in range(8))
    rs = kb.sbuf("rs", [128, 8], F32)
    rs_k = Trk()
    ya_s = [kb.sbuf("ya_s%d" % b, [128, 512], BF16) for b in range(2)]
    ya_k = [Trk() for _ in range(2)]

    def loadq(i):
        b = i % 2
        kb.dma("sync", q_s[b][:, :, :], qh[:, :, i * 128:(i + 1) * 128], writes=[q_k[b]])
        kb.dma("sync", qi_s[b][:, :, :], qih[:, :, i * 128:(i + 1) * 128], writes=[qi_k[b]])

    blocks = list(range(NQB)) if blk_list is None else blk_list
    loadq(blocks[0])
    rb = 0
    for bi, i in enumerate(blocks):
        b = i % 2
        if bi + 1 < len(blocks):
            loadq(blocks[bi + 1])
        n = 2 * i + 2
        nk = 128 * n
        tiles = [(k0, min(512, nk - k0)) for k0 in range(0, nk, 512)]
        for (k0, kw) in tiles:
            for h in range(8):
                ps, pk = getps()
                kb.op("tensor", lambda e, ps=ps, h=h, k0=k0, kw=kw: e.matmul(ps[:, :kw], lhsT=qi_s[b][:, h, :], rhs=ki_t[:, k0:k0 + kw], start=True, stop=True),
                      reads=[qi_k[b], ki_k], writes=[pk])
                j = rb % 4
                rb += 1
                kb.op("scalar", lambda e, ps=ps, j=j, kw=kw: e.activation(out=r_s[j][:, :kw], in_=ps[:, :kw], func=AF.Relu), reads=[pk], writes=[r_k[j]])
                if h == 0:
                    kb.op("vector", lambda e, j=j, k0=k0, kw=kw: e.tensor_scalar(out=score[:, k0:k0 + kw], in0=r_s[j][:, :kw], scalar1=wi_t[:, i, 0:1], scalar2=None, op0=ALU.mult),
                          reads=[r_k[j], wi_k], writes=[score_k])
                else:
                    kb.op("vector", lambda e, j=j, k0=k0, kw=kw, h=h: e.scalar_tensor_tensor(out=score[:, k0:k0 + kw], in0=r_s[j][:, :kw], scalar=wi_t[:, i, h:h + 1],
                                                                                              in1=score[:, k0:k0 + kw], op0=ALU.mult, op1=ALU.add),
                          reads=[r_k[j], wi_k], writes=[score_k])
        kb.op("vector", lambda e: e.tensor_reduce(out=LO, in_=score[:, :nk], axis=AX.X, op=ALU.min), reads=[score_k], writes=[lo_k])
        kb.op("vector", lambda e: e.tensor_scalar(out=mb_s[:, :], in0=io_t[:, :], scalar1=qr_t[:, i:i + 1], scalar2=NEG, op0=ALU.is_gt, op1=ALU.mult),
              reads=[io_k, qr_k], writes=[mb_k])
        kb.op("gpsimd", lambda e: e.tensor_tensor(out=score[:, nk - 256:nk], in0=score[:, nk - 256:nk], in1=mb_s[:, :], op=ALU.add),
              reads=[mb_k, lo_k], writes=[score_k])
        kb.op("vector", lambda e: e.tensor_reduce(out=HI, in_=score[:, :nk], axis=AX.X, op=ALU.max), reads=[score_k], writes=[hi_k])
        kb.op("vector", lambda e: e.tensor_tensor(out=W0, in0=HI, in1=LO, op=ALU.subtract), reads=[hi_k, lo_k], writes=[w0_k])
        for it in range(NIT):
            f = 2.0 ** -(it + 1)
            kb.op("vector", lambda e, f=f: e.scalar_tensor_tensor(out=MID, in0=W0, scalar=f, in1=LO, op0=ALU.mult, op1=ALU.add),
                  reads=[w0_k, lo_k], writes=[mid_k])
            kb.op("vector", lambda e: e.tensor_scalar(out=junk[:, :nk], in0=score[:, :nk], scalar1=MID, scalar2=None, op0=ALU.is_ge, op1=ALU.add, accum_out=CNT),
                  reads=[score_k, mid_k], writes=[junk_k, cnt_k])
            kb.op("vector", lambda e: e.tensor_scalar(out=TMP, in0=CNT, scalar1=float(TOPK) - 0.5, scalar2=W0, op0=ALU.is_ge, op1=ALU.mult),
                  reads=[cnt_k, w0_k], writes=[tmp_k])
            kb.op("vector", lambda e, f=f: e.scalar_tensor_tensor(out=LO, in0=TMP, scalar=f, in1=LO, op0=ALU.mult, op1=ALU.add),
                  reads=[tmp_k], writes=[lo_k])
        kb.op("vector", lambda e: e.tensor_scalar(out=score[:, :nk], in0=score[:, :nk], scalar1=LO, scalar2=NEG, op0=ALU.is_lt, op1=ALU.mult),
              reads=[lo_k], writes=[score_k])
        yb_ = bi % 2
        for h in range(8):
            for (k0, kw) in tiles:
                ps, pk = getps()
                kb.op("tensor", lambda e, ps=ps, h=h, k0=k0, kw=kw: e.matmul(ps[:, :kw], lhsT=q_s[b][:, h, :], rhs=k_t[:, k0:k0 + kw], start=True, stop=True),
                      reads=[q_k[b], k_k], writes=[pk])
                kb.op("vector", lambda e, ps=ps, k0=k0, kw=kw: e.scalar_tensor_tensor(out=lg[:, k0:k0 + kw], in0=ps[:, :kw], scalar=0.125, in1=score[:, k0:k0 + kw],
                                                                                       op0=ALU.mult, op1=ALU.add),
                      reads=[pk, score_k], writes=[lg_k])
            kb.op("vector", lambda e: e.tensor_reduce(out=NEGM, in_=lg[:, :nk], axis=AX.X, op=ALU.max, negate=True), reads=[lg_k], writes=[negm_k])
            kb.op("scalar", lambda e, h=h: e.activation(out=P_s[:, :nk], in_=lg[:, :nk], func=AF.Exp, bias=NEGM, scale=1.0, accum_out=rs[:, h:h + 1]),
                  reads=[lg_k, negm_k], writes=[P_k, rs_k])
            for j0 in range(0, n, 8):
                jn = min(8, n - j0)
                tb = (j0 // 8) % 2
                for j in range(j0, j0 + jn):
                    s = j - j0
                    kb.op("tensor", lambda e, tb=tb, s=s, j=j: e.transpose(out=ps_t[tb][:, s * 128:(s + 1) * 128], in_=P_s[:, j * 128:(j + 1) * 128], identity=id_t[:, :]),
                          reads=[P_k, id_k], writes=[ps_tk[tb]])
                kb.op("scalar", lambda e, tb=tb, j0=j0, jn=jn: e.activation(out=PT_s[:, j0:j0 + jn, :], in_=ps_t[tb][:, 0:jn * 128].rearrange("p (j q) -> p j q", q=128), func=AF.Copy),
                      reads=[ps_tk[tb]], writes=[PT_k])
            for j in range(n):
                kb.op("tensor", lambda e, j=j, h=h: e.matmul(ps_o[:, h * 64:(h + 1) * 64], lhsT=PT_s[:, j, :], rhs=v_t[:, j, :], start=(j == 0), stop=(j == n - 1)),
                      reads=[PT_k, v_k], writes=[ps_ok])
        kb.op("vector", lambda e: e.reciprocal(out=rs[:, :], in_=rs[:, :]), reads=[rs_k], writes=[rs_k])
        for h in range(8):
            kb.op("scalar", lambda e, h=h: e.activation(out=ya_s[yb_][:, h * 64:(h + 1) * 64], in_=ps_o[:, h * 64:(h + 1) * 64], func=AF.Copy, scale=rs[:, h:h + 1]),
                  reads=[ps_ok, rs_k], writes=[ya_k[yb_]])
        kb.dma("sync", ya_o[:, i, :], ya_s[yb_][:, :], reads=[ya_k[yb_]], is_output=True)
    return kb.finish(), kb


def gdn_consts():
    i = np.arange(128)
    same = (i[:, None] // 64) == (i[None, :] // 64)
    c = {}
    c["ident"] = np.eye(128, dtype=np.float32)
    c["ones"] = np.ones((128, 128), np.float32)
    c["tri"] = (same & (i[:, None] <= i[None, :])).astype(np.float32)
    c["selblk"] = (i[:, None] == (i[None, :] // 64) * 64 + 63).astype(np.float32)
    c["selA"] = np.repeat((i[:, None] == 63), 128, 1).astype(np.float32)
    c["selB"] = np.repeat((i[:, None] == 127), 128, 1).astype(np.float32)
    c["mstrict"] = np.where(same & (i[:, None] > i[None, :]), 0.0, NEG).astype(np.float32)
    c["mTfull"] = np.where(same & (i[None, :] >= i[:, None]), 0.0, NEG).astype(np.float32)
    c["sT01"] = (same & (i[None, :] > i[:, None])).astype(np.float32)
    return c


GDN_CONST_NAMES = ["ident", "ones", "tri", "selblk", "selA", "selB", "mstrict", "mTfull", "sT01"]


def build_o1(L, NH=4):
    kb = KB()
    nc = kb.nc
    TT = 512
    NT = L // TT
    HW = NH * 128
    hnT = kb.dram("hnT", [D, L], BF16, "ExternalInput")
    w_qkv = kb.dram("w_qkv", [D, 3 * HW], F32, "ExternalInput")
    w_z = kb.dram("w_z", [D, HW], F32, "ExternalInput")
    w_ba = kb.dram("w_ba", [D, 2 * NH], F32, "ExternalInput")
    cw_in = kb.dram("cw", [128, 3 * NH * 4], F32, "ExternalInput")
    alog_in = kb.dram("alog_b", [128, NH], F32, "ExternalInput")
    dtb_in = kb.dram("dtb_b", [128, NH], F32, "ExternalInput")
    onorm_in = kb.dram("onorm_b", [128, 128], F32, "ExternalInput")
    cst_in = {n: kb.dram("c_" + n, [128, 128], F32, "ExternalInput") for n in GDN_CONST_NAMES}
    o_out = kb.dram("o_tok", [128, L // 128, HW], BF16, "ExternalOutput")

    def ld(name, ap, shape, dt):
        t = kb.sbuf(name, shape, dt)
        k = Trk()
        kb.dma("sync", t[tuple(slice(None) for _ in shape)], ap, writes=[k])
        return t, k

    wqkv_t, wqkv_k = load_weight_bf16(kb, "wqkv", w_qkv, D, 3 * HW)
    wz_t, wz_k = load_weight_bf16(kb, "wz", w_z, D, HW)
    wba_t, wba_k = load_weight_bf16(kb, "wba", w_ba, D, 2 * NH)
    cw_t, cw_k = ld("cw_t", cw_in[:, :], [128, 3 * NH * 4], F32)
    alog_t, alog_k = ld("alog_t", alog_in[:, :], [128, NH], F32)
    dtb_t, dtb_k = ld("dtb_t", dtb_in[:, :], [128, NH], F32)
    onorm_t, onorm_k = ld("onorm_t", onorm_in[:, :], [128, 128], F32)
    C = {}
    CK = {}
    for n in GDN_CONST_NAMES:
        C[n], CK[n] = ld("cs_" + n, cst_in[n][:, :], [128, 128], F32)
    ones_bf = kb.sbuf("ones_bf", [128, 128], BF16)
    ones_bfk = Trk()
    kb.op("vector", lambda e: e.memset(ones_bf[:], 1.0), writes=[ones_bfk])
    cst = kb.sbuf("cst", [128, 2], F32)
    cst_k = Trk()
    kb.op("vector", lambda e: e.memset(cst[:, 0:1], EPS), writes=[cst_k])
    kb.op("vector", lambda e: e.memset(cst[:, 1:2], 1.0), writes=[cst_k])
    EPSC, ONEC = cst[:, 0:1], cst[:, 1:2]
    ea_t = kb.sbuf("ea_t", [128, NH], F32)
    ea_k = Trk()
    kb.op("scalar", lambda e: e.activation(out=ea_t[:, :], in_=alog_t[:, :], func=AF.Exp), reads=[alog_k], writes=[ea_k])

    pp = PsumPool(kb)
    hn_s = [kb.sbuf("hn%d" % b, [128, 8, TT], BF16) for b in range(2)]
    hn_k = [Trk() for _ in range(2)]
    XW = TT + 3
    x_ext = [[kb.sbuf("x%d_%d" % (ty, h), [128, XW], F32) for h in range(NH)] for ty in range(3)]
    x_k = [[Trk() for _ in range(NH)] for _ in range(3)]
    xc = [[kb.sbuf("xc%d_%d" % (ty, h), [128, TT], F32) for h in range(NH)] for ty in range(3)]
    xc_k = [[Trk() for _ in range(NH)] for _ in range(3)]
    acc_s = [kb.sbuf("acc%d" % b, [128, TT], F32) for b in range(2)]
    acc_k = [Trk() for _ in range(2)]
    sq_s = kb.sbuf("sq", [128, TT], BF16)
    sq_k = Trk()
    rn_s = kb.sbuf("rn", [128, TT], F32)
    rn_k = Trk()
    gz_s = [kb.sbuf("gz%d" % j, [128, HW], F32) for j in range(4)]
    gz_k = [Trk() for _ in range(4)]
    NS = 10
    sc_s = [kb.sbuf("sc%d" % j, [128, NS, NH], F32) for j in range(4)]
    sc_k = [[Trk() for _ in range(NS)] for _ in range(4)]
    BETA, GC, EG, EDL, EGLA, EGLB, BEG, GG, T0, T1 = range(NS)
    S_s = [kb.sbuf("S%d" % h, [128, 128], F32) for h in range(NH)]
    S_k = [Trk() for _ in range(NH)]
    for h in range(NH):
        kb.op("gpsimd", lambda e, h=h: e.memset(S_s[h][:, :], 0.0), writes=[S_k[h]])
    for ty in range(3):
        for h in range(NH):
            kb.op("gpsimd", lambda e, ty=ty, h=h: e.memset(x_ext[ty][h][:, 0:3], 0.0), writes=[x_k[ty][h]])
    o_s = [kb.sbuf("o_s%d" % b, [128, HW], BF16) for b in range(2)]
    o_k = [Trk() for _ in range(2)]

    NSET = 2
    TN = ["kbe", "kdec", "vb", "dg", "egf", "decT", "decS", "WT", "nkk", "M", "MT", "M2", "M2T", "TT", "attnT", "qdecT", "val", "kcdT", "vn", "tmpa", "tmpb"]
    tmp = [{n: kb.sbuf("t%d_%s" % (s, n), [128, 256 if n == "dg" else 128], F32) for n in TN} for s in range(NSET)]
    tmk = [{n: Trk() for n in TN} for s in range(NSET)]
    ss_s = kb.sbuf("ss_s", [128, 4], F32)
    ss_k = Trk()
    junk = kb.sbuf("junk_o", [128, 128], F32)
    junk_k = Trk()

    hn_v = hnT.rearrange("(c p) t -> p c t", p=128)

    def load(i):
        b = i % 2
        kb.dma("sync", hn_s[b][:, :, :], hn_v[:, :, i * TT:(i + 1) * TT], writes=[hn_k[b]])

    def mm(ps, pk, M, N, lhsT, rhs, reads, start=True, stop=True, p0=0):
        kb.op("tensor", lambda e: e.matmul(ps[p0:p0 + M, :N], lhsT=lhsT, rhs=rhs, start=start, stop=stop), reads=reads, writes=[pk])

    load(0)
    unit = 0
    for i in range(NT):
        b = i % 2
        if i + 1 < NT:
            load(i + 1)
        for ty in range(3):
            for h in range(NH):
                ps, pk = pp.get()
                c0 = ty * HW + h * 128
                for kc in range(8):
                    mm(ps, pk, 128, TT, wqkv_t[:, kc, c0:c0 + 128], hn_s[b][:, kc, :], [wqkv_k, hn_k[b]], kc == 0, kc == 7)
                if i > 0:
                    kb.op("gpsimd", lambda e, ty=ty, h=h: e.tensor_copy(out=x_ext[ty][h][:, 0:3], in_=x_ext[ty][h][:, TT:TT + 3]),
                          reads=[x_k[ty][h]], writes=[x_k[ty][h]])
                kb.op("scalar", lambda e, ps=ps, ty=ty, h=h: e.activation(out=x_ext[ty][h][:, 3:XW], in_=ps[:, :TT], func=AF.Copy),
                      reads=[pk], writes=[x_k[ty][h]])
        for j in range(4):
            ps, pk = pp.get()
            for kc in range(8):
                mm(ps, pk, 128, HW, hn_s[b][:, kc, j * 128:(j + 1) * 128], wz_t[:, kc, :], [wz_k, hn_k[b]], kc == 0, kc == 7)
            kb.op("scalar", lambda e, ps=ps, j=j: e.activation(out=gz_s[j][:, :], in_=ps[:, :HW], func=AF.Silu), reads=[pk], writes=[gz_k[j]])
            for h in range(NH):
                kb.op("gpsimd", lambda e, j=j, h=h: e.tensor_tensor(out=gz_s[j][:, h * 128:(h + 1) * 128], in0=gz_s[j][:, h * 128:(h + 1) * 128], in1=onorm_t[:, :], op=ALU.mult),
                      reads=[onorm_k], writes=[gz_k[j]])
            ps, pk = pp.get()
            for kc in range(8):
                mm(ps, pk, 128, 2 * NH, hn_s[b][:, kc, j * 128:(j + 1) * 128], wba_t[:, kc, :], [wba_k, hn_k[b]], kc == 0, kc == 7)
            sc, sk = sc_s[j], sc_k[j]
            kb.op("scalar", lambda e, ps=ps, sc=sc: e.activation(out=sc[:, BETA, :], in_=ps[:, 0:NH], func=AF.Sigmoid), reads=[pk], writes=[sk[BETA]])
            kb.op("vector", lambda e, ps=ps, sc=sc: e.tensor_tensor(out=sc[:, T0, :], in0=ps[:, NH:2 * NH], in1=dtb_t[:, :], op=ALU.add), reads=[pk, dtb_k], writes=[sk[T0]])
            kb.op("vector", lambda e, sc=sc: e.scalar_tensor_tensor(out=sc[:, T1, :], in0=sc[:, T0, :], scalar=-1.0, in1=sc[:, T0, :], op0=ALU.mult, op1=ALU.max), reads=[sk[T0]], writes=[sk[T1]])
            kb.op("scalar", lambda e, sc=sc: e.activation(out=sc[:, T1, :], in_=sc[:, T1, :], func=AF.Exp, scale=-1.0), reads=[sk[T1]], writes=[sk[T1]])
            kb.op("scalar", lambda e, sc=sc: e.activation(out=sc[:, T1, :], in_=sc[:, T1, :], func=AF.Ln, bias=ONEC, scale=1.0), reads=[sk[T1], cst_k], writes=[sk[T1]])
            kb.op("vector", lambda e, sc=sc: e.scalar_tensor_tensor(out=sc[:, T0, :], in0=sc[:, T0, :], scalar=0.0, in1=sc[:, T1, :], op0=ALU.max, op1=ALU.add),
                  reads=[sk[T1]], writes=[sk[T0]])
            kb.op("vector", lambda e, sc=sc: e.scalar_tensor_tensor(out=sc[:, GG, :], in0=sc[:, T0, :], scalar=-1.0, in1=ea_t[:, :], op0=ALU.mult, op1=ALU.mult),
                  reads=[sk[T0], ea_k], writes=[sk[GG]])
            ps2, pk2 = pp.get()
            mm(ps2, pk2, 128, NH, C["tri"][:, :], sc[:, GG, :], [CK["tri"], sk[GG]])
            kb.op("scalar", lambda e, ps2=ps2, sc=sc: e.activation(out=sc[:, GC, :], in_=ps2[:, 0:NH], func=AF.Copy), reads=[pk2], writes=[sk[GC]])
            kb.op("scalar", lambda e, ps2=ps2, sc=sc: e.activation(out=sc[:, EG, :], in_=ps2[:, 0:NH], func=AF.Exp), reads=[pk2], writes=[sk[EG]])
            ps3, pk3 = pp.get()
            mm(ps3, pk3, 128, NH, C["selblk"][:, :], sc[:, GC, :], [CK["selblk"], sk[GC]])
            kb.op("vector", lambda e, ps3=ps3, sc=sc: e.tensor_tensor(out=sc[:, EDL, :], in0=ps3[:, 0:NH], in1=sc[:, GC, :], op=ALU.subtract), reads=[pk3, sk[GC]], writes=[sk[EDL]])
            kb.op("scalar", lambda e, sc=sc: e.activation(out=sc[:, EDL, :], in_=sc[:, EDL, :], func=AF.Exp), reads=[sk[EDL]], writes=[sk[EDL]])
            ps4, pk4 = pp.get()
            mm(ps4, pk4, 128, NH, C["selA"][:, :], sc[:, GC, :], [CK["selA"], sk[GC]])
            kb.op("scalar", lambda e, ps4=ps4, sc=sc: e.activation(out=sc[:, EGLA, :], in_=ps4[:, 0:NH], func=AF.Exp), reads=[pk4], writes=[sk[EGLA]])
            ps5, pk5 = pp.get()
            mm(ps5, pk5, 128, NH, C["selB"][:, :], sc[:, GC, :], [CK["selB"], sk[GC]])
            kb.op("scalar", lambda e, ps5=ps5, sc=sc: e.activation(out=sc[:, EGLB, :], in_=ps5[:, 0:NH], func=AF.Exp), reads=[pk5], writes=[sk[EGLB]])
            kb.op("vector", lambda e, sc=sc: e.tensor_tensor(out=sc[:, BEG, :], in0=sc[:, BETA, :], in1=sc[:, EG, :], op=ALU.mult), reads=[sk[BETA], sk[EG]], writes=[sk[BEG]])
        for ty in range(3):
            for h in range(NH):
                a = (ty * NH + h) % 2
                xe, xk = x_ext[ty][h], x_k[ty][h]
                cb = (ty * NH + h) * 4
                kb.op("vector", lambda e, xe=xe, a=a, cb=cb: e.tensor_scalar(out=acc_s[a][:, :], in0=xe[:, 0:TT], scalar1=cw_t[:, cb:cb + 1], scalar2=None, op0=ALU.mult),
                      reads=[xk, cw_k], writes=[acc_k[a]])
                for tap in range(1, 4):
                    kb.op("vector", lambda e, xe=xe, a=a, cb=cb, tap=tap: e.scalar_tensor_tensor(out=acc_s[a][:, :], in0=xe[:, tap:tap + TT], scalar=cw_t[:, cb + tap:cb + tap + 1],
                                                                                                in1=acc_s[a][:, :], op0=ALU.mult, op1=ALU.add),
                          reads=[xk, cw_k], writes=[acc_k[a]])
                kb.op("scalar", lambda e, a=a, ty=ty, h=h: e.activation(out=xc[ty][h][:, :], in_=acc_s[a][:, :], func=AF.Silu), reads=[acc_k[a]], writes=[xc_k[ty][h]])
                if ty < 2:
                    kb.op("scalar", lambda e, ty=ty, h=h: e.activation(out=sq_s[:, :], in_=xc[ty][h][:, :], func=AF.Square), reads=[xc_k[ty][h]], writes=[sq_k])
                    ps, pk = pp.get()
                    mm(ps, pk, 128, TT, ones_bf[:, :], sq_s[:, :], [ones_bfk, sq_k])
                    kb.op("scalar", lambda e, ps=ps: e.activation(out=rn_s[:, :], in_=ps[:, :TT], func=AF.Sqrt, bias=EPSC, scale=1.0), reads=[pk, cst_k], writes=[rn_k])
                    kb.op("vector", lambda e: e.reciprocal(out=rn_s[:, :], in_=rn_s[:, :]), reads=[rn_k], writes=[rn_k])
                    scl = float(128 ** -0.5) if ty == 0 else 1.0
                    kb.op("vector", lambda e, ty=ty, h=h, scl=scl: e.scalar_tensor_tensor(out=xc[ty][h][:, :], in0=xc[ty][h][:, :], scalar=scl, in1=rn_s[:, :], op0=ALU.mult, op1=ALU.mult),
                          reads=[rn_k], writes=[xc_k[ty][h]])
        for j in range(4):
            sc, sk = sc_s[j], sc_k[j]
            cols = slice(j * 128, (j + 1) * 128)
            ob = (i * 4 + j) % 2
            for h in range(NH):
                s = unit % NSET
                unit += 1
                t, tk = tmp[s], tmk[s]
                qn, kn, vc = xc[0][h], xc[1][h], xc[2][h]
                qk_, kk_, vk_ = xc_k[0][h], xc_k[1][h], xc_k[2][h]
                hs = slice(h, h + 1)
                ps, pk = pp.get()
                kb.op("tensor", lambda e, ps=ps, kn=kn: e.transpose(out=ps[:, 0:128], in_=kn[:, cols], identity=C["ident"][:, :]), reads=[kk_, CK["ident"]], writes=[pk])
                kb.op("scalar", lambda e, ps=ps, t=t: e.activation(out=t["kbe"][:, :], in_=ps[:, 0:128], func=AF.Copy, scale=sc[:, BEG, hs]), reads=[pk, sk[BEG]], writes=[tk["kbe"]])
                kb.op("vector", lambda e, ps=ps, t=t: e.tensor_scalar(out=t["kdec"][:, :], in0=ps[:, 0:128], scalar1=sc[:, EDL, hs], scalar2=None, op0=ALU.mult), reads=[pk, sk[EDL]], writes=[tk["kdec"]])
                ps, pk = pp.get()
                kb.op("tensor", lambda e, ps=ps, vc=vc: e.transpose(out=ps[:, 0:128], in_=vc[:, cols], identity=C["ident"][:, :]), reads=[vk_, CK["ident"]], writes=[pk])
                kb.op("scalar", lambda e, ps=ps, t=t: e.activation(out=t["vb"][:, :], in_=ps[:, 0:128], func=AF.Copy, scale=sc[:, BETA, hs]), reads=[pk, sk[BETA]], writes=[tk["vb"]])
                kb.op("gpsimd", lambda e, t=t: e.tensor_scalar(out=t["dg"][:, 0:128], in0=C["ident"][:, :], scalar1=sc[:, GC, hs], scalar2=None, op0=ALU.mult), reads=[CK["ident"], sk[GC]], writes=[tk["dg"]])
                kb.op("gpsimd", lambda e, t=t: e.tensor_scalar(out=t["dg"][:, 128:256], in0=C["ident"][:, :], scalar1=sc[:, BETA, hs], scalar2=None, op0=ALU.mult), reads=[CK["ident"], sk[BETA]], writes=[tk["dg"]])
                psr, pkr = pp.get()
                mm(psr, pkr, 128, 256, C["ones"][:, :], t["dg"][:, :], [CK["ones"], tk["dg"]])
                kb.op("scalar", lambda e, t=t, psr=psr: e.activation(out=t["egf"][:, :], in_=psr[:, 0:128], func=AF.Exp), reads=[pkr], writes=[tk["egf"]])
                kb.op("vector", lambda e, t=t, psr=psr: e.scalar_tensor_tensor(out=t["decT"][:, :], in0=psr[:, 0:128], scalar=sc[:, GC, hs], in1=C["mTfull"][:, :], op0=ALU.subtract, op1=ALU.add),
                      reads=[pkr, sk[GC], CK["mTfull"]], writes=[tk["decT"]])
                kb.op("scalar", lambda e, t=t: e.activation(out=t["decT"][:, :], in_=t["decT"][:, :], func=AF.Exp), reads=[tk["decT"]], writes=[tk["decT"]])
                kb.op("vector", lambda e, t=t, psr=psr: e.scalar_tensor_tensor(out=t["decS"][:, :], in0=psr[:, 0:128], scalar=sc[:, GC, hs], in1=C["mstrict"][:, :], op0=ALU.subtract, op1=ALU.subtract),
                      reads=[pkr, sk[GC], CK["mstrict"]], writes=[tk["decS"]])
                kb.op("scalar", lambda e, t=t: e.activation(out=t["decS"][:, :], in_=t["decS"][:, :], func=AF.Exp, scale=-1.0), reads=[tk["decS"]], writes=[tk["decS"]])
                kb.op("vector", lambda e, t=t, psr=psr: e.tensor_tensor(out=t["WT"][:, :], in0=psr[:, 128:256], in1=t["decT"][:, :], op=ALU.mult), reads=[pkr, tk["decT"]], writes=[tk["WT"]])
                psk, pkk = pp.get()
                mm(psk, pkk, 128, 128, kn[:, cols], kn[:, cols], [kk_])
                kb.op("vector", lambda e, t=t, psk=psk: e.scalar_tensor_tensor(out=t["nkk"][:, :], in0=psk[:, 0:128], scalar=-1.0, in1=C["sT01"][:, :], op0=ALU.mult, op1=ALU.mult),
                      reads=[pkk, CK["sT01"]], writes=[tk["nkk"]])
                kb.op("vector", lambda e, t=t, psk=psk: e.scalar_tensor_tensor(out=t["M"][:, :], in0=psk[:, 0:128], scalar=sc[:, BETA, hs], in1=t["decS"][:, :], op0=ALU.mult, op1=ALU.mult),
                      reads=[pkk, sk[BETA], tk["decS"]], writes=[tk["M"]])
                kb.op("gpsimd", lambda e, t=t: e.tensor_scalar(out=t["M"][:, :], in0=t["M"][:, :], scalar1=-1.0, scalar2=None, op0=ALU.mult), reads=[tk["M"]], writes=[tk["M"]])
                kb.op("gpsimd", lambda e, t=t: e.tensor_tensor(out=t["MT"][:, :], in0=t["nkk"][:, :], in1=t["WT"][:, :], op=ALU.mult), reads=[tk["nkk"], tk["WT"]], writes=[tk["MT"]])
                kb.op("gpsimd", lambda e, t=t: e.tensor_tensor(out=t["TT"][:, :], in0=t["MT"][:, :], in1=C["ident"][:, :], op=ALU.add), reads=[tk["MT"], CK["ident"]], writes=[tk["TT"]])
                psa, pka = pp.get()
                mm(psa, pka, 128, 128, kn[:, cols], qn[:, cols], [kk_, qk_])
                kb.op("vector", lambda e, t=t, psa=psa: e.tensor_tensor(out=t["attnT"][:, :], in0=psa[:, 0:128], in1=t["decT"][:, :], op=ALU.mult), reads=[pka, tk["decT"]], writes=[tk["attnT"]])
                kb.op("gpsimd", lambda e, t=t, qn=qn: e.tensor_tensor(out=t["qdecT"][:, :], in0=qn[:, cols], in1=t["egf"][:, :], op=ALU.mult), reads=[qk_, tk["egf"]], writes=[tk["qdecT"]])
                cur, curT = "M", "MT"
                nxt, nxtT = "M2", "M2T"
                for lev in range(1, 6):
                    ps1, pk1 = pp.get()
                    mm(ps1, pk1, 128, 128, t[curT][:, :], t[cur][:, :], [tk[curT], tk[cur]])
                    kb.op("scalar", lambda e, t=t, ps1=ps1, nxt=nxt: e.activation(out=t[nxt][:, :], in_=ps1[:, 0:128], func=AF.Copy), reads=[pk1], writes=[tk[nxt]])
                    if lev < 5:
                        ps2, pk2 = pp.get()
                        mm(ps2, pk2, 128, 128, t[cur][:, :], t[curT][:, :], [tk[curT], tk[cur]])
                        kb.op("vector", lambda e, t=t, ps2=ps2, nxtT=nxtT: e.tensor_copy(out=t[nxtT][:, :], in_=ps2[:, 0:128]), reads=[pk2], writes=[tk[nxtT]])
                    ps3, pk3 = pp.get()
                    mm(ps3, pk3, 128, 128, t[nxt][:, :], t["TT"][:, :], [tk[nxt], tk["TT"]])
                    kb.op("vector", lambda e, t=t, ps3=ps3: e.tensor_tensor(out=t["TT"][:, :], in0=t["TT"][:, :], in1=ps3[:, 0:128], op=ALU.add), reads=[pk3], writes=[tk["TT"]])
                    cur, curT, nxt, nxtT = nxt, nxtT, cur, curT
                psv, pkv = pp.get()
                mm(psv, pkv, 128, 128, t["TT"][:, :], t["vb"][:, :], [tk["TT"], tk["vb"]])
                kb.op("scalar", lambda e, t=t, psv=psv: e.activation(out=t["val"][:, :], in_=psv[:, 0:128], func=AF.Copy), reads=[pkv], writes=[tk["val"]])
                psc, pkc = pp.get()
                mm(psc, pkc, 128, 128, t["kbe"][:, :], t["TT"][:, :], [tk["TT"], tk["kbe"]])
                kb.op("scalar", lambda e, t=t, psc=psc: e.activation(out=t["kcdT"][:, :], in_=psc[:, 0:128], func=AF.Copy), reads=[pkc], writes=[tk["kcdT"]])
                pso, pko = pp.get()
                for c in range(2):
                    r0 = c * 64
                    rows = slice(r0, r0 + 64)
                    psn, pkn = pp.get()
                    mm(psn, pkn, 64, 128, t["kcdT"][:, rows], S_s[h][:, :], [tk["kcdT"], S_k[h]], p0=r0)
                    kb.op("vector", lambda e, t=t, psn=psn, rows=rows: e.tensor_tensor(out=t["vn"][rows, :], in0=t["val"][rows, :], in1=psn[rows, 0:128], op=ALU.subtract),
                          reads=[pkn, tk["val"]], writes=[tk["vn"]])
                    mm(pso, pko, 64, 128, t["qdecT"][:, rows], S_s[h][:, :], [tk["qdecT"], S_k[h]], True, False, p0=r0)
                    mm(pso, pko, 64, 128, t["attnT"][rows, rows], t["vn"][rows, :], [tk["attnT"], tk["vn"]], False, True, p0=r0)
                    pss, pks = pp.get()
                    mm(pss, pks, 128, 128, t["kdec"][rows, :], t["vn"][rows, :], [tk["kdec"], tk["vn"]])
                    egl = EGLA if c == 0 else EGLB
                    kb.op("vector", lambda e, pss=pss, egl=egl: e.scalar_tensor_tensor(out=S_s[h][:, :], in0=S_s[h][:, :], scalar=sc[:, egl, hs], in1=pss[:, 0:128], op0=ALU.mult, op1=ALU.add),
                          reads=[pks, sk[egl]], writes=[S_k[h]])
                kb.op("scalar", lambda e, pso=pso, h=h: e.activation(out=junk[:, :], in_=pso[:, 0:128], func=AF.Square, accum_out=ss_s[:, h:h + 1]), reads=[pko], writes=[junk_k, ss_k])
                kb.op("scalar", lambda e, h=h: e.activation(out=ss_s[:, h:h + 1], in_=ss_s[:, h:h + 1], func=AF.Sqrt, bias=EPSC, scale=1.0 / 128), reads=[ss_k, cst_k], writes=[ss_k])
                kb.op("vector", lambda e, h=h: e.reciprocal(out=ss_s[:, h:h + 1], in_=ss_s[:, h:h + 1]), reads=[ss_k], writes=[ss_k])
                kb.op("vector", lambda e, pso=pso, h=h: e.scalar_tensor_tensor(out=o_s[ob][:, h * 128:(h + 1) * 128], in0=pso[:, 0:128], scalar=ss_s[:, h:h + 1],
                                                                               in1=gz_s[j][:, h * 128:(h + 1) * 128], op0=ALU.mult, op1=ALU.mult),
                      reads=[pko, ss_k, gz_k[j]], writes=[o_k[ob]])
            kb.dma("sync", o_out[:, i * 4 + j, :], o_s[ob][:, :], reads=[o_k[ob]], is_output=True)
    return kb.finish(), kb


def build_norm0(T):
    kb = KB()
    TT = 512
    NT = T // TT
    hT = kb.dram("hT", [D, T], F32, "ExternalInput")
    g_in = kb.dram("g", [D], F32, "ExternalInput")
    hn_out = kb.dram("hn_out", [D, T], BF16, "ExternalOutput")
    g_t, g_k = load_vec_col(kb, "g_s", g_in, D)
    ones = kb.sbuf("ones", [128, 128], BF16)
    ones_trk = Trk()
    kb.op("vector", lambda h: h.memset(ones[:], 1.0), writes=[ones_trk])
    eps_t = kb.sbuf("eps", [128, 1], F32)
    eps_trk = Trk()
    kb.op("vector", lambda h: h.memset(eps_t[:], EPS), writes=[eps_trk])
    pp = PsumPool(kb)
    h_s = [kb.sbuf("h%d" % b, [128, 8, TT], F32) for b in range(2)]
    h_k = [[Trk() for _ in range(8)] for _ in range(2)]
    hn_s = [kb.sbuf("hn%d" % b, [128, 8, TT], BF16) for b in range(2)]
    hn_k = [[Trk() for _ in range(8)] for _ in range(2)]
    sq_s = kb.sbuf("sq", [128, 8, TT], BF16)
    sq_k = [Trk() for _ in range(8)]
    rstd_s = kb.sbuf("rstd", [128, TT], F32)
    rstd_k = Trk()
    scr = {"sq": [(sq_s[:, c, :], sq_k[c]) for c in range(8)], "rstd": (rstd_s[:, :], rstd_k), "eps": (eps_t[:, 0:1], eps_trk)}
    hT_v = hT.rearrange("(c p) t -> p c t", p=128)
    hno_v = hn_out.rearrange("(c p) t -> p c t", p=128)
    for i in range(NT):
        b = i % 2
        t0 = i * TT
        kb.dma("sync", h_s[b][:, :, :], hT_v[:, :, t0:t0 + TT], writes=h_k[b])
        rmsnorm_fm(kb, pp, [h_s[b][:, c, :] for c in range(8)], h_k[b], g_t, g_k, ones[:, :], ones_trk,
                   [hn_s[b][:, c, :] for c in range(8)], hn_k[b], scr, 8, TT, D)
        kb.dma("sync", hno_v[:, :, t0:t0 + TT], hn_s[b][:, :, :], reads=hn_k[b], is_output=True)
    return kb.finish(), kb


BF = ml_dtypes.bfloat16
B_, L_, T_ = 4, 8192, 4096
NQB_ = 32
_PROGS = {}


def _prog(name):
    if name not in _PROGS:
        if name == "n0":
            _PROGS[name] = build_norm0(T_)[0]
        elif name == "e1":
            _PROGS[name] = build_e1(T_)[0]
        elif name == "e2":
            _PROGS[name] = build_e2(NQB_)[0]
        elif name == "o1":
            _PROGS[name] = build_o1(L_, 4)[0]
        elif name == "p":
            _PROGS[name] = build_post(T_, False)[0]
        elif name == "pf":
            _PROGS[name] = build_post(T_, True)[0]
    return _PROGS[name]


def _run(name, in_maps):
    res = run_bass_kernel_spmd(_prog(name), in_maps, core_ids=list(range(NCORES)))
    return res.results


def _rot_cols(w):
    n = w.shape[1] // 64
    w4 = w.reshape(w.shape[0], n, 2, 32)
    return np.ascontiguousarray(w4[:, :, ::-1, :]).reshape(w.shape[0], n * 64)


def _rope_tabs(pos):
    inv = (np.float32(10000.0) ** (-np.arange(0, 64, 2, dtype=np.float32) / np.float32(64))).astype(np.float32)
    ang = (pos.astype(np.float32)[:, None] * inv[None, :]).astype(np.float32)
    c = np.cos(ang).astype(np.float32).T
    s = np.sin(ang).astype(np.float32).T
    return np.ascontiguousarray(np.concatenate([c, c, c, c], 0)), np.ascontiguousarray(np.concatenate([-s, s, -s, s], 0))


def _c(a):
    return np.ascontiguousarray(a)


def kernel(x, mix_norm, mlp_norm, w_ff1, w_ff2, ev_w_in, ev_kv_norm, ev_w_uk, ev_w_uv, ev_pool_w, ev_pool_scale, ev_w_out,
           od_w_in, od_conv_w, od_a_log, od_dt_bias, od_o_norm, od_w_out, final_norm):
    f32 = np.float32
    x = np.asarray(x, f32)
    cores = [(c // 2, c % 2) for c in range(NCORES)]
    hT = [_c(x[b].T) for b in range(B_)]
    res = _run("n0", [{"hT": _c(hT[b][:, hf * T_:(hf + 1) * T_]), "g": _c(np.asarray(mix_norm[0], f32))} for (b, hf) in cores])
    hn = [np.concatenate([res[2 * b]["hn_out"], res[2 * b + 1]["hn_out"]], 1) for b in range(B_)]
    gconst = gdn_consts()
    iota = np.tile(np.arange(256, dtype=f32), (128, 1))
    ident_bf = np.eye(128, dtype=f32).astype(BF)
    out = None
    for layer in range(4):
        j = layer // 2
        if layer % 2 == 0:
            w_in = np.asarray(ev_w_in[j], f32)
            w_rot = _c(np.concatenate([_rot_cols(w_in[:, 0:512]), _rot_cols(w_in[:, 640:1152]), _rot_cols(w_in[:, 1152:1216])], 1))
            w_uk = np.asarray(ev_w_uk[j], f32)
            ims = []
            for (b, hf) in cores:
                pos = hf * T_ + np.arange(T_)
                cosT, sinT = _rope_tabs(pos)
                invc0 = np.zeros((128, 4, 512), f32)
                for g in range(4):
                    invc0[:, g, :] = (1.0 / np.minimum(pos[:512] + 1, 2 ** (g + 1))).astype(f32)
                halo = np.zeros((D, HALO), BF) if hf == 0 else hn[b][:, T_ - HALO:T_]
                ims.append({"hnT": _c(np.concatenate([halo, hn[b][:, hf * T_:(hf + 1) * T_]], 1)), "w_in": _c(w_in), "w_rot": w_rot,
                            "kvn": _c(np.asarray(ev_kv_norm[j], f32)), "w_uk": _c(w_uk), "w_ukr": _rot_cols(w_uk), "w_uv": _c(np.asarray(ev_w_uv[j], f32)),
                            "pool_w": _c(np.asarray(ev_pool_w[j], f32)), "pool_sc": _c(np.asarray(ev_pool_scale[j], f32)),
                            "cosT": cosT, "sinT": sinT, "invc0": invc0})
            r1 = _run("e1", ims)
            cat = lambda k, ax: [np.concatenate([r1[2 * b][k], r1[2 * b + 1][k]], ax) for b in range(B_)]
            qT, qiT, kiT, kT, vv, wiT, ybT = cat("qT", 1), cat("qiT", 1), cat("kiT", 1), cat("kT", 1), cat("v", 0), cat("wiT", 1), cat("ybT", 1)
            ims = []
            qposs = []
            for (b, par) in cores:
                blocks = [2 * i + ((i % 2) ^ par) for i in range(NQB_)]
                qpos = np.concatenate([np.arange(g * 128, (g + 1) * 128) for g in blocks])
                qposs.append(qpos)
                ims.append({"qh": _c(qT[b].reshape(8, 64, L_)[:, :, qpos].transpose(1, 0, 2)),
                            "qih": _c(qiT[b].reshape(8, 64, L_)[:, :, qpos].transpose(1, 0, 2)),
                            "wi_tok": _c(wiT[b][:, qpos].T.reshape(NQB_, 128, 8).transpose(1, 0, 2)),
                            "qrel": _c((qpos.reshape(NQB_, 128) - 256 * np.arange(NQB_)[:, None]).T.astype(f32)),
                            "kiT": _c(kiT[b]), "kT": _c(kT[b]), "v": _c(vv[b].reshape(L_ // 128, 128, 64).transpose(1, 0, 2)),
                            "iota": iota, "ident": ident_bf})
            r2 = _run("e2", ims)
            yT = []
            for b in range(B_):
                ya = np.zeros((L_, 512), BF)
                for par in range(2):
                    ya[qposs[2 * b + par]] = r2[2 * b + par]["ya"].transpose(1, 0, 2).reshape(NQB_ * 128, 512)
                yT.append(np.concatenate([_c(ya.T), ybT[b]], 0))
            w_out = np.asarray(ev_w_out[j], f32)
        else:
            w_in = np.asarray(od_w_in[j], f32)
            conv_w = np.asarray(od_conv_w[j], f32)
            ims = []
            for (b, hg) in cores:
                heads = list(range(hg * 4, hg * 4 + 4))
                cols = lambda base: np.concatenate([np.arange(base + h * 128, base + (h + 1) * 128) for h in heads])
                cw = np.zeros((128, 3, 4, 4), f32)
                for ty in range(3):
                    for hi, h in enumerate(heads):
                        cw[:, ty, hi, :] = conv_w[:, ty * 1024 + h * 128: ty * 1024 + (h + 1) * 128].T
                im = {"hnT": _c(hn[b]), "w_qkv": _c(np.concatenate([w_in[:, cols(0)], w_in[:, cols(1024)], w_in[:, cols(2048)]], 1)),
                      "w_z": _c(w_in[:, cols(3072)]),
                      "w_ba": _c(np.concatenate([w_in[:, [4096 + h for h in heads]], w_in[:, [4104 + h for h in heads]]], 1)),
                      "cw": _c(cw.reshape(128, -1)), "alog_b": _c(np.tile(np.asarray(od_a_log[j], f32)[heads][None, :], (128, 1))),
                      "dtb_b": _c(np.tile(np.asarray(od_dt_bias[j], f32)[heads][None, :], (128, 1))),
                      "onorm_b": _c(np.tile(np.asarray(od_o_norm[j], f32)[None, :], (128, 1)))}
                for n, a in gconst.items():
                    im["c_" + n] = a
                ims.append(im)
            r1 = _run("o1", ims)
            yT = []
            for b in range(B_):
                o = np.concatenate([r1[2 * b + hg]["o_tok"].transpose(1, 0, 2).reshape(L_, 512) for hg in range(2)], 1)
                yT.append(_c(o.T))
            w_out = np.asarray(od_w_out[j], f32)
        final = layer == 3
        g_next = np.asarray(final_norm if final else mix_norm[layer + 1], f32)
        ims = [{"hT": _c(hT[b][:, hf * T_:(hf + 1) * T_]), "yT": _c(yT[b][:, hf * T_:(hf + 1) * T_]), "w_out": _c(w_out),
                "w1": _c(np.asarray(w_ff1[layer], f32)), "w2": _c(np.asarray(w_ff2[layer], f32)),
                "g_mlp": _c(np.asarray(mlp_norm[layer], f32)), "g_next": _c(g_next)} for (b, hf) in cores]
        rp = _run("pf" if final else "p", ims)
        hT = [np.concatenate([rp[2 * b]["hT_out"], rp[2 * b + 1]["hT_out"]], 1) for b in range(B_)]
        hn = [np.concatenate([rp[2 * b]["hn_out"], rp[2 * b + 1]["hn_out"]], 1) for b in range(B_)]
    out = np.stack([_c(hn[b].T) for b in range(B_)], 0).astype(np.float32)
    return out
```

```python
import numpy as np
import ml_dtypes
from contextlib import ExitStack
import concourse.bass as bass
import concourse.mybir as mybir
from concourse.bass_utils import run_bass_kernel_spmd

F32 = mybir.dt.float32
BF16 = mybir.dt.bfloat16
I32 = mybir.dt.int32
AF = mybir.ActivationFunctionType
ALU = mybir.AluOpType
AX = mybir.AxisListType

NCORES = 8
D = 1024
DFF = 4096
EPS = 1e-6


class Trk:
    __slots__ = ("w", "r")

    def __init__(self):
        self.w = None
        self.r = {}


class SemObj:
    __slots__ = ("h", "val")

    def __init__(self, h):
        self.h = h
        self.val = 0


class Eng:
    def __init__(self, kb, name, h):
        self.kb = kb
        self.name = name
        self.h = h
        self.sem = SemObj(kb.es.enter_context(kb.nc.semaphore("s_" + name)))
        self.seen = {}


class KB:
    NDMASEM = 24

    def __init__(self):
        self.nc = bass.Bass("TRN2", target_bir_lowering=False)
        self.es = ExitStack()
        self.E = {n: Eng(self, n, getattr(self.nc, n)) for n in ("tensor", "vector", "scalar", "gpsimd", "sync")}
        self.dsems = [SemObj(self.es.enter_context(self.nc.semaphore("s_dma%d" % i))) for i in range(self.NDMASEM)]
        self.dma_i = 0
        self.out_toks = []
        self.ninstr = 0

    def sbuf(self, name, shape, dt):
        return self.es.enter_context(self.nc.sbuf_tensor(name, list(shape), dt))

    def psum(self, name, shape, dt):
        return self.es.enter_context(self.nc.psum_tensor(name, list(shape), dt))

    def dram(self, name, shape, dt, kind):
        return self.nc.dram_tensor(name, list(shape), dt, kind=kind).ap()

    def _waits(self, e, reads, writes, acc=False):
        need = {}

        def req(tok):
            if tok is None:
                return
            s, v = tok
            if need.get(s, (None, 0))[1] < v:
                need[s] = (s, v)

        for t in reads:
            req(t.w)
        for t in writes:
            if not (acc and t.w is not None and t.w[0] is e.sem):
                req(t.w)
            for r in t.r.items():
                req(r)
        for s, v in need.values():
            if e.seen.get(s, 0) < v:
                e.h.wait_ge(s.h, v)
                e.seen[s] = v
                self.ninstr += 1

    def _post(self, tok, reads, writes):
        for t in reads:
            if t.r.get(tok[0], 0) < tok[1]:
                t.r[tok[0]] = tok[1]
        for t in writes:
            t.w = tok
            t.r = {}

    def op(self, eng, fn, reads=(), writes=(), acc=False):
        e = self.E[eng]
        self._waits(e, reads, writes, acc or eng == "tensor")
        ins = fn(e.h)
        e.sem.val += 1
        ins.then_inc(e.sem.h, 1)
        self.ninstr += 1
        tok = (e.sem, e.sem.val)
        self._post(tok, reads, writes)
        return tok

    def dma(self, eng, out, in_, reads=(), writes=(), is_output=False, **kw):
        e = self.E[eng]
        s = self.dsems[self.dma_i % self.NDMASEM]
        self.dma_i += 1
        if s.val > 0 and e.seen.get(s, 0) < s.val:
            e.h.wait_ge(s.h, s.val)
            e.seen[s] = s.val
        self._waits(e, reads, writes)
        ins = e.h.dma_start(out=out, in_=in_, **kw)
        s.val += 16
        ins.then_inc(s.h, 16)
        self.ninstr += 1
        tok = (s, s.val)
        self._post(tok, reads, writes)
        if is_output:
            self.out_toks.append(tok)
        return tok

    def finish(self):
        e = self.E["sync"]
        for s, v in self.out_toks:
            if e.seen.get(s, 0) < v:
                e.h.wait_ge(s.h, v)
                e.seen[s] = v
        for n, en in self.E.items():
            if en.sem.val > 0 and n != "sync":
                e.h.wait_ge(en.sem.h, en.sem.val)
        self.es.close()
        return self.nc


def load_weight_bf16(kb, name, w_ap, K, N, eng="gpsimd"):
    kc = K // 128
    t = kb.sbuf(name, [128, kc, N], BF16)
    trk = Trk()
    src = w_ap.rearrange("(kc p) n -> p kc n", p=128)
    step = 2048
    for c in range(kc):
        for n0 in range(0, N, step):
            n1 = min(N, n0 + step)
            kb.dma(eng, t[:, c, n0:n1], src[:, c, n0:n1], writes=[trk])
    return t, trk


def load_vec_col(kb, name, v_ap, n):
    c = n // 128
    t = kb.sbuf(name, [128, c], F32)
    trk = Trk()
    with kb.nc.allow_non_contiguous_dma(reason="tiny per-feature vector"):
        kb.dma("sync", t[:, :], v_ap.rearrange("(c p) -> p c", p=128), writes=[trk])
    return t, trk


class PsumPool:
    def __init__(self, kb, n=8):
        self.kb = kb
        self.t = [kb.psum("ps%d" % i, [128, 512], F32) for i in range(n)]
        self.trk = [Trk() for _ in range(n)]
        self.i = 0
        self.n = n

    def get(self):
        i = self.i % self.n
        self.i += 1
        return self.t[i], self.trk[i]


def rmsnorm_fm(kb, pp, x_tiles, x_trks, g_t, g_trk, ones_t, ones_trk, out_tiles, out_trks, scr, nd, TT, Dn):
    ps, ps_trk = pp.get()
    for c in range(nd):
        sq, sq_trk = scr["sq"][c]
        kb.op("scalar", lambda h, c=c, sq=sq: h.activation(out=sq, in_=x_tiles[c], func=AF.Square),
              reads=[x_trks[c]], writes=[sq_trk])
    for c in range(nd):
        sq, sq_trk = scr["sq"][c]
        kb.op("tensor", lambda h, c=c, sq=sq: h.matmul(ps[:, :TT], lhsT=ones_t, rhs=sq, start=(c == 0), stop=(c == nd - 1)),
              reads=[sq_trk, ones_trk], writes=[ps_trk])
    rstd, rstd_trk = scr["rstd"]
    kb.op("scalar", lambda h: h.activation(out=rstd, in_=ps[:, :TT], func=AF.Sqrt, bias=scr["eps"][0], scale=1.0 / Dn),
          reads=[ps_trk, scr["eps"][1]], writes=[rstd_trk])
    kb.op("vector", lambda h: h.reciprocal(out=rstd, in_=rstd), reads=[rstd_trk], writes=[rstd_trk])
    for c in range(nd):
        kb.op("vector", lambda h, c=c: h.scalar_tensor_tensor(out=out_tiles[c], in0=x_tiles[c], scalar=g_t[:, c:c + 1], in1=rstd,
                                                               op0=ALU.mult, op1=ALU.mult),
              reads=[x_trks[c], rstd_trk, g_trk], writes=[out_trks[c]])


def build_post(T, final):
    kb = KB()
    nc = kb.nc
    TT = 256
    NT = T // TT
    hT = kb.dram("hT", [D, T], F32, "ExternalInput")
    yT = kb.dram("yT", [D, T], BF16, "ExternalInput")
    w_out = kb.dram("w_out", [D, D], F32, "ExternalInput")
    w1 = kb.dram("w1", [D, DFF], F32, "ExternalInput")
    w2 = kb.dram("w2", [DFF, D], F32, "ExternalInput")
    g_mlp = kb.dram("g_mlp", [D], F32, "ExternalInput")
    g_next = kb.dram("g_next", [D], F32, "ExternalInput")
    hT_out = kb.dram("hT_out", [D, T], F32, "ExternalOutput")
    if final:
        hn_out = kb.dram("hn_out", [D, T], F32, "ExternalOutput")
    else:
        hn_out = kb.dram("hn_out", [D, T], BF16, "ExternalOutput")

    wo_t, wo_trk = load_weight_bf16(kb, "wo", w_out, D, D)
    gm_t, gm_trk = load_vec_col(kb, "gm", g_mlp, D)
    gn_t, gn_trk = load_vec_col(kb, "gn", g_next, D)
    w1_t, w1_trk = load_weight_bf16(kb, "w1s", w1, D, DFF)
    w2_t, w2_trk = load_weight_bf16(kb, "w2s", w2, DFF, D)

    ones = kb.sbuf("ones", [128, 128], BF16)
    ones_trk = Trk()
    kb.op("vector", lambda h: h.memset(ones[:], 1.0), writes=[ones_trk])
    eps_t = kb.sbuf("eps", [128, 1], F32)
    eps_trk = Trk()
    kb.op("vector", lambda h: h.memset(eps_t[:], EPS), writes=[eps_trk])

    pp = PsumPool(kb)
    NB = 2
    y_s = [kb.sbuf("y%d" % b, [128, 8, TT], BF16) for b in range(NB)]
    y_k = [Trk() for _ in range(NB)]
    h_s = [kb.sbuf("h%d" % b, [128, 8, TT], F32) for b in range(NB)]
    h_k = [[Trk() for _ in range(8)] for _ in range(NB)]
    hload_k = [Trk() for _ in range(NB)]
    xn_s = kb.sbuf("xn", [128, 8, TT], BF16)
    xn_k = [Trk() for _ in range(8)]
    sq_s = kb.sbuf("sq", [128, 8, TT], BF16)
    sq_k = [Trk() for _ in range(8)]
    rstd_s = kb.sbuf("rstd", [128, TT], F32)
    rstd_k = Trk()
    a_s = kb.sbuf("a", [128, 32, TT], BF16)
    a_k = [Trk() for _ in range(32)]
    r_s = [kb.sbuf("r%d" % b, [128, TT], F32) for b in range(4)]
    r_k = [Trk() for _ in range(4)]
    if final:
        hn_s = kb.sbuf("hn", [128, 8, TT], F32)
    else:
        hn_s = kb.sbuf("hn", [128, 8, TT], BF16)
    hn_k = [Trk() for _ in range(8)]

    hT_v = hT.rearrange("(c p) t -> p c t", p=128)
    yT_v = yT.rearrange("(c p) t -> p c t", p=128)
    hTo_v = hT_out.rearrange("(c p) t -> p c t", p=128)
    hno_v = hn_out.rearrange("(c p) t -> p c t", p=128)

    scr = {"sq": [(sq_s[:, c, :], sq_k[c]) for c in range(8)], "rstd": (rstd_s[:, :], rstd_k), "eps": (eps_t[:, 0:1], eps_trk)}

    def load(i):
        b = i % NB
        t0 = i * TT
        kb.dma("sync", y_s[b][:, :, :], yT_v[:, :, t0:t0 + TT], writes=[y_k[b]])
        kb.dma("sync", h_s[b][:, :, :], hT_v[:, :, t0:t0 + TT], writes=h_k[b])

    load(0)
    for i in range(NT):
        b = i % NB
        t0 = i * TT
        if i + 1 < NT:
            load(i + 1)
        for oc in range(8):
            ps, pk = pp.get()
            for kc in range(8):
                kb.op("tensor", lambda h, kc=kc, oc=oc, ps=ps: h.matmul(ps[:, :TT], lhsT=wo_t[:, kc, oc * 128:(oc + 1) * 128], rhs=y_s[b][:, kc, :],
                                                                        start=(kc == 0), stop=(kc == 7)),
                      reads=[wo_trk, y_k[b]], writes=[pk])
            kb.op("vector", lambda h, oc=oc, ps=ps: h.tensor_tensor(out=h_s[b][:, oc, :], in0=h_s[b][:, oc, :], in1=ps[:, :TT], op=ALU.add),
                  reads=[pk], writes=[h_k[b][oc]])
        rmsnorm_fm(kb, pp, [h_s[b][:, c, :] for c in range(8)], h_k[b], gm_t, gm_trk, ones[:, :], ones_trk,
                   [xn_s[:, c, :] for c in range(8)], xn_k, scr, 8, TT, D)
        for fc in range(32):
            ps, pk = pp.get()
            for kc in range(8):
                kb.op("tensor", lambda h, kc=kc, fc=fc, ps=ps: h.matmul(ps[:, :TT], lhsT=w1_t[:, kc, fc * 128:(fc + 1) * 128], rhs=xn_s[:, kc, :],
                                                                        start=(kc == 0), stop=(kc == 7)),
                      reads=[w1_trk, xn_k[kc]], writes=[pk])
            rb = fc % 4
            kb.op("scalar", lambda h, ps=ps, rb=rb: h.activation(out=r_s[rb][:, :], in_=ps[:, :TT], func=AF.Relu),
                  reads=[pk], writes=[r_k[rb]])
            kb.op("gpsimd", lambda h, fc=fc, rb=rb: h.tensor_tensor(out=a_s[:, fc, :], in0=r_s[rb][:, :], in1=r_s[rb][:, :], op=ALU.mult),
                  reads=[r_k[rb]], writes=[a_k[fc]])
        for oc in range(8):
            ps, pk = pp.get()
            for fc in range(32):
                kb.op("tensor", lambda h, fc=fc, oc=oc, ps=ps: h.matmul(ps[:, :TT], lhsT=w2_t[:, fc, oc * 128:(oc + 1) * 128], rhs=a_s[:, fc, :],
                                                                        start=(fc == 0), stop=(fc == 31)),
                      reads=[w2_trk, a_k[fc]], writes=[pk])
            kb.op("vector", lambda h, oc=oc, ps=ps: h.tensor_tensor(out=h_s[b][:, oc, :], in0=h_s[b][:, oc, :], in1=ps[:, :TT], op=ALU.add),
                  reads=[pk], writes=[h_k[b][oc]])
        kb.dma("sync", hTo_v[:, :, t0:t0 + TT], h_s[b][:, :, :], reads=h_k[b], is_output=True)
        rmsnorm_fm(kb, pp, [h_s[b][:, c, :] for c in range(8)], h_k[b], gn_t, gn_trk, ones[:, :], ones_trk,
                   [hn_s[:, c, :] for c in range(8)], hn_k, scr, 8, TT, D)
        kb.dma("sync", hno_v[:, :, t0:t0 + TT], hn_s[:, :, :], reads=hn_k, is_output=True)
    return kb.finish(), kb


EV_IN = 1736
C_Q, C_KV, C_QI, C_KI, C_WI, C_U = 0, 512, 640, 1152, 1216, 1224
HALO = 16


def build_e1(T):
    kb = KB()
    nc = kb.nc
    TT = 512
    NT = T // TT
    W = TT + HALO
    hnT = kb.dram("hnT", [D, HALO + T], BF16, "ExternalInput")
    w_in = kb.dram("w_in", [D, EV_IN], F32, "ExternalInput")
    w_rot = kb.dram("w_rot", [D, 1088], F32, "ExternalInput")
    kvn = kb.dram("kvn", [128], F32, "ExternalInput")
    w_uk = kb.dram("w_uk", [128, 64], F32, "ExternalInput")
    w_ukr = kb.dram("w_ukr", [128, 64], F32, "ExternalInput")
    w_uv = kb.dram("w_uv", [128, 64], F32, "ExternalInput")
    pool_w = kb.dram("pool_w", [4, 128, 128], F32, "ExternalInput")
    pool_sc = kb.dram("pool_sc", [512], F32, "ExternalInput")
    cosT = kb.dram("cosT", [128, T], F32, "ExternalInput")
    sinT = kb.dram("sinT", [128, T], F32, "ExternalInput")
    invc0 = kb.dram("invc0", [128, 4, TT], F32, "ExternalInput")
    qT_o = kb.dram("qT", [512, T], BF16, "ExternalOutput")
    qiT_o = kb.dram("qiT", [512, T], BF16, "ExternalOutput")
    kiT_o = kb.dram("kiT", [64, T], BF16, "ExternalOutput")
    kT_o = kb.dram("kT", [64, T], BF16, "ExternalOutput")
    v_o = kb.dram("v", [T, 64], BF16, "ExternalOutput")
    wiT_o = kb.dram("wiT", [8, T], F32, "ExternalOutput")
    ybT_o = kb.dram("ybT", [512, T], BF16, "ExternalOutput")

    win_t, win_k = load_weight_bf16(kb, "win", w_in, D, EV_IN)
    wrot_t, wrot_k = load_weight_bf16(kb, "wrot", w_rot, D, 1088)
    wuk_t, wuk_k = load_weight_bf16(kb, "wuk", w_uk, 128, 64)
    wukr_t, wukr_k = load_weight_bf16(kb, "wukr", w_ukr, 128, 64)
    wuv_t, wuv_k = load_weight_bf16(kb, "wuv", w_uv, 128, 64)
    pw_t = kb.sbuf("pw", [128, 4, 128], BF16)
    pw_k = Trk()
    kb.dma("gpsimd", pw_t[:, :, :], pool_w.rearrange("g c d -> c g d"), writes=[pw_k])
    psc_t, psc_k = load_vec_col(kb, "psc", pool_sc, 512)
    kvn_t, kvn_k = load_vec_col(kb, "kvn_s", kvn, 128)
    invc0_t = kb.sbuf("invc0_s", [128, 4, TT], F32)
    invc0_k = Trk()
    kb.dma("sync", invc0_t[:, :, :], invc0[:, :, :], writes=[invc0_k])
    invc_t = kb.sbuf("invc_s", [128, 4, TT], F32)
    invc_k = Trk()
    for g in range(4):
        kb.op("vector", lambda h, g=g: h.memset(invc_t[:, g, :], 1.0 / (2 ** (g + 1))), writes=[invc_k])
    ones = kb.sbuf("ones", [128, 128], BF16)
    ones_trk = Trk()
    kb.op("vector", lambda h: h.memset(ones[:], 1.0), writes=[ones_trk])
    eps_t = kb.sbuf("eps", [128, 1], F32)
    eps_trk = Trk()
    kb.op("vector", lambda h: h.memset(eps_t[:], EPS), writes=[eps_trk])

    pp = PsumPool(kb)
    NB = 2
    hn_s = [kb.sbuf("hn%d" % b, [128, 8, TT], BF16) for b in range(NB)]
    hn_k = [Trk() for _ in range(NB)]
    halo_s = kb.sbuf("halo", [128, 8, HALO], BF16)
    halo_k = Trk()
    cs_s = [kb.sbuf("cs%d" % b, [128, 2, TT], F32) for b in range(NB)]
    cs_k = [Trk() for _ in range(NB)]
    t1_s = [kb.sbuf("t1_%d" % b, [128, TT], F32) for b in range(2)]
    t1_k = [Trk() for _ in range(2)]
    t2_s = [kb.sbuf("t2_%d" % b, [128, TT], F32) for b in range(2)]
    t2_k = [Trk() for _ in range(2)]
    q_s = kb.sbuf("q_s", [128, 4, TT], BF16)
    q_k = Trk()
    qi_s = kb.sbuf("qi_s", [128, 4, TT], BF16)
    qi_k = Trk()
    ki_s = kb.sbuf("ki_s", [64, TT], BF16)
    ki_k = Trk()
    k_s = kb.sbuf("k_s", [64, TT], BF16)
    k_k = Trk()
    v_s = kb.sbuf("v_s", [128, 4, 64], BF16)
    v_k = Trk()
    wi_s = kb.sbuf("wi_s", [8, TT], F32)
    wi_k = Trk()
    ckv_s = kb.sbuf("ckv_s", [128, TT], F32)
    ckv_k = Trk()
    ckvn_s = kb.sbuf("ckvn_s", [128, TT], BF16)
    ckvn_k = Trk()
    sq_s = kb.sbuf("sq", [128, TT], BF16)
    sq_k = Trk()
    rstd_s = kb.sbuf("rstd", [128, TT], F32)
    rstd_k = Trk()
    u_s = [kb.sbuf("u%d" % g, [128, W], F32) for g in range(4)]
    u_k = [Trk() for _ in range(4)]
    sA = kb.sbuf("sA", [128, W], F32)
    sA_k = Trk()
    sB = kb.sbuf("sB", [128, W], F32)
    sB_k = Trk()
    pl_s = [kb.sbuf("pl%d" % g, [128, TT], BF16) for g in range(4)]
    pl_k = [Trk() for _ in range(4)]
    yb_s = kb.sbuf("yb_s", [128, 4, TT], BF16)
    yb_k = Trk()
    scr = {"sq": [(sq_s[:, :], sq_k)], "rstd": (rstd_s[:, :], rstd_k), "eps": (eps_t[:, 0:1], eps_trk)}

    hn_v = hnT.rearrange("(c p) t -> p c t", p=128)
    rope_i = [0]

    def load(i):
        b = i % NB
        t0 = i * TT
        kb.dma("sync", hn_s[b][:, :, :], hn_v[:, :, HALO + t0:HALO + t0 + TT], writes=[hn_k[b]])
        kb.dma("sync", cs_s[b][:, 0, :], cosT[:, t0:t0 + TT], writes=[cs_k[b]])
        kb.dma("sync", cs_s[b][:, 1, :], sinT[:, t0:t0 + TT], writes=[cs_k[b]])

    def proj(ps, pk, wt, wk, c0, c1, rhs_fn, rk, N):
        M = c1 - c0
        for kc in range(8):
            kb.op("tensor", lambda h, kc=kc: h.matmul(ps[:M, :N], lhsT=wt[:, kc, c0:c1], rhs=rhs_fn(kc), start=(kc == 0), stop=(kc == 7)),
                  reads=[wk, rk], writes=[pk])

    def rope(b, psa, pka, psb, pkb, M, out_ap, out_k):
        j = rope_i[0] % 2
        rope_i[0] += 1
        kb.op("vector", lambda h: h.tensor_tensor(out=t1_s[j][:M, :], in0=psa[:M, :TT], in1=cs_s[b][:M, 0, :], op=ALU.mult),
              reads=[pka, cs_k[b]], writes=[t1_k[j]])
        kb.op("vector", lambda h: h.tensor_tensor(out=t2_s[j][:M, :], in0=psb[:M, :TT], in1=cs_s[b][:M, 1, :], op=ALU.mult),
              reads=[pkb, cs_k[b]], writes=[t2_k[j]])
        kb.op("gpsimd", lambda h: h.tensor_tensor(out=out_ap, in0=t1_s[j][:M, :], in1=t2_s[j][:M, :], op=ALU.add),
              reads=[t1_k[j], t2_k[j]], writes=[out_k])

    kb.dma("sync", halo_s[:, :, :], hn_v[:, :, 0:HALO], writes=[halo_k])
    load(0)
    for i in range(NT):
        b = i % NB
        t0 = i * TT
        if i + 1 < NT:
            load(i + 1)
        rhs_fn = lambda kc, b=b: hn_s[b][:, kc, :]
        for (cbase, rbase, dst, dk) in ((C_Q, 0, q_s, q_k), (C_QI, 512, qi_s, qi_k)):
            for c in range(4):
                psa, pka = pp.get()
                proj(psa, pka, win_t, win_k, cbase + c * 128, cbase + (c + 1) * 128, rhs_fn, hn_k[b], TT)
                psb, pkb = pp.get()
                proj(psb, pkb, wrot_t, wrot_k, rbase + c * 128, rbase + (c + 1) * 128, rhs_fn, hn_k[b], TT)
                rope(b, psa, pka, psb, pkb, 128, dst[:, c, :], dk)
        kb.dma("sync", qT_o.rearrange("(c p) t -> p c t", p=128)[:, :, t0:t0 + TT], q_s[:, :, :], reads=[q_k], is_output=True)
        kb.dma("sync", qiT_o.rearrange("(c p) t -> p c t", p=128)[:, :, t0:t0 + TT], qi_s[:, :, :], reads=[qi_k], is_output=True)
        psa, pka = pp.get()
        proj(psa, pka, win_t, win_k, C_KI, C_KI + 64, rhs_fn, hn_k[b], TT)
        psb, pkb = pp.get()
        proj(psb, pkb, wrot_t, wrot_k, 1024, 1088, rhs_fn, hn_k[b], TT)
        rope(b, psa, pka, psb, pkb, 64, ki_s[:, :], ki_k)
        kb.dma("sync", kiT_o[:, t0:t0 + TT], ki_s[:, :], reads=[ki_k], is_output=True)
        ps, pk = pp.get()
        proj(ps, pk, win_t, win_k, C_WI, C_WI + 8, rhs_fn, hn_k[b], TT)
        kb.op("scalar", lambda h, ps=ps: h.activation(out=wi_s[:, :], in_=ps[:8, :TT], func=AF.Copy, scale=float(8 ** -0.5 * 64 ** -0.5)),
              reads=[pk], writes=[wi_k])
        kb.dma("sync", wiT_o[:, t0:t0 + TT], wi_s[:, :], reads=[wi_k], is_output=True)
        ps, pk = pp.get()
        proj(ps, pk, win_t, win_k, C_KV, C_KV + 128, rhs_fn, hn_k[b], TT)
        kb.op("scalar", lambda h, ps=ps: h.activation(out=ckv_s[:, :], in_=ps[:, :TT], func=AF.Copy), reads=[pk], writes=[ckv_k])
        rmsnorm_fm(kb, pp, [ckv_s[:, :]], [ckv_k], kvn_t, kvn_k, ones[:, :], ones_trk, [ckvn_s[:, :]], [ckvn_k], scr, 1, TT, 128)
        psa, pka = pp.get()
        kb.op("tensor", lambda h, psa=psa: h.matmul(psa[:64, :TT], lhsT=wuk_t[:, 0, :], rhs=ckvn_s[:, :], start=True, stop=True),
              reads=[wuk_k, ckvn_k], writes=[pka])
        psb, pkb = pp.get()
        kb.op("tensor", lambda h, psb=psb: h.matmul(psb[:64, :TT], lhsT=wukr_t[:, 0, :], rhs=ckvn_s[:, :], start=True, stop=True),
              reads=[wukr_k, ckvn_k], writes=[pkb])
        rope(b, psa, pka, psb, pkb, 64, k_s[:, :], k_k)
        kb.dma("sync", kT_o[:, t0:t0 + TT], k_s[:, :], reads=[k_k], is_output=True)
        ps, pk = pp.get()
        for j in range(4):
            kb.op("tensor", lambda h, ps=ps, j=j: h.matmul(ps[:, j * 64:(j + 1) * 64], lhsT=ckvn_s[:, j * 128:(j + 1) * 128], rhs=wuv_t[:, 0, :],
                                                           start=True, stop=True),
                  reads=[wuv_k, ckvn_k], writes=[pk])
        kb.op("scalar", lambda h, ps=ps: h.activation(out=v_s[:, :, :], in_=ps[:, 0:256].rearrange("p (j d) -> p j d", d=64), func=AF.Copy),
              reads=[pk], writes=[v_k])
        kb.dma("sync", v_o[t0:t0 + TT, :].rearrange("(j p) d -> p j d", p=128), v_s[:, :, :], reads=[v_k], is_output=True)
        for g in range(4):
            w = 2 ** (g + 1)
            if i == 0:
                ps, pk = pp.get()
                proj(ps, pk, win_t, win_k, C_U + g * 128, C_U + (g + 1) * 128, lambda kc: halo_s[:, kc, :], halo_k, HALO)
                kb.op("scalar", lambda h, ps=ps, g=g: h.activation(out=u_s[g][:, 0:HALO], in_=ps[:, :HALO], func=AF.Copy),
                      reads=[pk], writes=[u_k[g]])
            else:
                kb.op("gpsimd", lambda h, g=g: h.tensor_copy(out=u_s[g][:, 0:HALO], in_=u_s[g][:, TT:TT + HALO]),
                      reads=[u_k[g]], writes=[u_k[g]])
            ps, pk = pp.get()
            proj(ps, pk, win_t, win_k, C_U + g * 128, C_U + (g + 1) * 128, rhs_fn, hn_k[b], TT)
            kb.op("scalar", lambda h, ps=ps, g=g: h.activation(out=u_s[g][:, HALO:W], in_=ps[:, :TT], func=AF.Copy),
                  reads=[pk], writes=[u_k[g]])
            src, srck = u_s[g], u_k[g]
            bufs = [(sA, sA_k), (sB, sB_k)]
            sh = 1
            lo = 0
            for step in range(g + 1):
                dst, dstk = bufs[step % 2]
                lo = lo + sh
                kb.op("gpsimd", lambda h, src=src, dst=dst, lo=lo, sh=sh: h.tensor_tensor(out=dst[:, lo:W], in0=src[:, lo:W], in1=src[:, lo - sh:W - sh], op=ALU.add),
                      reads=[srck], writes=[dstk])
                src, srck = dst, dstk
                sh *= 2
            tab, tabk = (invc0_t, invc0_k) if i == 0 else (invc_t, invc_k)
            j = rope_i[0] % 2
            rope_i[0] += 1
            kb.op("gpsimd", lambda h, src=src, tab=tab, g=g, j=j: h.tensor_tensor(out=t1_s[j][:, :], in0=src[:, HALO:W], in1=tab[:, g, :], op=ALU.mult),
                  reads=[srck, tabk], writes=[t1_k[j]])
            kb.op("gpsimd", lambda h, g=g, j=j: h.tensor_tensor(out=pl_s[g][:, :], in0=t1_s[j][:, :], in1=u_s[g][:, HALO:W], op=ALU.subtract),
                  reads=[t1_k[j], u_k[g]], writes=[pl_k[g]])
            ps, pk = pp.get()
            kb.op("tensor", lambda h, ps=ps, g=g: h.matmul(ps[:, :TT], lhsT=pw_t[:, g, :], rhs=pl_s[g][:, :], start=True, stop=True),
                  reads=[pw_k, pl_k[g]], writes=[pk])
            kb.op("scalar", lambda h, ps=ps, g=g: h.activation(out=yb_s[:, g, :], in_=ps[:, :TT], func=AF.Copy, scale=psc_t[:, g:g + 1]),
                  reads=[pk, psc_k], writes=[yb_k])
        kb.dma("sync", ybT_o.rearrange("(c p) t -> p c t", p=128)[:, :, t0:t0 + TT], yb_s[:, :, :], reads=[yb_k], is_output=True)
    return kb.finish(), kb


TOPK = 256
NEG = -1.0e30


def build_e2(NQB, NIT=20, blk_list=None):
    kb = KB()
    nc = kb.nc
    NQ = NQB * 128
    nmax = 2 * NQB
    L = nmax * 128
    qh = kb.dram("qh", [64, 8, NQ], BF16, "ExternalInput")
    qih = kb.dram("qih", [64, 8, NQ], BF16, "ExternalInput")
    wi_tok = kb.dram("wi_tok", [128, NQB, 8], F32, "ExternalInput")
    qrel = kb.dram("qrel", [128, NQB], F32, "ExternalInput")
    kiT = kb.dram("kiT", [64, L], BF16, "ExternalInput")
    kT = kb.dram("kT", [64, L], BF16, "ExternalInput")
    vv = kb.dram("v", [128, nmax, 64], BF16, "ExternalInput")
    iota_in = kb.dram("iota", [128, 256], F32, "ExternalInput")
    ident_in = kb.dram("ident", [128, 128], BF16, "ExternalInput")
    ya_o = kb.dram("ya", [128, NQB, 512], BF16, "ExternalOutput")

    def ld(name, ap, shape, dt):
        t = kb.sbuf(name, shape, dt)
        k = Trk()
        kb.dma("sync", t[tuple(slice(None) for _ in shape)], ap, writes=[k])
        return t, k

    ki_t, ki_k = ld("ki_t", kiT[:, :], [64, L], BF16)
    k_t, k_k = ld("k_t", kT[:, :], [64, L], BF16)
    v_t, v_k = ld("v_t", vv[:, :, :], [128, nmax, 64], BF16)
    wi_t, wi_k = ld("wi_t", wi_tok[:, :, :], [128, NQB, 8], F32)
    qr_t, qr_k = ld("qr_t", qrel[:, :], [128, NQB], F32)
    io_t, io_k = ld("io_t", iota_in[:, :], [128, 256], F32)
    id_t, id_k = ld("id_t", ident_in[:, :], [128, 128], BF16)

    ps_f = [kb.psum("psf%d" % i, [128, 512], F32) for i in range(5)]
    ps_fk = [Trk() for _ in range(5)]
    ps_i = [0]

    def getps():
        j = ps_i[0] % 5
        ps_i[0] += 1
        return ps_f[j], ps_fk[j]

    ps_t = [kb.psum("pst%d" % i, [128, 1024], BF16) for i in range(2)]
    ps_tk = [Trk() for _ in range(2)]
    ps_o = kb.psum("pso", [128, 512], F32)
    ps_ok = Trk()

    q_s = [kb.sbuf("q_s%d" % b, [64, 8, 128], BF16) for b in range(2)]
    q_k = [Trk() for _ in range(2)]
    qi_s = [kb.sbuf("qi_s%d" % b, [64, 8, 128], BF16) for b in range(2)]
    qi_k = [Trk() for _ in range(2)]
    score = kb.sbuf("score", [128, L], F32)
    score_k = Trk()
    lg = kb.sbuf("lg", [128, L], F32)
    lg_k = Trk()
    P_s = kb.sbuf("P_s", [128, L], BF16)
    P_k = Trk()
    PT_s = kb.sbuf("PT_s", [128, nmax, 128], BF16)
    PT_k = Trk()
    junk = kb.sbuf("junk", [128, L], BF16)
    junk_k = Trk()
    r_s = [kb.sbuf("r%d" % b, [128, 512], F32) for b in range(4)]
    r_k = [Trk() for _ in range(4)]
    mb_s = kb.sbuf("mb", [128, 256], F32)
    mb_k = Trk()
    sm = kb.sbuf("sm", [128, 16], F32)
    lo_k, w0_k, mid_k, cnt_k, tmp_k, hi_k, negm_k, rinv_k = (Trk() for _ in range(8))
    LO, W0, MID, CNT, TMP, HI, NEGM, RINV = (sm[:, j:j + 1] for j in range(8))
    rs = kb.sbuf("rs", [128, 8], F32)
    rs_k = Trk()
    ya_s = [kb.sbuf("ya_s%d" % b, [128, 512], BF16) for b in range(2)]
    ya_k = [Trk() for _ in range(2)]

    def loadq(i):
        b = i % 2
        kb.dma("sync", q_s[b][:, :, :], qh[:, :, i * 128:(i + 1) * 128], writes=[q_k[b]])
        kb.dma("sync", qi_s[b][:, :, :], qih[:, :, i * 128:(i + 1) * 128], writes=[qi_k[b]])

    blocks = list(range(NQB)) if blk_list is None else blk_list
    loadq(blocks[0])
    rb = 0
    for bi, i in enumerate(blocks):
        b = i % 2
        if bi + 1 < len(blocks):
            loadq(blocks[bi + 1])
        n = 2 * i + 2
        nk = 128 * n
        tiles = [(k0, min(512, nk - k0)) for k0 in range(0, nk, 512)]
        for (k0, kw) in tiles:
            for h in range(8):
                ps, pk = getps()
                kb.op("tensor", lambda e, ps=ps, h=h, k0=k0, kw=kw: e.matmul(ps[:, :kw], lhsT=qi_s[b][:, h, :], rhs=ki_t[:, k0:k0 + kw], start=True, stop=True),
                      reads=[qi_k[b], ki_k], writes=[pk])
                j = rb % 4
                rb += 1
                kb.op("scalar", lambda e, ps=ps, j=j, kw=kw: e.activation(out=r_s[j][:, :kw], in_=ps[:, :kw], func=AF.Relu), reads=[pk], writes=[r_k[j]])
                if h == 0:
                    kb.op("vector", lambda e, j=j, k0=k0, kw=kw: e.tensor_scalar(out=score[:, k0:k0 + kw], in0=r_s[j][:, :kw], scalar1=wi_t[:, i, 0:1], scalar2=None, op0=ALU.mult),
                          reads=[r_k[j], wi_k], writes=[score_k])
                else:
                    kb.op("vector", lambda e, j=j, k0=k0, kw=kw, h=h: e.scalar_tensor_tensor(out=score[:, k0:k0 + kw], in0=r_s[j][:, :kw], scalar=wi_t[:, i, h:h + 1],
                                                                                              in1=score[:, k0:k0 + kw], op0=ALU.mult, op1=ALU.add),
                          reads=[r_k[j], wi_k], writes=[score_k])
        kb.op("vector", lambda e: e.tensor_reduce(out=LO, in_=score[:, :nk], axis=AX.X, op=ALU.min), reads=[score_k], writes=[lo_k])
        kb.op("vector", lambda e: e.tensor_scalar(out=mb_s[:, :], in0=io_t[:, :], scalar1=qr_t[:, i:i + 1], scalar2=NEG, op0=ALU.is_gt, op1=ALU.mult),
              reads=[io_k, qr_k], writes=[mb_k])
        kb.op("gpsimd", lambda e: e.tensor_tensor(out=score[:, nk - 256:nk], in0=score[:, nk - 256:nk], in1=mb_s[:, :], op=ALU.add),
              reads=[mb_k, lo_k], writes=[score_k])
        kb.op("vector", lambda e: e.tensor_reduce(out=HI, in_=score[:, :nk], axis=AX.X, op=ALU.max), reads=[score_k], writes=[hi_k])
        kb.op("vector", lambda e: e.tensor_tensor(out=W0, in0=HI, in1=LO, op=ALU.subtract), reads=[hi_k, lo_k], writes=[w0_k])
        thr = float(2 * TOPK - nk) - 0.5
        for it in range(NIT):
            f = 2.0 ** -(it + 1)
            kb.op("vector", lambda e, f=f: e.scalar_tensor_tensor(out=MID, in0=W0, scalar=-f, in1=LO, op0=ALU.mult, op1=ALU.subtract),
                  reads=[w0_k, lo_k], writes=[mid_k])
            kb.op("scalar", lambda e: e.activation(out=junk[:, :nk], in_=score[:, :nk], func=AF.Sign, bias=MID, scale=1.0, accum_out=CNT),
                  reads=[score_k, mid_k], writes=[junk_k, cnt_k])
            kb.op("vector", lambda e, thr=thr: e.tensor_scalar(out=TMP, in0=CNT, scalar1=thr, scalar2=W0, op0=ALU.is_ge, op1=ALU.mult),
                  reads=[cnt_k, w0_k], writes=[tmp_k])
            kb.op("vector", lambda e, f=f: e.scalar_tensor_tensor(out=LO, in0=TMP, scalar=f, in1=LO, op0=ALU.mult, op1=ALU.add),
                  reads=[tmp_k], writes=[lo_k])
        kb.op("vector", lambda e: e.tensor_scalar(out=score[:, :nk], in0=score[:, :nk], scalar1=LO, scalar2=NEG, op0=ALU.is_lt, op1=ALU.mult),
              reads=[lo_k], writes=[score_k])
        yb_ = bi % 2
        for h in range(8):
            for (k0, kw) in tiles:
                ps, pk = getps()
                kb.op("tensor", lambda e, ps=ps, h=h, k0=k0, kw=kw: e.matmul(ps[:, :kw], lhsT=q_s[b][:, h, :], rhs=k_t[:, k0:k0 + kw], start=True, stop=True),
                      reads=[q_k[b], k_k], writes=[pk])
                kb.op("vector", lambda e, ps=ps, k0=k0, kw=kw: e.scalar_tensor_tensor(out=lg[:, k0:k0 + kw], in0=ps[:, :kw], scalar=0.125, in1=score[:, k0:k0 + kw],
                                                                                       op0=ALU.mult, op1=ALU.add),
                      reads=[pk, score_k], writes=[lg_k])
            kb.op("vector", lambda e: e.tensor_reduce(out=NEGM, in_=lg[:, :nk], axis=AX.X, op=ALU.max, negate=True), reads=[lg_k], writes=[negm_k])
            kb.op("scalar", lambda e, h=h: e.activation(out=P_s[:, :nk], in_=lg[:, :nk], func=AF.Exp, bias=NEGM, scale=1.0, accum_out=rs[:, h:h + 1]),
                  reads=[lg_k, negm_k], writes=[P_k, rs_k])
            for j0 in range(0, n, 8):
                jn = min(8, n - j0)
                tb = (j0 // 8) % 2
                for j in range(j0, j0 + jn):
                    s = j - j0
                    kb.op("tensor", lambda e, tb=tb, s=s, j=j: e.transpose(out=ps_t[tb][:, s * 128:(s + 1) * 128], in_=P_s[:, j * 128:(j + 1) * 128], identity=id_t[:, :]),
                          reads=[P_k, id_k], writes=[ps_tk[tb]])
                kb.op("scalar", lambda e, tb=tb, j0=j0, jn=jn: e.activation(out=PT_s[:, j0:j0 + jn, :], in_=ps_t[tb][:, 0:jn * 128].rearrange("p (j q) -> p j q", q=128), func=AF.Copy),
                      reads=[ps_tk[tb]], writes=[PT_k])
            for j in range(n):
                kb.op("tensor", lambda e, j=j, h=h: e.matmul(ps_o[:, h * 64:(h + 1) * 64], lhsT=PT_s[:, j, :], rhs=v_t[:, j, :], start=(j == 0), stop=(j == n - 1)),
                      reads=[PT_k, v_k], writes=[ps_ok])
        kb.op("vector", lambda e: e.reciprocal(out=rs[:, :], in_=rs[:, :]), reads=[rs_k], writes=[rs_k])
        for h in range(8):
            kb.op("scalar", lambda e, h=h: e.activation(out=ya_s[yb_][:, h * 64:(h + 1) * 64], in_=ps_o[:, h * 64:(h + 1) * 64], func=AF.Copy, scale=rs[:, h:h + 1]),
                  reads=[ps_ok, rs_k], writes=[ya_k[yb_]])
        kb.dma("sync", ya_o[:, i, :], ya_s[yb_][:, :], reads=[ya_k[yb_]], is_output=True)
    return kb.finish(), kb


def gdn_consts():
    i = np.arange(128)
    same = (i[:, None] // 64) == (i[None, :] // 64)
    c = {}
    c["ident"] = np.eye(128, dtype=np.float32)
    c["ones"] = np.ones((128, 128), np.float32)
    c["tri"] = (same & (i[:, None] <= i[None, :])).astype(np.float32)
    c["selblk"] = (i[:, None] == (i[None, :] // 64) * 64 + 63).astype(np.float32)
    c["selA"] = np.repeat((i[:, None] == 63), 128, 1).astype(np.float32)
    c["selB"] = np.repeat((i[:, None] == 127), 128, 1).astype(np.float32)
    c["mstrict"] = np.where(same & (i[:, None] > i[None, :]), 0.0, NEG).astype(np.float32)
    c["mTfull"] = np.where(same & (i[None, :] >= i[:, None]), 0.0, NEG).astype(np.float32)
    c["sT01"] = (same & (i[None, :] > i[:, None])).astype(np.float32)
    return c


GDN_CONST_NAMES = ["ident", "ones", "tri", "selblk", "selA", "selB", "mstrict", "mTfull", "sT01"]


def build_o1(L, NH=4):
    kb = KB()
    nc = kb.nc
    TT = 512
    NT = L // TT
    HW = NH * 128
    hnT = kb.dram("hnT", [D, L], BF16, "ExternalInput")
    w_qkv = kb.dram("w_qkv", [D, 3 * HW], F32, "ExternalInput")
    w_z = kb.dram("w_z", [D, HW], F32, "ExternalInput")
    w_ba = kb.dram("w_ba", [D, 2 * NH], F32, "ExternalInput")
    cw_in = kb.dram("cw", [128, 3 * NH * 4], F32, "ExternalInput")
    alog_in = kb.dram("alog_b", [128, NH], F32, "ExternalInput")
    dtb_in = kb.dram("dtb_b", [128, NH], F32, "ExternalInput")
    onorm_in = kb.dram("onorm_b", [128, 128], F32, "ExternalInput")
    cst_in = {n: kb.dram("c_" + n, [128, 128], F32, "ExternalInput") for n in GDN_CONST_NAMES}
    o_out = kb.dram("o_tok", [128, L // 128, HW], BF16, "ExternalOutput")

    def ld(name, ap, shape, dt):
        t = kb.sbuf(name, shape, dt)
        k = Trk()
        kb.dma("sync", t[tuple(slice(None) for _ in shape)], ap, writes=[k])
        return t, k

    wqkv_t, wqkv_k = load_weight_bf16(kb, "wqkv", w_qkv, D, 3 * HW)
    wz_t, wz_k = load_weight_bf16(kb, "wz", w_z, D, HW)
    wba_t, wba_k = load_weight_bf16(kb, "wba", w_ba, D, 2 * NH)
    cw_t, cw_k = ld("cw_t", cw_in[:, :], [128, 3 * NH * 4], F32)
    alog_t, alog_k = ld("alog_t", alog_in[:, :], [128, NH], F32)
    dtb_t, dtb_k = ld("dtb_t", dtb_in[:, :], [128, NH], F32)
    onorm_t, onorm_k = ld("onorm_t", onorm_in[:, :], [128, 128], F32)
    C = {}
    CK = {}
    for n in GDN_CONST_NAMES:
        C[n], CK[n] = ld("cs_" + n, cst_in[n][:, :], [128, 128], F32)
    ones_bf = kb.sbuf("ones_bf", [128, 128], BF16)
    ones_bfk = Trk()
    kb.op("vector", lambda e: e.memset(ones_bf[:], 1.0), writes=[ones_bfk])
    cst = kb.sbuf("cst", [128, 2], F32)
    cst_k = Trk()
    kb.op("vector", lambda e: e.memset(cst[:, 0:1], EPS), writes=[cst_k])
    kb.op("vector", lambda e: e.memset(cst[:, 1:2], 1.0), writes=[cst_k])
    EPSC, ONEC = cst[:, 0:1], cst[:, 1:2]
    ea_t = kb.sbuf("ea_t", [128, NH], F32)
    ea_k = Trk()
    kb.op("scalar", lambda e: e.activation(out=ea_t[:, :], in_=alog_t[:, :], func=AF.Exp), reads=[alog_k], writes=[ea_k])

    pp = PsumPool(kb)
    hn_s = [kb.sbuf("hn%d" % b, [128, 8, TT], BF16) for b in range(2)]
    hn_k = [Trk() for _ in range(2)]
    XW = TT + 3
    x_ext = [[kb.sbuf("x%d_%d" % (ty, h), [128, XW], F32) for h in range(NH)] for ty in range(3)]
    x_k = [[Trk() for _ in range(NH)] for _ in range(3)]
    xc = [[kb.sbuf("xc%d_%d" % (ty, h), [128, TT], F32) for h in range(NH)] for ty in range(3)]
    xc_k = [[Trk() for _ in range(NH)] for _ in range(3)]
    acc_s = [kb.sbuf("acc%d" % b, [128, TT], F32) for b in range(2)]
    acc_k = [Trk() for _ in range(2)]
    sq_s = kb.sbuf("sq", [128, TT], BF16)
    sq_k = Trk()
    rn_s = kb.sbuf("rn", [128, TT], F32)
    rn_k = Trk()
    gz_s = [kb.sbuf("gz%d" % j, [128, HW], F32) for j in range(4)]
    gz_k = [Trk() for _ in range(4)]
    NS = 10
    sc_s = [kb.sbuf("sc%d" % j, [128, NS, NH], F32) for j in range(4)]
    sc_k = [[Trk() for _ in range(NS)] for _ in range(4)]
    BETA, GC, EG, EDL, EGLA, EGLB, BEG, GG, T0, T1 = range(NS)
    S_s = [kb.sbuf("S%d" % h, [128, 128], F32) for h in range(NH)]
    S_k = [Trk() for _ in range(NH)]
    for h in range(NH):
        kb.op("gpsimd", lambda e, h=h: e.memset(S_s[h][:, :], 0.0), writes=[S_k[h]])
    for ty in range(3):
        for h in range(NH):
            kb.op("gpsimd", lambda e, ty=ty, h=h: e.memset(x_ext[ty][h][:, 0:3], 0.0), writes=[x_k[ty][h]])
    o_s = [kb.sbuf("o_s%d" % b, [128, HW], BF16) for b in range(2)]
    o_k = [Trk() for _ in range(2)]

    NSET = 2
    TN = ["kbe", "kdec", "vb", "dg", "egf", "decT", "decS", "WT", "nkk", "M", "MT", "M2", "M2T", "TT", "attnT", "qdecT", "val", "kcdT", "vn", "tmpa", "tmpb"]
    tmp = [{n: kb.sbuf("t%d_%s" % (s, n), [128, 256 if n == "dg" else 128], F32) for n in TN} for s in range(NSET)]
    tmk = [{n: Trk() for n in TN} for s in range(NSET)]
    ss_s = kb.sbuf("ss_s", [128, 4], F32)
    ss_k = Trk()
    junk = kb.sbuf("junk_o", [128, 128], F32)
    junk_k = Trk()

    hn_v = hnT.rearrange("(c p) t -> p c t", p=128)

    def load(i):
        b = i % 2
        kb.dma("sync", hn_s[b][:, :, :], hn_v[:, :, i * TT:(i + 1) * TT], writes=[hn_k[b]])

    def mm(ps, pk, M, N, lhsT, rhs, reads, start=True, stop=True, p0=0):
        kb.op("tensor", lambda e: e.matmul(ps[p0:p0 + M, :N], lhsT=lhsT, rhs=rhs, start=start, stop=stop), reads=reads, writes=[pk])

    load(0)
    unit = 0
    for i in range(NT):
        b = i % 2
        if i + 1 < NT:
            load(i + 1)
        for ty in range(3):
            for h in range(NH):
                ps, pk = pp.get()
                c0 = ty * HW + h * 128
                for kc in range(8):
                    mm(ps, pk, 128, TT, wqkv_t[:, kc, c0:c0 + 128], hn_s[b][:, kc, :], [wqkv_k, hn_k[b]], kc == 0, kc == 7)
                if i > 0:
                    kb.op("gpsimd", lambda e, ty=ty, h=h: e.tensor_copy(out=x_ext[ty][h][:, 0:3], in_=x_ext[ty][h][:, TT:TT + 3]),
                          reads=[x_k[ty][h]], writes=[x_k[ty][h]])
                kb.op("scalar", lambda e, ps=ps, ty=ty, h=h: e.activation(out=x_ext[ty][h][:, 3:XW], in_=ps[:, :TT], func=AF.Copy),
                      reads=[pk], writes=[x_k[ty][h]])
        for j in range(4):
            ps, pk = pp.get()
            for kc in range(8):
                mm(ps, pk, 128, HW, hn_s[b][:, kc, j * 128:(j + 1) * 128], wz_t[:, kc, :], [wz_k, hn_k[b]], kc == 0, kc == 7)
            kb.op("scalar", lambda e, ps=ps, j=j: e.activation(out=gz_s[j][:, :], in_=ps[:, :HW], func=AF.Silu), reads=[pk], writes=[gz_k[j]])
            for h in range(NH):
                kb.op("gpsimd", lambda e, j=j, h=h: e.tensor_tensor(out=gz_s[j][:, h * 128:(h + 1) * 128], in0=gz_s[j][:, h * 128:(h + 1) * 128], in1=onorm_t[:, :], op=ALU.mult),
                      reads=[onorm_k], writes=[gz_k[j]])
            ps, pk = pp.get()
            for kc in range(8):
                mm(ps, pk, 128, 2 * NH, hn_s[b][:, kc, j * 128:(j + 1) * 128], wba_t[:, kc, :], [wba_k, hn_k[b]], kc == 0, kc == 7)
            sc, sk = sc_s[j], sc_k[j]
            kb.op("scalar", lambda e, ps=ps, sc=sc: e.activation(out=sc[:, BETA, :], in_=ps[:, 0:NH], func=AF.Sigmoid), reads=[pk], writes=[sk[BETA]])
            kb.op("vector", lambda e, ps=ps, sc=sc: e.tensor_tensor(out=sc[:, T0, :], in0=ps[:, NH:2 * NH], in1=dtb_t[:, :], op=ALU.add), reads=[pk, dtb_k], writes=[sk[T0]])
            kb.op("vector", lambda e, sc=sc: e.scalar_tensor_tensor(out=sc[:, T1, :], in0=sc[:, T0, :], scalar=-1.0, in1=sc[:, T0, :], op0=ALU.mult, op1=ALU.max), reads=[sk[T0]], writes=[sk[T1]])
            kb.op("scalar", lambda e, sc=sc: e.activation(out=sc[:, T1, :], in_=sc[:, T1, :], func=AF.Exp, scale=-1.0), reads=[sk[T1]], writes=[sk[T1]])
            kb.op("scalar", lambda e, sc=sc: e.activation(out=sc[:, T1, :], in_=sc[:, T1, :], func=AF.Ln, bias=ONEC, scale=1.0), reads=[sk[T1], cst_k], writes=[sk[T1]])
            kb.op("vector", lambda e, sc=sc: e.scalar_tensor_tensor(out=sc[:, T0, :], in0=sc[:, T0, :], scalar=0.0, in1=sc[:, T1, :], op0=ALU.max, op1=ALU.add),
                  reads=[sk[T1]], writes=[sk[T0]])
            kb.op("vector", lambda e, sc=sc: e.scalar_tensor_tensor(out=sc[:, GG, :], in0=sc[:, T0, :], scalar=-1.0, in1=ea_t[:, :], op0=ALU.mult, op1=ALU.mult),
                  reads=[sk[T0], ea_k], writes=[sk[GG]])
            ps2, pk2 = pp.get()
            mm(ps2, pk2, 128, NH, C["tri"][:, :], sc[:, GG, :], [CK["tri"], sk[GG]])
            kb.op("scalar", lambda e, ps2=ps2, sc=sc: e.activation(out=sc[:, GC, :], in_=ps2[:, 0:NH], func=AF.Copy), reads=[pk2], writes=[sk[GC]])
            kb.op("scalar", lambda e, ps2=ps2, sc=sc: e.activation(out=sc[:, EG, :], in_=ps2[:, 0:NH], func=AF.Exp), reads=[pk2], writes=[sk[EG]])
            ps3, pk3 = pp.get()
            mm(ps3, pk3, 128, NH, C["selblk"][:, :], sc[:, GC, :], [CK["selblk"], sk[GC]])
            kb.op("vector", lambda e, ps3=ps3, sc=sc: e.tensor_tensor(out=sc[:, EDL, :], in0=ps3[:, 0:NH], in1=sc[:, GC, :], op=ALU.subtract), reads=[pk3, sk[GC]], writes=[sk[EDL]])
            kb.op("scalar", lambda e, sc=sc: e.activation(out=sc[:, EDL, :], in_=sc[:, EDL, :], func=AF.Exp), reads=[sk[EDL]], writes=[sk[EDL]])
            ps4, pk4 = pp.get()
            mm(ps4, pk4, 128, NH, C["selA"][:, :], sc[:, GC, :], [CK["selA"], sk[GC]])
            kb.op("scalar", lambda e, ps4=ps4, sc=sc: e.activation(out=sc[:, EGLA, :], in_=ps4[:, 0:NH], func=AF.Exp), reads=[pk4], writes=[sk[EGLA]])
            ps5, pk5 = pp.get()
            mm(ps5, pk5, 128, NH, C["selB"][:, :], sc[:, GC, :], [CK["selB"], sk[GC]])
            kb.op("scalar", lambda e, ps5=ps5, sc=sc: e.activation(out=sc[:, EGLB, :], in_=ps5[:, 0:NH], func=AF.Exp), reads=[pk5], writes=[sk[EGLB]])
            kb.op("vector", lambda e, sc=sc: e.tensor_tensor(out=sc[:, BEG, :], in0=sc[:, BETA, :], in1=sc[:, EG, :], op=ALU.mult), reads=[sk[BETA], sk[EG]], writes=[sk[BEG]])
        for ty in range(3):
            for h in range(NH):
                a = (ty * NH + h) % 2
                xe, xk = x_ext[ty][h], x_k[ty][h]
                cb = (ty * NH + h) * 4
                kb.op("vector", lambda e, xe=xe, a=a, cb=cb: e.tensor_scalar(out=acc_s[a][:, :], in0=xe[:, 0:TT], scalar1=cw_t[:, cb:cb + 1], scalar2=None, op0=ALU.mult),
                      reads=[xk, cw_k], writes=[acc_k[a]])
                for tap in range(1, 4):
                    kb.op("vector", lambda e, xe=xe, a=a, cb=cb, tap=tap: e.scalar_tensor_tensor(out=acc_s[a][:, :], in0=xe[:, tap:tap + TT], scalar=cw_t[:, cb + tap:cb + tap + 1],
                                                                                                in1=acc_s[a][:, :], op0=ALU.mult, op1=ALU.add),
                          reads=[xk, cw_k], writes=[acc_k[a]])
                kb.op("scalar", lambda e, a=a, ty=ty, h=h: e.activation(out=xc[ty][h][:, :], in_=acc_s[a][:, :], func=AF.Silu), reads=[acc_k[a]], writes=[xc_k[ty][h]])
                if ty < 2:
                    kb.op("scalar", lambda e, ty=ty, h=h: e.activation(out=sq_s[:, :], in_=xc[ty][h][:, :], func=AF.Square), reads=[xc_k[ty][h]], writes=[sq_k])
                    ps, pk = pp.get()
                    mm(ps, pk, 128, TT, ones_bf[:, :], sq_s[:, :], [ones_bfk, sq_k])
                    kb.op("scalar", lambda e, ps=ps: e.activation(out=rn_s[:, :], in_=ps[:, :TT], func=AF.Sqrt, bias=EPSC, scale=1.0), reads=[pk, cst_k], writes=[rn_k])
                    kb.op("vector", lambda e: e.reciprocal(out=rn_s[:, :], in_=rn_s[:, :]), reads=[rn_k], writes=[rn_k])
                    scl = float(128 ** -0.5) if ty == 0 else 1.0
                    kb.op("vector", lambda e, ty=ty, h=h, scl=scl: e.scalar_tensor_tensor(out=xc[ty][h][:, :], in0=xc[ty][h][:, :], scalar=scl, in1=rn_s[:, :], op0=ALU.mult, op1=ALU.mult),
                          reads=[rn_k], writes=[xc_k[ty][h]])
        for j in range(4):
            sc, sk = sc_s[j], sc_k[j]
            cols = slice(j * 128, (j + 1) * 128)
            ob = (i * 4 + j) % 2
            for h in range(NH):
                s = unit % NSET
                unit += 1
                t, tk = tmp[s], tmk[s]
                qn, kn, vc = xc[0][h], xc[1][h], xc[2][h]
                qk_, kk_, vk_ = xc_k[0][h], xc_k[1][h], xc_k[2][h]
                hs = slice(h, h + 1)
                ps, pk = pp.get()
                kb.op("tensor", lambda e, ps=ps, kn=kn: e.transpose(out=ps[:, 0:128], in_=kn[:, cols], identity=C["ident"][:, :]), reads=[kk_, CK["ident"]], writes=[pk])
                kb.op("scalar", lambda e, ps=ps, t=t: e.activation(out=t["kbe"][:, :], in_=ps[:, 0:128], func=AF.Copy, scale=sc[:, BEG, hs]), reads=[pk, sk[BEG]], writes=[tk["kbe"]])
                kb.op("vector", lambda e, ps=ps, t=t: e.tensor_scalar(out=t["kdec"][:, :], in0=ps[:, 0:128], scalar1=sc[:, EDL, hs], scalar2=None, op0=ALU.mult), reads=[pk, sk[EDL]], writes=[tk["kdec"]])
                ps, pk = pp.get()
                kb.op("tensor", lambda e, ps=ps, vc=vc: e.transpose(out=ps[:, 0:128], in_=vc[:, cols], identity=C["ident"][:, :]), reads=[vk_, CK["ident"]], writes=[pk])
                kb.op("scalar", lambda e, ps=ps, t=t: e.activation(out=t["vb"][:, :], in_=ps[:, 0:128], func=AF.Copy, scale=sc[:, BETA, hs]), reads=[pk, sk[BETA]], writes=[tk["vb"]])
                kb.op("gpsimd", lambda e, t=t: e.tensor_scalar(out=t["dg"][:, 0:128], in0=C["ident"][:, :], scalar1=sc[:, GC, hs], scalar2=None, op0=ALU.mult), reads=[CK["ident"], sk[GC]], writes=[tk["dg"]])
                kb.op("gpsimd", lambda e, t=t: e.tensor_scalar(out=t["dg"][:, 128:256], in0=C["ident"][:, :], scalar1=sc[:, BETA, hs], scalar2=None, op0=ALU.mult), reads=[CK["ident"], sk[BETA]], writes=[tk["dg"]])
                psr, pkr = pp.get()
                mm(psr, pkr, 128, 256, C["ones"][:, :], t["dg"][:, :], [CK["ones"], tk["dg"]])
                kb.op("scalar", lambda e, t=t, psr=psr: e.activation(out=t["egf"][:, :], in_=psr[:, 0:128], func=AF.Exp), reads=[pkr], writes=[tk["egf"]])
                kb.op("vector", lambda e, t=t, psr=psr: e.scalar_tensor_tensor(out=t["decT"][:, :], in0=psr[:, 0:128], scalar=sc[:, GC, hs], in1=C["mTfull"][:, :], op0=ALU.subtract, op1=ALU.add),
                      reads=[pkr, sk[GC], CK["mTfull"]], writes=[tk["decT"]])
                kb.op("scalar", lambda e, t=t: e.activation(out=t["decT"][:, :], in_=t["decT"][:, :], func=AF.Exp), reads=[tk["decT"]], writes=[tk["decT"]])
                kb.op("vector", lambda e, t=t, psr=psr: e.scalar_tensor_tensor(out=t["decS"][:, :], in0=psr[:, 0:128], scalar=sc[:, GC, hs], in1=C["mstrict"][:, :], op0=ALU.subtract, op1=ALU.subtract),
                      reads=[pkr, sk[GC], CK["mstrict"]], writes=[tk["decS"]])
                kb.op("scalar", lambda e, t=t: e.activation(out=t["decS"][:, :], in_=t["decS"][:, :], func=AF.Exp, scale=-1.0), reads=[tk["decS"]], writes=[tk["decS"]])
                kb.op("vector", lambda e, t=t, psr=psr: e.tensor_tensor(out=t["WT"][:, :], in0=psr[:, 128:256], in1=t["decT"][:, :], op=ALU.mult), reads=[pkr, tk["decT"]], writes=[tk["WT"]])
                psk, pkk = pp.get()
                mm(psk, pkk, 128, 128, kn[:, cols], kn[:, cols], [kk_])
                kb.op("vector", lambda e, t=t, psk=psk: e.scalar_tensor_tensor(out=t["nkk"][:, :], in0=psk[:, 0:128], scalar=-1.0, in1=C["sT01"][:, :], op0=ALU.mult, op1=ALU.mult),
                      reads=[pkk, CK["sT01"]], writes=[tk["nkk"]])
                kb.op("vector", lambda e, t=t, psk=psk: e.scalar_tensor_tensor(out=t["M"][:, :], in0=psk[:, 0:128], scalar=sc[:, BETA, hs], in1=t["decS"][:, :], op0=ALU.mult, op1=ALU.mult),
                      reads=[pkk, sk[BETA], tk["decS"]], writes=[tk["M"]])
                kb.op("gpsimd", lambda e, t=t: e.tensor_scalar(out=t["M"][:, :], in0=t["M"][:, :], scalar1=-1.0, scalar2=None, op0=ALU.mult), reads=[tk["M"]], writes=[tk["M"]])
                kb.op("gpsimd", lambda e, t=t: e.tensor_tensor(out=t["MT"][:, :], in0=t["nkk"][:, :], in1=t["WT"][:, :], op=ALU.mult), reads=[tk["nkk"], tk["WT"]], writes=[tk["MT"]])
                kb.op("gpsimd", lambda e, t=t: e.tensor_tensor(out=t["TT"][:, :], in0=t["MT"][:, :], in1=C["ident"][:, :], op=ALU.add), reads=[tk["MT"], CK["ident"]], writes=[tk["TT"]])
                psa, pka = pp.get()
                mm(psa, pka, 128, 128, kn[:, cols], qn[:, cols], [kk_, qk_])
                kb.op("vector", lambda e, t=t, psa=psa: e.tensor_tensor(out=t["attnT"][:, :], in0=psa[:, 0:128], in1=t["decT"][:, :], op=ALU.mult), reads=[pka, tk["decT"]], writes=[tk["attnT"]])
                kb.op("gpsimd", lambda e, t=t, qn=qn: e.tensor_tensor(out=t["qdecT"][:, :], in0=qn[:, cols], in1=t["egf"][:, :], op=ALU.mult), reads=[qk_, tk["egf"]], writes=[tk["qdecT"]])
                cur, curT = "M", "MT"
                nxt, nxtT = "M2", "M2T"
                for lev in range(1, 6):
                    ps1, pk1 = pp.get()
                    mm(ps1, pk1, 128, 128, t[curT][:, :], t[cur][:, :], [tk[curT], tk[cur]])
                    kb.op("scalar", lambda e, t=t, ps1=ps1, nxt=nxt: e.activation(out=t[nxt][:, :], in_=ps1[:, 0:128], func=AF.Copy), reads=[pk1], writes=[tk[nxt]])
                    if lev < 5:
                        ps2, pk2 = pp.get()
                        mm(ps2, pk2, 128, 128, t[cur][:, :], t[curT][:, :], [tk[curT], tk[cur]])
                        kb.op("vector", lambda e, t=t, ps2=ps2, nxtT=nxtT: e.tensor_copy(out=t[nxtT][:, :], in_=ps2[:, 0:128]), reads=[pk2], writes=[tk[nxtT]])
                    ps3, pk3 = pp.get()
                    mm(ps3, pk3, 128, 128, t[nxt][:, :], t["TT"][:, :], [tk[nxt], tk["TT"]])
                    kb.op("vector", lambda e, t=t, ps3=ps3: e.tensor_tensor(out=t["TT"][:, :], in0=t["TT"][:, :], in1=ps3[:, 0:128], op=ALU.add), reads=[pk3], writes=[tk["TT"]])
                    cur, curT, nxt, nxtT = nxt, nxtT, cur, curT
                psv, pkv = pp.get()
                mm(psv, pkv, 128, 128, t["TT"][:, :], t["vb"][:, :], [tk["TT"], tk["vb"]])
                kb.op("scalar", lambda e, t=t, psv=psv: e.activation(out=t["val"][:, :], in_=psv[:, 0:128], func=AF.Copy), reads=[pkv], writes=[tk["val"]])
                psc, pkc = pp.get()
                mm(psc, pkc, 128, 128, t["kbe"][:, :], t["TT"][:, :], [tk["TT"], tk["kbe"]])
                kb.op("scalar", lambda e, t=t, psc=psc: e.activation(out=t["kcdT"][:, :], in_=psc[:, 0:128], func=AF.Copy), reads=[pkc], writes=[tk["kcdT"]])
                pso, pko = pp.get()
                for c in range(2):
                    r0 = c * 64
                    rows = slice(r0, r0 + 64)
                    psn, pkn = pp.get()
                    mm(psn, pkn, 64, 128, t["kcdT"][:, rows], S_s[h][:, :], [tk["kcdT"], S_k[h]], p0=r0)
                    kb.op("vector", lambda e, t=t, psn=psn, rows=rows: e.tensor_tensor(out=t["vn"][rows, :], in0=t["val"][rows, :], in1=psn[rows, 0:128], op=ALU.subtract),
                          reads=[pkn, tk["val"]], writes=[tk["vn"]])
                    mm(pso, pko, 64, 128, t["qdecT"][:, rows], S_s[h][:, :], [tk["qdecT"], S_k[h]], True, False, p0=r0)
                    mm(pso, pko, 64, 128, t["attnT"][rows, rows], t["vn"][rows, :], [tk["attnT"], tk["vn"]], False, True, p0=r0)
                    pss, pks = pp.get()
                    mm(pss, pks, 128, 128, t["kdec"][rows, :], t["vn"][rows, :], [tk["kdec"], tk["vn"]])
                    egl = EGLA if c == 0 else EGLB
                    kb.op("vector", lambda e, pss=pss, egl=egl: e.scalar_tensor_tensor(out=S_s[h][:, :], in0=S_s[h][:, :], scalar=sc[:, egl, hs], in1=pss[:, 0:128], op0=ALU.mult, op1=ALU.add),
                          reads=[pks, sk[egl]], writes=[S_k[h]])
                kb.op("scalar", lambda e, pso=pso, h=h: e.activation(out=junk[:, :], in_=pso[:, 0:128], func=AF.Square, accum_out=ss_s[:, h:h + 1]), reads=[pko], writes=[junk_k, ss_k])
                kb.op("scalar", lambda e, h=h: e.activation(out=ss_s[:, h:h + 1], in_=ss_s[:, h:h + 1], func=AF.Sqrt, bias=EPSC, scale=1.0 / 128), reads=[ss_k, cst_k], writes=[ss_k])
                kb.op("vector", lambda e, h=h: e.reciprocal(out=ss_s[:, h:h + 1], in_=ss_s[:, h:h + 1]), reads=[ss_k], writes=[ss_k])
                kb.op("vector", lambda e, pso=pso, h=h: e.scalar_tensor_tensor(out=o_s[ob][:, h * 128:(h + 1) * 128], in0=pso[:, 0:128], scalar=ss_s[:, h:h + 1],
                                                                               in1=gz_s[j][:, h * 128:(h + 1) * 128], op0=ALU.mult, op1=ALU.mult),
                      reads=[pko, ss_k, gz_k[j]], writes=[o_k[ob]])
            kb.dma("sync", o_out[:, i * 4 + j, :], o_s[ob][:, :], reads=[o_k[ob]], is_output=True)
    return kb.finish(), kb


def build_norm0(T):
    kb = KB()
    TT = 512
    NT = T // TT
    hT = kb.dram("hT", [D, T], F32, "ExternalInput")
    g_in = kb.dram("g", [D], F32, "ExternalInput")
    hn_out = kb.dram("hn_out", [D, T], BF16, "ExternalOutput")
    g_t, g_k = load_vec_col(kb, "g_s", g_in, D)
    ones = kb.sbuf("ones", [128, 128], BF16)
    ones_trk = Trk()
    kb.op("vector", lambda h: h.memset(ones[:], 1.0), writes=[ones_trk])
    eps_t = kb.sbuf("eps", [128, 1], F32)
    eps_trk = Trk()
    kb.op("vector", lambda h: h.memset(eps_t[:], EPS), writes=[eps_trk])
    pp = PsumPool(kb)
    h_s = [kb.sbuf("h%d" % b, [128, 8, TT], F32) for b in range(2)]
    h_k = [[Trk() for _ in range(8)] for _ in range(2)]
    hn_s = [kb.sbuf("hn%d" % b, [128, 8, TT], BF16) for b in range(2)]
    hn_k = [[Trk() for _ in range(8)] for _ in range(2)]
    sq_s = kb.sbuf("sq", [128, 8, TT], BF16)
    sq_k = [Trk() for _ in range(8)]
    rstd_s = kb.sbuf("rstd", [128, TT], F32)
    rstd_k = Trk()
    scr = {"sq": [(sq_s[:, c, :], sq_k[c]) for c in range(8)], "rstd": (rstd_s[:, :], rstd_k), "eps": (eps_t[:, 0:1], eps_trk)}
    hT_v = hT.rearrange("(c p) t -> p c t", p=128)
    hno_v = hn_out.rearrange("(c p) t -> p c t", p=128)
    for i in range(NT):
        b = i % 2
        t0 = i * TT
        kb.dma("sync", h_s[b][:, :, :], hT_v[:, :, t0:t0 + TT], writes=h_k[b])
        rmsnorm_fm(kb, pp, [h_s[b][:, c, :] for c in range(8)], h_k[b], g_t, g_k, ones[:, :], ones_trk,
                   [hn_s[b][:, c, :] for c in range(8)], hn_k[b], scr, 8, TT, D)
        kb.dma("sync", hno_v[:, :, t0:t0 + TT], hn_s[b][:, :, :], reads=hn_k[b], is_output=True)
    return kb.finish(), kb


BF = ml_dtypes.bfloat16
B_, L_, T_ = 4, 8192, 4096
NQB_ = 32
_PROGS = {}


def _prog(name):
    if name not in _PROGS:
        if name == "n0":
            _PROGS[name] = build_norm0(T_)[0]
        elif name == "e1":
            _PROGS[name] = build_e1(T_)[0]
        elif name == "e2":
            _PROGS[name] = build_e2(NQB_)[0]
        elif name == "o1":
            _PROGS[name] = build_o1(L_, 4)[0]
        elif name == "p":
            _PROGS[name] = build_post(T_, False)[0]
        elif name == "pf":
            _PROGS[name] = build_post(T_, True)[0]
    return _PROGS[name]


def _run(name, in_maps):
    res = run_bass_kernel_spmd(_prog(name), in_maps, core_ids=list(range(NCORES)))
    return res.results


def _rot_cols(w):
    n = w.shape[1] // 64
    w4 = w.reshape(w.shape[0], n, 2, 32)
    return np.ascontiguousarray(w4[:, :, ::-1, :]).reshape(w.shape[0], n * 64)


def _rope_tabs(pos):
    inv = (np.float32(10000.0) ** (-np.arange(0, 64, 2, dtype=np.float32) / np.float32(64))).astype(np.float32)
    ang = (pos.astype(np.float32)[:, None] * inv[None, :]).astype(np.float32)
    c = np.cos(ang).astype(np.float32).T
    s = np.sin(ang).astype(np.float32).T
    return np.ascontiguousarray(np.concatenate([c, c, c, c], 0)), np.ascontiguousarray(np.concatenate([-s, s, -s, s], 0))


def _c(a):
    return np.ascontiguousarray(a)


def kernel(x, mix_norm, mlp_norm, w_ff1, w_ff2, ev_w_in, ev_kv_norm, ev_w_uk, ev_w_uv, ev_pool_w, ev_pool_scale, ev_w_out,
           od_w_in, od_conv_w, od_a_log, od_dt_bias, od_o_norm, od_w_out, final_norm):
    f32 = np.float32
    x = np.asarray(x, f32)
    cores = [(c // 2, c % 2) for c in range(NCORES)]
    hT = [_c(x[b].T) for b in range(B_)]
    res = _run("n0", [{"hT": _c(hT[b][:, hf * T_:(hf + 1) * T_]), "g": _c(np.asarray(mix_norm[0], f32))} for (b, hf) in cores])
    hn = [np.concatenate([res[2 * b]["hn_out"], res[2 * b + 1]["hn_out"]], 1) for b in range(B_)]
    gconst = gdn_consts()
    iota = np.tile(np.arange(256, dtype=f32), (128, 1))
    ident_bf = np.eye(128, dtype=f32).astype(BF)
    out = None
    for layer in range(4):
        j = layer // 2
        if layer % 2 == 0:
            w_in = np.asarray(ev_w_in[j], f32)
            w_rot = _c(np.concatenate([_rot_cols(w_in[:, 0:512]), _rot_cols(w_in[:, 640:1152]), _rot_cols(w_in[:, 1152:1216])], 1))
            w_uk = np.asarray(ev_w_uk[j], f32)
            ims = []
            for (b, hf) in cores:
                pos = hf * T_ + np.arange(T_)
                cosT, sinT = _rope_tabs(pos)
                invc0 = np.zeros((128, 4, 512), f32)
                for g in range(4):
                    invc0[:, g, :] = (1.0 / np.minimum(pos[:512] + 1, 2 ** (g + 1))).astype(f32)
                halo = np.zeros((D, HALO), BF) if hf == 0 else hn[b][:, T_ - HALO:T_]
                ims.append({"hnT": _c(np.concatenate([halo, hn[b][:, hf * T_:(hf + 1) * T_]], 1)), "w_in": _c(w_in), "w_rot": w_rot,
                            "kvn": _c(np.asarray(ev_kv_norm[j], f32)), "w_uk": _c(w_uk), "w_ukr": _rot_cols(w_uk), "w_uv": _c(np.asarray(ev_w_uv[j], f32)),
                            "pool_w": _c(np.asarray(ev_pool_w[j], f32)), "pool_sc": _c(np.asarray(ev_pool_scale[j], f32)),
                            "cosT": cosT, "sinT": sinT, "invc0": invc0})
            r1 = _run("e1", ims)
            cat = lambda k, ax: [np.concatenate([r1[2 * b][k], r1[2 * b + 1][k]], ax) for b in range(B_)]
            qT, qiT, kiT, kT, vv, wiT, ybT = cat("qT", 1), cat("qiT", 1), cat("kiT", 1), cat("kT", 1), cat("v", 0), cat("wiT", 1), cat("ybT", 1)
            ims = []
            qposs = []
            for (b, par) in cores:
                blocks = [2 * i + ((i % 2) ^ par) for i in range(NQB_)]
                qpos = np.concatenate([np.arange(g * 128, (g + 1) * 128) for g in blocks])
                qposs.append(qpos)
                ims.append({"qh": _c(qT[b].reshape(8, 64, L_)[:, :, qpos].transpose(1, 0, 2)),
                            "qih": _c(qiT[b].reshape(8, 64, L_)[:, :, qpos].transpose(1, 0, 2)),
                            "wi_tok": _c(wiT[b][:, qpos].T.reshape(NQB_, 128, 8).transpose(1, 0, 2)),
                            "qrel": _c((qpos.reshape(NQB_, 128) - 256 * np.arange(NQB_)[:, None]).T.astype(f32)),
                            "kiT": _c(kiT[b]), "kT": _c(kT[b]), "v": _c(vv[b].reshape(L_ // 128, 128, 64).transpose(1, 0, 2)),
                            "iota": iota, "ident": ident_bf})
            r2 = _run("e2", ims)
            yT = []
            for b in range(B_):
                ya = np.zeros((L_, 512), BF)
                for par in range(2):
                    ya[qposs[2 * b + par]] = r2[2 * b + par]["ya"].transpose(1, 0, 2).reshape(NQB_ * 128, 512)
                yT.append(np.concatenate([_c(ya.T), ybT[b]], 0))
            w_out = np.asarray(ev_w_out[j], f32)
        else:
            w_in = np.asarray(od_w_in[j], f32)
            conv_w = np.asarray(od_conv_w[j], f32)
            ims = []
            for (b, hg) in cores:
                heads = list(range(hg * 4, hg * 4 + 4))
                cols = lambda base: np.concatenate([np.arange(base + h * 128, base + (h + 1) * 128) for h in heads])
                cw = np.zeros((128, 3, 4, 4), f32)
                for ty in range(3):
                    for hi, h in enumerate(heads):
                        cw[:, ty, hi, :] = conv_w[:, ty * 1024 + h * 128: ty * 1024 + (h + 1) * 128].T
                im = {"hnT": _c(hn[b]), "w_qkv": _c(np.concatenate([w_in[:, cols(0)], w_in[:, cols(1024)], w_in[:, cols(2048)]], 1)),
                      "w_z": _c(w_in[:, cols(3072)]),
                      "w_ba": _c(np.concatenate([w_in[:, [4096 + h for h in heads]], w_in[:, [4104 + h for h in heads]]], 1)),
                      "cw": _c(cw.reshape(128, -1)), "alog_b": _c(np.tile(np.asarray(od_a_log[j], f32)[heads][None, :], (128, 1))),
                      "dtb_b": _c(np.tile(np.asarray(od_dt_bias[j], f32)[heads][None, :], (128, 1))),
                      "onorm_b": _c(np.tile(np.asarray(od_o_norm[j], f32)[None, :], (128, 1)))}
                for n, a in gconst.items():
                    im["c_" + n] = a
                ims.append(im)
            r1 = _run("o1", ims)
            yT = []
            for b in range(B_):
                o = np.concatenate([r1[2 * b + hg]["o_tok"].transpose(1, 0, 2).reshape(L_, 512) for hg in range(2)], 1)
                yT.append(_c(o.T))
            w_out = np.asarray(od_w_out[j], f32)
        final = layer == 3
        g_next = np.asarray(final_norm if final else mix_norm[layer + 1], f32)
        ims = [{"hT": _c(hT[b][:, hf * T_:(hf + 1) * T_]), "yT": _c(yT[b][:, hf * T_:(hf + 1) * T_]), "w_out": _c(w_out),
                "w1": _c(np.asarray(w_ff1[layer], f32)), "w2": _c(np.asarray(w_ff2[layer], f32)),
                "g_mlp": _c(np.asarray(mlp_norm[layer], f32)), "g_next": _c(g_next)} for (b, hf) in cores]
        rp = _run("pf" if final else "p", ims)
        hT = [np.concatenate([rp[2 * b]["hT_out"], rp[2 * b + 1]["hT_out"]], 1) for b in range(B_)]
        hn = [np.concatenate([rp[2 * b]["hn_out"], rp[2 * b + 1]["hn_out"]], 1) for b in range(B_)]
    out = np.stack([_c(hn[b].T) for b in range(B_)], 0).astype(np.float32)
    return out
```

```python
import numpy as np
import ml_dtypes
from contextlib import ExitStack
import concourse.bass as bass
import concourse.mybir as mybir
from concourse.bass_utils import run_bass_kernel_spmd

F32 = mybir.dt.float32
BF16 = mybir.dt.bfloat16
I32 = mybir.dt.int32
AF = mybir.ActivationFunctionType
ALU = mybir.AluOpType
AX = mybir.AxisListType

NCORES = 8
D = 1024
DFF = 4096
EPS = 1e-6


class Trk:
    __slots__ = ("w", "r")

    def __init__(self):
        self.w = None
        self.r = {}


class SemObj:
    __slots__ = ("h", "val")

    def __init__(self, h):
        self.h = h
        self.val = 0


class Eng:
    def __init__(self, kb, name, h):
        self.kb = kb
        self.name = name
        self.h = h
        self.sem = SemObj(kb.es.enter_context(kb.nc.semaphore("s_" + name)))
        self.seen = {}


class KB:
    NDMASEM = 24

    def __init__(self):
        self.nc = bass.Bass("TRN2", target_bir_lowering=False)
        self.es = ExitStack()
        self.E = {n: Eng(self, n, getattr(self.nc, n)) for n in ("tensor", "vector", "scalar", "gpsimd", "sync")}
        self.dsems = [SemObj(self.es.enter_context(self.nc.semaphore("s_dma%d" % i))) for i in range(self.NDMASEM)]
        self.dma_i = 0
        self.out_toks = []
        self.ninstr = 0

    def sbuf(self, name, shape, dt):
        return self.es.enter_context(self.nc.sbuf_tensor(name, list(shape), dt))

    def psum(self, name, shape, dt):
        return self.es.enter_context(self.nc.psum_tensor(name, list(shape), dt))

    def dram(self, name, shape, dt, kind):
        return self.nc.dram_tensor(name, list(shape), dt, kind=kind).ap()

    def _waits(self, e, reads, writes, acc=False):
        need = {}

        def req(tok):
            if tok is None:
                return
            s, v = tok
            if need.get(s, (None, 0))[1] < v:
                need[s] = (s, v)

        for t in reads:
            req(t.w)
        for t in writes:
            if not (acc and t.w is not None and t.w[0] is e.sem):
                req(t.w)
            for r in t.r.items():
                req(r)
        for s, v in need.values():
            if e.seen.get(s, 0) < v:
                e.h.wait_ge(s.h, v)
                e.seen[s] = v
                self.ninstr += 1

    def _post(self, tok, reads, writes):
        for t in reads:
            if t.r.get(tok[0], 0) < tok[1]:
                t.r[tok[0]] = tok[1]
        for t in writes:
            t.w = tok
            t.r = {}

    def op(self, eng, fn, reads=(), writes=(), acc=False):
        e = self.E[eng]
        self._waits(e, reads, writes, acc or eng == "tensor")
        ins = fn(e.h)
        e.sem.val += 1
        ins.then_inc(e.sem.h, 1)
        self.ninstr += 1
        tok = (e.sem, e.sem.val)
        self._post(tok, reads, writes)
        return tok

    def dma(self, eng, out, in_, reads=(), writes=(), is_output=False, **kw):
        e = self.E[eng]
        s = self.dsems[self.dma_i % self.NDMASEM]
        self.dma_i += 1
        if s.val > 0 and e.seen.get(s, 0) < s.val:
            e.h.wait_ge(s.h, s.val)
            e.seen[s] = s.val
        self._waits(e, reads, writes)
        ins = e.h.dma_start(out=out, in_=in_, **kw)
        s.val += 16
        ins.then_inc(s.h, 16)
        self.ninstr += 1
        tok = (s, s.val)
        self._post(tok, reads, writes)
        if is_output:
            self.out_toks.append(tok)
        return tok

    def finish(self):
        e = self.E["sync"]
        for s, v in self.out_toks:
            if e.seen.get(s, 0) < v:
                e.h.wait_ge(s.h, v)
                e.seen[s] = v
        for n, en in self.E.items():
            if en.sem.val > 0 and n != "sync":
                e.h.wait_ge(en.sem.h, en.sem.val)
        self.es.close()
        return self.nc


def load_weight_bf16(kb, name, w_ap, K, N, eng="gpsimd"):
    kc = K // 128
    t = kb.sbuf(name, [128, kc, N], BF16)
    trk = Trk()
    src = w_ap.rearrange("(kc p) n -> p kc n", p=128)
    step = 2048
    for c in range(kc):
        for n0 in range(0, N, step):
            n1 = min(N, n0 + step)
            kb.dma(eng, t[:, c, n0:n1], src[:, c, n0:n1], writes=[trk])
    return t, trk


def load_vec_col(kb, name, v_ap, n):
    c = n // 128
    t = kb.sbuf(name, [128, c], F32)
    trk = Trk()
    with kb.nc.allow_non_contiguous_dma(reason="tiny per-feature vector"):
        kb.dma("sync", t[:, :], v_ap.rearrange("(c p) -> p c", p=128), writes=[trk])
    return t, trk


class PsumPool:
    def __init__(self, kb, n=8):
        self.kb = kb
        self.t = [kb.psum("ps%d" % i, [128, 512], F32) for i in range(n)]
        self.trk = [Trk() for _ in range(n)]
        self.i = 0
        self.n = n

    def get(self):
        i = self.i % self.n
        self.i += 1
        return self.t[i], self.trk[i]


def rmsnorm_fm(kb, pp, x_tiles, x_trks, g_t, g_trk, ones_t, ones_trk, out_tiles, out_trks, scr, nd, TT, Dn):
    ps, ps_trk = pp.get()
    for c in range(nd):
        sq, sq_trk = scr["sq"][c]
        kb.op("scalar", lambda h, c=c, sq=sq: h.activation(out=sq, in_=x_tiles[c], func=AF.Square),
              reads=[x_trks[c]], writes=[sq_trk])
    for c in range(nd):
        sq, sq_trk = scr["sq"][c]
        kb.op("tensor", lambda h, c=c, sq=sq: h.matmul(ps[:, :TT], lhsT=ones_t, rhs=sq, start=(c == 0), stop=(c == nd - 1)),
              reads=[sq_trk, ones_trk], writes=[ps_trk])
    rstd, rstd_trk = scr["rstd"]
    kb.op("scalar", lambda h: h.activation(out=rstd, in_=ps[:, :TT], func=AF.Sqrt, bias=scr["eps"][0], scale=1.0 / Dn),
          reads=[ps_trk, scr["eps"][1]], writes=[rstd_trk])
    kb.op("vector", lambda h: h.reciprocal(out=rstd, in_=rstd), reads=[rstd_trk], writes=[rstd_trk])
    for c in range(nd):
        kb.op("vector", lambda h, c=c: h.scalar_tensor_tensor(out=out_tiles[c], in0=x_tiles[c], scalar=g_t[:, c:c + 1], in1=rstd,
                                                               op0=ALU.mult, op1=ALU.mult),
              reads=[x_trks[c], rstd_trk, g_trk], writes=[out_trks[c]])


def build_post(T, final):
    kb = KB()
    nc = kb.nc
    TT = 256
    NT = T // TT
    hT = kb.dram("hT", [D, T], F32, "ExternalInput")
    yT = kb.dram("yT", [D, T], BF16, "ExternalInput")
    w_out = kb.dram("w_out", [D, D], F32, "ExternalInput")
    w1 = kb.dram("w1", [D, DFF], F32, "ExternalInput")
    w2 = kb.dram("w2", [DFF, D], F32, "ExternalInput")
    g_mlp = kb.dram("g_mlp", [D], F32, "ExternalInput")
    g_next = kb.dram("g_next", [D], F32, "ExternalInput")
    hT_out = kb.dram("hT_out", [D, T], F32, "ExternalOutput")
    if final:
        hn_out = kb.dram("hn_out", [D, T], F32, "ExternalOutput")
    else:
        hn_out = kb.dram("hn_out", [D, T], BF16, "ExternalOutput")

    wo_t, wo_trk = load_weight_bf16(kb, "wo", w_out, D, D)
    gm_t, gm_trk = load_vec_col(kb, "gm", g_mlp, D)
    gn_t, gn_trk = load_vec_col(kb, "gn", g_next, D)
    w1_t, w1_trk = load_weight_bf16(kb, "w1s", w1, D, DFF)
    w2_t, w2_trk = load_weight_bf16(kb, "w2s", w2, DFF, D)

    ones = kb.sbuf("ones", [128, 128], BF16)
    ones_trk = Trk()
    kb.op("vector", lambda h: h.memset(ones[:], 1.0), writes=[ones_trk])
    eps_t = kb.sbuf("eps", [128, 1], F32)
    eps_trk = Trk()
    kb.op("vector", lambda h: h.memset(eps_t[:], EPS), writes=[eps_trk])

    pp = PsumPool(kb)
    NB = 2
    y_s = [kb.sbuf("y%d" % b, [128, 8, TT], BF16) for b in range(NB)]
    y_k = [Trk() for _ in range(NB)]
    h_s = [kb.sbuf("h%d" % b, [128, 8, TT], F32) for b in range(NB)]
    h_k = [[Trk() for _ in range(8)] for _ in range(NB)]
    hload_k = [Trk() for _ in range(NB)]
    xn_s = kb.sbuf("xn", [128, 8, TT], BF16)
    xn_k = [Trk() for _ in range(8)]
    sq_s = kb.sbuf("sq", [128, 8, TT], BF16)
    sq_k = [Trk() for _ in range(8)]
    rstd_s = kb.sbuf("rstd", [128, TT], F32)
    rstd_k = Trk()
    a_s = kb.sbuf("a", [128, 32, TT], BF16)
    a_k = [Trk() for _ in range(32)]
    r_s = [kb.sbuf("r%d" % b, [128, TT], F32) for b in range(4)]
    r_k = [Trk() for _ in range(4)]
    if final:
        hn_s = kb.sbuf("hn", [128, 8, TT], F32)
    else:
        hn_s = kb.sbuf("hn", [128, 8, TT], BF16)
    hn_k = [Trk() for _ in range(8)]

    hT_v = hT.rearrange("(c p) t -> p c t", p=128)
    yT_v = yT.rearrange("(c p) t -> p c t", p=128)
    hTo_v = hT_out.rearrange("(c p) t -> p c t", p=128)
    hno_v = hn_out.rearrange("(c p) t -> p c t", p=128)

    scr = {"sq": [(sq_s[:, c, :], sq_k[c]) for c in range(8)], "rstd": (rstd_s[:, :], rstd_k), "eps": (eps_t[:, 0:1], eps_trk)}

    def load(i):
        b = i % NB
        t0 = i * TT
        kb.dma("sync", y_s[b][:, :, :], yT_v[:, :, t0:t0 + TT], writes=[y_k[b]])
        kb.dma("sync", h_s[b][:, :, :], hT_v[:, :, t0:t0 + TT], writes=h_k[b])

    load(0)
    for i in range(NT):
        b = i % NB
        t0 = i * TT
        if i + 1 < NT:
            load(i + 1)
        for oc in range(8):
            ps, pk = pp.get()
            for kc in range(8):
                kb.op("tensor", lambda h, kc=kc, oc=oc, ps=ps: h.matmul(ps[:, :TT], lhsT=wo_t[:, kc, oc * 128:(oc + 1) * 128], rhs=y_s[b][:, kc, :],
                                                                        start=(kc == 0), stop=(kc == 7)),
                      reads=[wo_trk, y_k[b]], writes=[pk])
            kb.op("vector", lambda h, oc=oc, ps=ps: h.tensor_tensor(out=h_s[b][:, oc, :], in0=h_s[b][:, oc, :], in1=ps[:, :TT], op=ALU.add),
                  reads=[pk], writes=[h_k[b][oc]])
        rmsnorm_fm(kb, pp, [h_s[b][:, c, :] for c in range(8)], h_k[b], gm_t, gm_trk, ones[:, :], ones_trk,
                   [xn_s[:, c, :] for c in range(8)], xn_k, scr, 8, TT, D)
        for fc in range(32):
            ps, pk = pp.get()
            for kc in range(8):
                kb.op("tensor", lambda h, kc=kc, fc=fc, ps=ps: h.matmul(ps[:, :TT], lhsT=w1_t[:, kc, fc * 128:(fc + 1) * 128], rhs=xn_s[:, kc, :],
                                                                        start=(kc == 0), stop=(kc == 7)),
                      reads=[w1_trk, xn_k[kc]], writes=[pk])
            rb = fc % 4
            kb.op("scalar", lambda h, ps=ps, rb=rb: h.activation(out=r_s[rb][:, :], in_=ps[:, :TT], func=AF.Relu),
                  reads=[pk], writes=[r_k[rb]])
            kb.op("gpsimd", lambda h, fc=fc, rb=rb: h.tensor_tensor(out=a_s[:, fc, :], in0=r_s[rb][:, :], in1=r_s[rb][:, :], op=ALU.mult),
                  reads=[r_k[rb]], writes=[a_k[fc]])
        for oc in range(8):
            ps, pk = pp.get()
            for fc in range(32):
                kb.op("tensor", lambda h, fc=fc, oc=oc, ps=ps: h.matmul(ps[:, :TT], lhsT=w2_t[:, fc, oc * 128:(oc + 1) * 128], rhs=a_s[:, fc, :],
                                                                        start=(fc == 0), stop=(fc == 31)),
                      reads=[w2_trk, a_k[fc]], writes=[pk])
            kb.op("vector", lambda h, oc=oc, ps=ps: h.tensor_tensor(out=h_s[b][:, oc, :], in0=h_s[b][:, oc, :], in1=ps[:, :TT], op=ALU.add),
                  reads=[pk], writes=[h_k[b][oc]])
        kb.dma("sync", hTo_v[:, :, t0:t0 + TT], h_s[b][:, :, :], reads=h_k[b], is_output=True)
        rmsnorm_fm(kb, pp, [h_s[b][:, c, :] for c in range(8)], h_k[b], gn_t, gn_trk, ones[:, :], ones_trk,
                   [hn_s[:, c, :] for c in range(8)], hn_k, scr, 8, TT, D)
        kb.dma("sync", hno_v[:, :, t0:t0 + TT], hn_s[:, :, :], reads=hn_k, is_output=True)
    return kb.finish(), kb


EV_IN = 1736
C_Q, C_KV, C_QI, C_KI, C_WI, C_U = 0, 512, 640, 1152, 1216, 1224
HALO = 16


def build_e1(T):
    kb = KB()
    nc = kb.nc
    TT = 512
    NT = T // TT
    W = TT + HALO
    hnT = kb.dram("hnT", [D, HALO + T], BF16, "ExternalInput")
    w_in = kb.dram("w_in", [D, EV_IN], F32, "ExternalInput")
    w_rot = kb.dram("w_rot", [D, 1088], F32, "ExternalInput")
    kvn = kb.dram("kvn", [128], F32, "ExternalInput")
    w_uk = kb.dram("w_uk", [128, 64], F32, "ExternalInput")
    w_ukr = kb.dram("w_ukr", [128, 64], F32, "ExternalInput")
    w_uv = kb.dram("w_uv", [128, 64], F32, "ExternalInput")
    pool_w = kb.dram("pool_w", [4, 128, 128], F32, "ExternalInput")
    pool_sc = kb.dram("pool_sc", [512], F32, "ExternalInput")
    cosT = kb.dram("cosT", [128, T], F32, "ExternalInput")
    sinT = kb.dram("sinT", [128, T], F32, "ExternalInput")
    invc0 = kb.dram("invc0", [128, 4, TT], F32, "ExternalInput")
    qT_o = kb.dram("qT", [512, T], BF16, "ExternalOutput")
    qiT_o = kb.dram("qiT", [512, T], BF16, "ExternalOutput")
    kiT_o = kb.dram("kiT", [64, T], BF16, "ExternalOutput")
    kT_o = kb.dram("kT", [64, T], BF16, "ExternalOutput")
    v_o = kb.dram("v", [T, 64], BF16, "ExternalOutput")
    wiT_o = kb.dram("wiT", [8, T], F32, "ExternalOutput")
    ybT_o = kb.dram("ybT", [512, T], BF16, "ExternalOutput")

    win_t, win_k = load_weight_bf16(kb, "win", w_in, D, EV_IN)
    wrot_t, wrot_k = load_weight_bf16(kb, "wrot", w_rot, D, 1088)
    wuk_t, wuk_k = load_weight_bf16(kb, "wuk", w_uk, 128, 64)
    wukr_t, wukr_k = load_weight_bf16(kb, "wukr", w_ukr, 128, 64)
    wuv_t, wuv_k = load_weight_bf16(kb, "wuv", w_uv, 128, 64)
    pw_t = kb.sbuf("pw", [128, 4, 128], BF16)
    pw_k = Trk()
    kb.dma("gpsimd", pw_t[:, :, :], pool_w.rearrange("g c d -> c g d"), writes=[pw_k])
    psc_t, psc_k = load_vec_col(kb, "psc", pool_sc, 512)
    kvn_t, kvn_k = load_vec_col(kb, "kvn_s", kvn, 128)
    invc0_t = kb.sbuf("invc0_s", [128, 4, TT], F32)
    invc0_k = Trk()
    kb.dma("sync", invc0_t[:, :, :], invc0[:, :, :], writes=[invc0_k])
    invc_t = kb.sbuf("invc_s", [128, 4, TT], F32)
    invc_k = Trk()
    for g in range(4):
        kb.op("vector", lambda h, g=g: h.memset(invc_t[:, g, :], 1.0 / (2 ** (g + 1))), writes=[invc_k])
    ones = kb.sbuf("ones", [128, 128], BF16)
    ones_trk = Trk()
    kb.op("vector", lambda h: h.memset(ones[:], 1.0), writes=[ones_trk])
    eps_t = kb.sbuf("eps", [128, 1], F32)
    eps_trk = Trk()
    kb.op("vector", lambda h: h.memset(eps_t[:], EPS), writes=[eps_trk])

    pp = PsumPool(kb)
    NB = 2
    hn_s = [kb.sbuf("hn%d" % b, [128, 8, TT], BF16) for b in range(NB)]
    hn_k = [Trk() for _ in range(NB)]
    halo_s = kb.sbuf("halo", [128, 8, HALO], BF16)
    halo_k = Trk()
    cs_s = [kb.sbuf("cs%d" % b, [128, 2, TT], F32) for b in range(NB)]
    cs_k = [Trk() for _ in range(NB)]
    t1_s = [kb.sbuf("t1_%d" % b, [128, TT], F32) for b in range(2)]
    t1_k = [Trk() for _ in range(2)]
    t2_s = [kb.sbuf("t2_%d" % b, [128, TT], F32) for b in range(2)]
    t2_k = [Trk() for _ in range(2)]
    q_s = kb.sbuf("q_s", [128, 4, TT], BF16)
    q_k = Trk()
    qi_s = kb.sbuf("qi_s", [128, 4, TT], BF16)
    qi_k = Trk()
    ki_s = kb.sbuf("ki_s", [64, TT], BF16)
    ki_k = Trk()
    k_s = kb.sbuf("k_s", [64, TT], BF16)
    k_k = Trk()
    v_s = kb.sbuf("v_s", [128, 4, 64], BF16)
    v_k = Trk()
    wi_s = kb.sbuf("wi_s", [8, TT], F32)
    wi_k = Trk()
    ckv_s = kb.sbuf("ckv_s", [128, TT], F32)
    ckv_k = Trk()
    ckvn_s = kb.sbuf("ckvn_s", [128, TT], BF16)
    ckvn_k = Trk()
    sq_s = kb.sbuf("sq", [128, TT], BF16)
    sq_k = Trk()
    rstd_s = kb.sbuf("rstd", [128, TT], F32)
    rstd_k = Trk()
    u_s = [kb.sbuf("u%d" % g, [128, W], F32) for g in range(4)]
    u_k = [Trk() for _ in range(4)]
    sA = kb.sbuf("sA", [128, W], F32)
    sA_k = Trk()
    sB = kb.sbuf("sB", [128, W], F32)
    sB_k = Trk()
    pl_s = [kb.sbuf("pl%d" % g, [128, TT], BF16) for g in range(4)]
    pl_k = [Trk() for _ in range(4)]
    yb_s = kb.sbuf("yb_s", [128, 4, TT], BF16)
    yb_k = Trk()
    scr = {"sq": [(sq_s[:, :], sq_k)], "rstd": (rstd_s[:, :], rstd_k), "eps": (eps_t[:, 0:1], eps_trk)}

    hn_v = hnT.rearrange("(c p) t -> p c t", p=128)
    rope_i = [0]

    def load(i):
        b = i % NB
        t0 = i * TT
        kb.dma("sync", hn_s[b][:, :, :], hn_v[:, :, HALO + t0:HALO + t0 + TT], writes=[hn_k[b]])
        kb.dma("sync", cs_s[b][:, 0, :], cosT[:, t0:t0 + TT], writes=[cs_k[b]])
        kb.dma("sync", cs_s[b][:, 1, :], sinT[:, t0:t0 + TT], writes=[cs_k[b]])

    def proj(ps, pk, wt, wk, c0, c1, rhs_fn, rk, N):
        M = c1 - c0
        for kc in range(8):
            kb.op("tensor", lambda h, kc=kc: h.matmul(ps[:M, :N], lhsT=wt[:, kc, c0:c1], rhs=rhs_fn(kc), start=(kc == 0), stop=(kc == 7)),
                  reads=[wk, rk], writes=[pk])

    def rope(b, psa, pka, psb, pkb, M, out_ap, out_k):
        j = rope_i[0] % 2
        rope_i[0] += 1
        kb.op("vector", lambda h: h.tensor_tensor(out=t1_s[j][:M, :], in0=psa[:M, :TT], in1=cs_s[b][:M, 0, :], op=ALU.mult),
              reads=[pka, cs_k[b]], writes=[t1_k[j]])
        kb.op("vector", lambda h: h.tensor_tensor(out=t2_s[j][:M, :], in0=psb[:M, :TT], in1=cs_s[b][:M, 1, :], op=ALU.mult),
              reads=[pkb, cs_k[b]], writes=[t2_k[j]])
        kb.op("gpsimd", lambda h: h.tensor_tensor(out=out_ap, in0=t1_s[j][:M, :], in1=t2_s[j][:M, :], op=ALU.add),
              reads=[t1_k[j], t2_k[j]], writes=[out_k])

    kb.dma("sync", halo_s[:, :, :], hn_v[:, :, 0:HALO], writes=[halo_k])
    load(0)
    for i in range(NT):
        b = i % NB
        t0 = i * TT
        if i + 1 < NT:
            load(i + 1)
        rhs_fn = lambda kc, b=b: hn_s[b][:, kc, :]
        for (cbase, rbase, dst, dk) in ((C_Q, 0, q_s, q_k), (C_QI, 512, qi_s, qi_k)):
            for c in range(4):
                psa, pka = pp.get()
                proj(psa, pka, win_t, win_k, cbase + c * 128, cbase + (c + 1) * 128, rhs_fn, hn_k[b], TT)
                psb, pkb = pp.get()
                proj(psb, pkb, wrot_t, wrot_k, rbase + c * 128, rbase + (c + 1) * 128, rhs_fn, hn_k[b], TT)
                rope(b, psa, pka, psb, pkb, 128, dst[:, c, :], dk)
        kb.dma("sync", qT_o.rearrange("(c p) t -> p c t", p=128)[:, :, t0:t0 + TT], q_s[:, :, :], reads=[q_k], is_output=True)
        kb.dma("sync", qiT_o.rearrange("(c p) t -> p c t", p=128)[:, :, t0:t0 + TT], qi_s[:, :, :], reads=[qi_k], is_output=True)
        psa, pka = pp.get()
        proj(psa, pka, win_t, win_k, C_KI, C_KI + 64, rhs_fn, hn_k[b], TT)
        psb, pkb = pp.get()
        proj(psb, pkb, wrot_t, wrot_k, 1024, 1088, rhs_fn, hn_k[b], TT)
        rope(b, psa, pka, psb, pkb, 64, ki_s[:, :], ki_k)
        kb.dma("sync", kiT_o[:, t0:t0 + TT], ki_s[:, :], reads=[ki_k], is_output=True)
        ps, pk = pp.get()
        proj(ps, pk, win_t, win_k, C_WI, C_WI + 8, rhs_fn, hn_k[b], TT)
        kb.op("scalar", lambda h, ps=ps: h.activation(out=wi_s[:, :], in_=ps[:8, :TT], func=AF.Copy, scale=float(8 ** -0.5 * 64 ** -0.5)),
              reads=[pk], writes=[wi_k])
        kb.dma("sync", wiT_o[:, t0:t0 + TT], wi_s[:, :], reads=[wi_k], is_output=True)
        ps, pk = pp.get()
        proj(ps, pk, win_t, win_k, C_KV, C_KV + 128, rhs_fn, hn_k[b], TT)
        kb.op("scalar", lambda h, ps=ps: h.activation(out=ckv_s[:, :], in_=ps[:, :TT], func=AF.Copy), reads=[pk], writes=[ckv_k])
        rmsnorm_fm(kb, pp, [ckv_s[:, :]], [ckv_k], kvn_t, kvn_k, ones[:, :], ones_trk, [ckvn_s[:, :]], [ckvn_k], scr, 1, TT, 128)
        psa, pka = pp.get()
        kb.op("tensor", lambda h, psa=psa: h.matmul(psa[:64, :TT], lhsT=wuk_t[:, 0, :], rhs=ckvn_s[:, :], start=True, stop=True),
              reads=[wuk_k, ckvn_k], writes=[pka])
        psb, pkb = pp.get()
        kb.op("tensor", lambda h, psb=psb: h.matmul(psb[:64, :TT], lhsT=wukr_t[:, 0, :], rhs=ckvn_s[:, :], start=True, stop=True),
              reads=[wukr_k, ckvn_k], writes=[pkb])
        rope(b, psa, pka, psb, pkb, 64, k_s[:, :], k_k)
        kb.dma("sync", kT_o[:, t0:t0 + TT], k_s[:, :], reads=[k_k], is_output=True)
        ps, pk = pp.get()
        for j in range(4):
            kb.op("tensor", lambda h, ps=ps, j=j: h.matmul(ps[:, j * 64:(j + 1) * 64], lhsT=ckvn_s[:, j * 128:(j + 1) * 128], rhs=wuv_t[:, 0, :],
                                                           start=True, stop=True),
                  reads=[wuv_k, ckvn_k], writes=[pk])
        kb.op("scalar", lambda h, ps=ps: h.activation(out=v_s[:, :, :], in_=ps[:, 0:256].rearrange("p (j d) -> p j d", d=64), func=AF.Copy),
              reads=[pk], writes=[v_k])
        kb.dma("sync", v_o[t0:t0 + TT, :].rearrange("(j p) d -> p j d", p=128), v_s[:, :, :], reads=[v_k], is_output=True)
        for g in range(4):
            w = 2 ** (g + 1)
            if i == 0:
                ps, pk = pp.get()
                proj(ps, pk, win_t, win_k, C_U + g * 128, C_U + (g + 1) * 128, lambda kc: halo_s[:, kc, :], halo_k, HALO)
                kb.op("scalar", lambda h, ps=ps, g=g: h.activation(out=u_s[g][:, 0:HALO], in_=ps[:, :HALO], func=AF.Copy),
                      reads=[pk], writes=[u_k[g]])
            else:
                kb.op("gpsimd", lambda h, g=g: h.tensor_copy(out=u_s[g][:, 0:HALO], in_=u_s[g][:, TT:TT + HALO]),
                      reads=[u_k[g]], writes=[u_k[g]])
            ps, pk = pp.get()
            proj(ps, pk, win_t, win_k, C_U + g * 128, C_U + (g + 1) * 128, rhs_fn, hn_k[b], TT)
            kb.op("scalar", lambda h, ps=ps, g=g: h.activation(out=u_s[g][:, HALO:W], in_=ps[:, :TT], func=AF.Copy),
                  reads=[pk], writes=[u_k[g]])
            src, srck = u_s[g], u_k[g]
            bufs = [(sA, sA_k), (sB, sB_k)]
            sh = 1
            lo = 0
            for step in range(g + 1):
                dst, dstk = bufs[step % 2]
                lo = lo + sh
                kb.op("gpsimd", lambda h, src=src, dst=dst, lo=lo, sh=sh: h.tensor_tensor(out=dst[:, lo:W], in0=src[:, lo:W], in1=src[:, lo - sh:W - sh], op=ALU.add),
                      reads=[srck], writes=[dstk])
                src, srck = dst, dstk
                sh *= 2
            tab, tabk = (invc0_t, invc0_k) if i == 0 else (invc_t, invc_k)
            j = rope_i[0] % 2
            rope_i[0] += 1
            kb.op("gpsimd", lambda h, src=src, tab=tab, g=g, j=j: h.tensor_tensor(out=t1_s[j][:, :], in0=src[:, HALO:W], in1=tab[:, g, :], op=ALU.mult),
                  reads=[srck, tabk], writes=[t1_k[j]])
            kb.op("gpsimd", lambda h, g=g, j=j: h.tensor_tensor(out=pl_s[g][:, :], in0=t1_s[j][:, :], in1=u_s[g][:, HALO:W], op=ALU.subtract),
                  reads=[t1_k[j], u_k[g]], writes=[pl_k[g]])
            ps, pk = pp.get()
            kb.op("tensor", lambda h, ps=ps, g=g: h.matmul(ps[:, :TT], lhsT=pw_t[:, g, :], rhs=pl_s[g][:, :], start=True, stop=True),
                  reads=[pw_k, pl_k[g]], writes=[pk])
            kb.op("scalar", lambda h, ps=ps, g=g: h.activation(out=yb_s[:, g, :], in_=ps[:, :TT], func=AF.Copy, scale=psc_t[:, g:g + 1]),
                  reads=[pk, psc_k], writes=[yb_k])
        kb.dma("sync", ybT_o.rearrange("(c p) t -> p c t", p=128)[:, :, t0:t0 + TT], yb_s[:, :, :], reads=[yb_k], is_output=True)
    return kb.finish(), kb


TOPK = 256
NEG = -1.0e30


def build_e2(NQB, NIT=20, blk_list=None):
    kb = KB()
    nc = kb.nc
    NQ = NQB * 128
    nmax = 2 * NQB
    L = nmax * 128
    qh = kb.dram("qh", [64, 8, NQ], BF16, "ExternalInput")
    qih = kb.dram("qih", [64, 8, NQ], BF16, "ExternalInput")
    wi_tok = kb.dram("wi_tok", [128, NQB, 8], F32, "ExternalInput")
    qrel = kb.dram("qrel", [128, NQB], F32, "ExternalInput")
    kiT = kb.dram("kiT", [64, L], BF16, "ExternalInput")
    kT = kb.dram("kT", [64, L], BF16, "ExternalInput")
    vv = kb.dram("v", [128, nmax, 64], BF16, "ExternalInput")
    iota_in = kb.dram("iota", [128, 256], F32, "ExternalInput")
    ident_in = kb.dram("ident", [128, 128], BF16, "ExternalInput")
    ya_o = kb.dram("ya", [128, NQB, 512], BF16, "ExternalOutput")

    def ld(name, ap, shape, dt):
        t = kb.sbuf(name, shape, dt)
        k = Trk()
        kb.dma("sync", t[tuple(slice(None) for _ in shape)], ap, writes=[k])
        return t, k

    ki_t, ki_k = ld("ki_t", kiT[:, :], [64, L], BF16)
    k_t, k_k = ld("k_t", kT[:, :], [64, L], BF16)
    v_t, v_k = ld("v_t", vv[:, :, :], [128, nmax, 64], BF16)
    wi_t, wi_k = ld("wi_t", wi_tok[:, :, :], [128, NQB, 8], F32)
    qr_t, qr_k = ld("qr_t", qrel[:, :], [128, NQB], F32)
    io_t, io_k = ld("io_t", iota_in[:, :], [128, 256], F32)
    id_t, id_k = ld("id_t", ident_in[:, :], [128, 128], BF16)

    ps_f = [kb.psum("psf%d" % i, [128, 512], F32) for i in range(5)]
    ps_fk = [Trk() for _ in range(5)]
    ps_i = [0]

    def getps():
        j = ps_i[0] % 5
        ps_i[0] += 1
        return ps_f[j], ps_fk[j]

    ps_t = [kb.psum("pst%d" % i, [128, 1024], BF16) for i in range(2)]
    ps_tk = [Trk() for _ in range(2)]
    ps_o = kb.psum("pso", [128, 512], F32)
    ps_ok = Trk()

    q_s = [kb.sbuf("q_s%d" % b, [64, 8, 128], BF16) for b in range(2)]
    q_k = [Trk() for _ in range(2)]
    qi_s = [kb.sbuf("qi_s%d" % b, [64, 8, 128], BF16) for b in range(2)]
    qi_k = [Trk() for _ in range(2)]
    score = kb.sbuf("score", [128, L], F32)
    score_k = Trk()
    lg = kb.sbuf("lg", [128, L], F32)
    lg_k = Trk()
    P_sb = [kb.sbuf("P_s%d" % b_, [128, L], BF16) for b_ in range(2)]
    P_kb = [Trk() for _ in range(2)]
    PT_sb = [kb.sbuf("PT_s%d" % b_, [128, nmax, 128], BF16) for b_ in range(2)]
    PT_kb = [Trk() for _ in range(2)]
    junk, junk_k = P_sb[1], P_kb[1]
    r_s = [kb.sbuf("r%d" % b, [128, 512], F32) for b in range(4)]
    r_k = [Trk() for _ in range(4)]
    mb_s = kb.sbuf("mb", [128, 256], F32)
    mb_k = Trk()
    sm = kb.sbuf("sm", [128, 16], F32)
    lo_k, w0_k, mid_k, cnt_k, tmp_k, hi_k, negm_k, rinv_k = (Trk() for _ in range(8))
    LO, W0, MID, CNT, TMP, HI, NEGM, RINV = (sm[:, j:j + 1] for j in range(8))
    rs = kb.sbuf("rs", [128, 8], F32)
    rs_k = Trk()
    ya_s = [kb.sbuf("ya_s%d" % b, [128, 512], BF16) for b in range(2)]
    ya_k = [Trk() for _ in range(2)]

    def loadq(i):
        b = i % 2
        kb.dma("sync", q_s[b][:, :, :], qh[:, :, i * 128:(i + 1) * 128], writes=[q_k[b]])
        kb.dma("sync", qi_s[b][:, :, :], qih[:, :, i * 128:(i + 1) * 128], writes=[qi_k[b]])

    blocks = list(range(NQB)) if blk_list is None else blk_list
    loadq(blocks[0])
    rb = 0
    for bi, i in enumerate(blocks):
        b = i % 2
        if bi + 1 < len(blocks):
            loadq(blocks[bi + 1])
        n = 2 * i + 2
        nk = 128 * n
        tiles = [(k0, min(512, nk - k0)) for k0 in range(0, nk, 512)]
        for (k0, kw) in tiles:
            for h in range(8):
                ps, pk = getps()
                kb.op("tensor", lambda e, ps=ps, h=h, k0=k0, kw=kw: e.matmul(ps[:, :kw], lhsT=qi_s[b][:, h, :], rhs=ki_t[:, k0:k0 + kw], start=True, stop=True),
                      reads=[qi_k[b], ki_k], writes=[pk])
                j = rb % 4
                rb += 1
                kb.op("scalar", lambda e, ps=ps, j=j, kw=kw: e.activation(out=r_s[j][:, :kw], in_=ps[:, :kw], func=AF.Relu), reads=[pk], writes=[r_k[j]])
                if h == 0:
                    kb.op("vector", lambda e, j=j, k0=k0, kw=kw: e.tensor_scalar(out=score[:, k0:k0 + kw], in0=r_s[j][:, :kw], scalar1=wi_t[:, i, 0:1], scalar2=None, op0=ALU.mult),
                          reads=[r_k[j], wi_k], writes=[score_k])
                else:
                    kb.op("vector", lambda e, j=j, k0=k0, kw=kw, h=h: e.scalar_tensor_tensor(out=score[:, k0:k0 + kw], in0=r_s[j][:, :kw], scalar=wi_t[:, i, h:h + 1],
                                                                                              in1=score[:, k0:k0 + kw], op0=ALU.mult, op1=ALU.add),
                          reads=[r_k[j], wi_k], writes=[score_k])
        kb.op("vector", lambda e: e.tensor_reduce(out=LO, in_=score[:, :nk], axis=AX.X, op=ALU.min), reads=[score_k], writes=[lo_k])
        kb.op("vector", lambda e: e.tensor_scalar(out=mb_s[:, :], in0=io_t[:, :], scalar1=qr_t[:, i:i + 1], scalar2=NEG, op0=ALU.is_gt, op1=ALU.mult),
              reads=[io_k, qr_k], writes=[mb_k])
        kb.op("gpsimd", lambda e: e.tensor_tensor(out=score[:, nk - 256:nk], in0=score[:, nk - 256:nk], in1=mb_s[:, :], op=ALU.add),
              reads=[mb_k, lo_k], writes=[score_k])
        kb.op("vector", lambda e: e.tensor_reduce(out=HI, in_=score[:, :nk], axis=AX.X, op=ALU.max), reads=[score_k], writes=[hi_k])
        kb.op("vector", lambda e: e.tensor_tensor(out=W0, in0=HI, in1=LO, op=ALU.subtract), reads=[hi_k, lo_k], writes=[w0_k])
        thr = float(2 * TOPK - nk) - 0.5
        for it in range(NIT):
            f = 2.0 ** -(it + 1)
            kb.op("vector", lambda e, f=f: e.scalar_tensor_tensor(out=MID, in0=W0, scalar=-f, in1=LO, op0=ALU.mult, op1=ALU.subtract),
                  reads=[w0_k, lo_k], writes=[mid_k])
            kb.op("scalar", lambda e: e.activation(out=junk[:, :nk], in_=score[:, :nk], func=AF.Sign, bias=MID, scale=1.0, accum_out=CNT),
                  reads=[score_k, mid_k], writes=[junk_k, cnt_k])
            kb.op("vector", lambda e, thr=thr: e.tensor_scalar(out=TMP, in0=CNT, scalar1=thr, scalar2=W0, op0=ALU.is_ge, op1=ALU.mult),
                  reads=[cnt_k, w0_k], writes=[tmp_k])
            kb.op("vector", lambda e, f=f: e.scalar_tensor_tensor(out=LO, in0=TMP, scalar=f, in1=LO, op0=ALU.mult, op1=ALU.add),
                  reads=[tmp_k], writes=[lo_k])
        kb.op("vector", lambda e: e.tensor_scalar(out=score[:, :nk], in0=score[:, :nk], scalar1=LO, scalar2=NEG, op0=ALU.is_lt, op1=ALU.mult),
              reads=[lo_k], writes=[score_k])
        yb_ = bi % 2
        for h in range(8):
            P_s, P_k, PT_s, PT_k = P_sb[h % 2], P_kb[h % 2], PT_sb[h % 2], PT_kb[h % 2]
            for (k0, kw) in tiles:
                ps, pk = getps()
                kb.op("tensor", lambda e, ps=ps, h=h, k0=k0, kw=kw: e.matmul(ps[:, :kw], lhsT=q_s[b][:, h, :], rhs=k_t[:, k0:k0 + kw], start=True, stop=True),
                      reads=[q_k[b], k_k], writes=[pk])
                kb.op("vector", lambda e, ps=ps, k0=k0, kw=kw: e.scalar_tensor_tensor(out=lg[:, k0:k0 + kw], in0=ps[:, :kw], scalar=0.125, in1=score[:, k0:k0 + kw],
                                                                                       op0=ALU.mult, op1=ALU.add),
                      reads=[pk, score_k], writes=[lg_k])
            kb.op("vector", lambda e: e.tensor_reduce(out=NEGM, in_=lg[:, :nk], axis=AX.X, op=ALU.max, negate=True), reads=[lg_k], writes=[negm_k])
            kb.op("scalar", lambda e, h=h: e.activation(out=P_s[:, :nk], in_=lg[:, :nk], func=AF.Exp, bias=NEGM, scale=1.0, accum_out=rs[:, h:h + 1]),
                  reads=[lg_k, negm_k], writes=[P_k, rs_k])
            for j0 in range(0, n, 8):
                jn = min(8, n - j0)
                tb = (j0 // 8) % 2
                for j in range(j0, j0 + jn):
                    s = j - j0
                    kb.op("tensor", lambda e, tb=tb, s=s, j=j: e.transpose(out=ps_t[tb][:, s * 128:(s + 1) * 128], in_=P_s[:, j * 128:(j + 1) * 128], identity=id_t[:, :]),
                          reads=[P_k, id_k], writes=[ps_tk[tb]])
                kb.op("scalar", lambda e, tb=tb, j0=j0, jn=jn: e.activation(out=PT_s[:, j0:j0 + jn, :], in_=ps_t[tb][:, 0:jn * 128].rearrange("p (j q) -> p j q", q=128), func=AF.Copy),
                      reads=[ps_tk[tb]], writes=[PT_k])
            for j in range(n):
                kb.op("tensor", lambda e, j=j, h=h: e.matmul(ps_o[:, h * 64:(h + 1) * 64], lhsT=PT_s[:, j, :], rhs=v_t[:, j, :], start=(j == 0), stop=(j == n - 1)),
                      reads=[PT_k, v_k], writes=[ps_ok])
        kb.op("vector", lambda e: e.reciprocal(out=rs[:, :], in_=rs[:, :]), reads=[rs_k], writes=[rs_k])
        for h in range(8):
            kb.op("scalar", lambda e, h=h: e.activation(out=ya_s[yb_][:, h * 64:(h + 1) * 64], in_=ps_o[:, h * 64:(h + 1) * 64], func=AF.Copy, scale=rs[:, h:h + 1]),
                  reads=[ps_ok, rs_k], writes=[ya_k[yb_]])
        kb.dma("sync", ya_o[:, i, :], ya_s[yb_][:, :], reads=[ya_k[yb_]], is_output=True)
    return kb.finish(), kb


def gdn_consts():
    i = np.arange(128)
    same = (i[:, None] // 64) == (i[None, :] // 64)
    c = {}
    c["ident"] = np.eye(128, dtype=np.float32)
    c["ones"] = np.ones((128, 128), np.float32)
    c["tri"] = (same & (i[:, None] <= i[None, :])).astype(np.float32)
    c["selblk"] = (i[:, None] == (i[None, :] // 64) * 64 + 63).astype(np.float32)
    c["selA"] = np.repeat((i[:, None] == 63), 128, 1).astype(np.float32)
    c["selB"] = np.repeat((i[:, None] == 127), 128, 1).astype(np.float32)
    c["mstrict"] = np.where(same & (i[:, None] > i[None, :]), 0.0, NEG).astype(np.float32)
    c["mTfull"] = np.where(same & (i[None, :] >= i[:, None]), 0.0, NEG).astype(np.float32)
    c["sT01"] = (same & (i[None, :] > i[:, None])).astype(np.float32)
    return c


GDN_CONST_NAMES = ["ident", "ones", "tri", "selblk", "selA", "selB", "mstrict", "mTfull", "sT01"]


def build_o1(L, NH=4):
    kb = KB()
    nc = kb.nc
    TT = 512
    NT = L // TT
    HW = NH * 128
    hnT = kb.dram("hnT", [D, L], BF16, "ExternalInput")
    w_qkv = kb.dram("w_qkv", [D, 3 * HW], F32, "ExternalInput")
    w_z = kb.dram("w_z", [D, HW], F32, "ExternalInput")
    w_ba = kb.dram("w_ba", [D, 2 * NH], F32, "ExternalInput")
    cw_in = kb.dram("cw", [128, 3 * NH * 4], F32, "ExternalInput")
    alog_in = kb.dram("alog_b", [128, NH], F32, "ExternalInput")
    dtb_in = kb.dram("dtb_b", [128, NH], F32, "ExternalInput")
    onorm_in = kb.dram("onorm_b", [128, 128], F32, "ExternalInput")
    cst_in = {n: kb.dram("c_" + n, [128, 128], F32, "ExternalInput") for n in GDN_CONST_NAMES}
    o_out = kb.dram("o_tok", [128, L // 128, HW], BF16, "ExternalOutput")

    def ld(name, ap, shape, dt):
        t = kb.sbuf(name, shape, dt)
        k = Trk()
        kb.dma("sync", t[tuple(slice(None) for _ in shape)], ap, writes=[k])
        return t, k

    wqkv_t, wqkv_k = load_weight_bf16(kb, "wqkv", w_qkv, D, 3 * HW)
    wz_t, wz_k = load_weight_bf16(kb, "wz", w_z, D, HW)
    wba_t, wba_k = load_weight_bf16(kb, "wba", w_ba, D, 2 * NH)
    cw_t, cw_k = ld("cw_t", cw_in[:, :], [128, 3 * NH * 4], F32)
    alog_t, alog_k = ld("alog_t", alog_in[:, :], [128, NH], F32)
    dtb_t, dtb_k = ld("dtb_t", dtb_in[:, :], [128, NH], F32)
    onorm_t, onorm_k = ld("onorm_t", onorm_in[:, :], [128, 128], F32)
    C = {}
    CK = {}
    for n in GDN_CONST_NAMES:
        C[n], CK[n] = ld("cs_" + n, cst_in[n][:, :], [128, 128], F32)
    ones_bf = kb.sbuf("ones_bf", [128, 128], BF16)
    ones_bfk = Trk()
    kb.op("vector", lambda e: e.memset(ones_bf[:], 1.0), writes=[ones_bfk])
    cst = kb.sbuf("cst", [128, 2], F32)
    cst_k = Trk()
    kb.op("vector", lambda e: e.memset(cst[:, 0:1], EPS), writes=[cst_k])
    kb.op("vector", lambda e: e.memset(cst[:, 1:2], 1.0), writes=[cst_k])
    EPSC, ONEC = cst[:, 0:1], cst[:, 1:2]
    ea_t = kb.sbuf("ea_t", [128, NH], F32)
    ea_k = Trk()
    kb.op("scalar", lambda e: e.activation(out=ea_t[:, :], in_=alog_t[:, :], func=AF.Exp), reads=[alog_k], writes=[ea_k])

    pp = PsumPool(kb)
    hn_s = [kb.sbuf("hn%d" % b, [128, 8, TT], BF16) for b in range(2)]
    hn_k = [Trk() for _ in range(2)]
    XW = TT + 3
    x_ext = [[kb.sbuf("x%d_%d" % (ty, h), [128, XW], F32) for h in range(NH)] for ty in range(3)]
    x_k = [[Trk() for _ in range(NH)] for _ in range(3)]
    xc = [[kb.sbuf("xc%d_%d" % (ty, h), [128, TT], F32) for h in range(NH)] for ty in range(3)]
    xc_k = [[Trk() for _ in range(NH)] for _ in range(3)]
    acc_s = [kb.sbuf("acc%d" % b, [128, TT], F32) for b in range(2)]
    acc_k = [Trk() for _ in range(2)]
    sq_s = kb.sbuf("sq", [128, TT], BF16)
    sq_k = Trk()
    rn_s = kb.sbuf("rn", [128, TT], F32)
    rn_k = Trk()
    gz_s = [kb.sbuf("gz%d" % j, [128, HW], F32) for j in range(4)]
    gz_k = [Trk() for _ in range(4)]
    NS = 10
    sc_s = [kb.sbuf("sc%d" % j, [128, NS, NH], F32) for j in range(4)]
    sc_k = [[Trk() for _ in range(NS)] for _ in range(4)]
    BETA, GC, EG, EDL, EGLA, EGLB, BEG, GG, T0, T1 = range(NS)
    S_s = [kb.sbuf("S%d" % h, [128, 128], F32) for h in range(NH)]
    S_k = [Trk() for _ in range(NH)]
    for h in range(NH):
        kb.op("gpsimd", lambda e, h=h: e.memset(S_s[h][:, :], 0.0), writes=[S_k[h]])
    for ty in range(3):
        for h in range(NH):
            kb.op("gpsimd", lambda e, ty=ty, h=h: e.memset(x_ext[ty][h][:, 0:3], 0.0), writes=[x_k[ty][h]])
    o_s = [kb.sbuf("o_s%d" % b, [128, HW], BF16) for b in range(2)]
    o_k = [Trk() for _ in range(2)]

    NSET = 2
    TN = ["kbe", "kdec", "vb", "dg", "egf", "decT", "decS", "WT", "nkk", "M", "MT", "M2", "M2T", "TT", "attnT", "qdecT", "val", "kcdT", "vn", "tmpa", "tmpb"]
    tmp = [{n: kb.sbuf("t%d_%s" % (s, n), [128, 256 if n == "dg" else 128], F32) for n in TN} for s in range(NSET)]
    tmk = [{n: Trk() for n in TN} for s in range(NSET)]
    ss_s = kb.sbuf("ss_s", [128, 4], F32)
    ss_k = Trk()
    junk = kb.sbuf("junk_o", [128, 128], F32)
    junk_k = Trk()

    hn_v = hnT.rearrange("(c p) t -> p c t", p=128)

    def load(i):
        b = i % 2
        kb.dma("sync", hn_s[b][:, :, :], hn_v[:, :, i * TT:(i + 1) * TT], writes=[hn_k[b]])

    def mm(ps, pk, M, N, lhsT, rhs, reads, start=True, stop=True, p0=0):
        kb.op("tensor", lambda e: e.matmul(ps[p0:p0 + M, :N], lhsT=lhsT, rhs=rhs, start=start, stop=stop), reads=reads, writes=[pk])

    load(0)
    unit = 0
    for i in range(NT):
        b = i % 2
        if i + 1 < NT:
            load(i + 1)
        for ty in range(3):
            for h in range(NH):
                ps, pk = pp.get()
                c0 = ty * HW + h * 128
                for kc in range(8):
                    mm(ps, pk, 128, TT, wqkv_t[:, kc, c0:c0 + 128], hn_s[b][:, kc, :], [wqkv_k, hn_k[b]], kc == 0, kc == 7)
                if i > 0:
                    kb.op("gpsimd", lambda e, ty=ty, h=h: e.tensor_copy(out=x_ext[ty][h][:, 0:3], in_=x_ext[ty][h][:, TT:TT + 3]),
                          reads=[x_k[ty][h]], writes=[x_k[ty][h]])
                kb.op("scalar", lambda e, ps=ps, ty=ty, h=h: e.activation(out=x_ext[ty][h][:, 3:XW], in_=ps[:, :TT], func=AF.Copy),
                      reads=[pk], writes=[x_k[ty][h]])
        for j in range(4):
            ps, pk = pp.get()
            for kc in range(8):
                mm(ps, pk, 128, HW, hn_s[b][:, kc, j * 128:(j + 1) * 128], wz_t[:, kc, :], [wz_k, hn_k[b]], kc == 0, kc == 7)
            kb.op("scalar", lambda e, ps=ps, j=j: e.activation(out=gz_s[j][:, :], in_=ps[:, :HW], func=AF.Silu), reads=[pk], writes=[gz_k[j]])
            for h in range(NH):
                kb.op("gpsimd", lambda e, j=j, h=h: e.tensor_tensor(out=gz_s[j][:, h * 128:(h + 1) * 128], in0=gz_s[j][:, h * 128:(h + 1) * 128], in1=onorm_t[:, :], op=ALU.mult),
                      reads=[onorm_k], writes=[gz_k[j]])
            ps, pk = pp.get()
            for kc in range(8):
                mm(ps, pk, 128, 2 * NH, hn_s[b][:, kc, j * 128:(j + 1) * 128], wba_t[:, kc, :], [wba_k, hn_k[b]], kc == 0, kc == 7)
            sc, sk = sc_s[j], sc_k[j]
            kb.op("scalar", lambda e, ps=ps, sc=sc: e.activation(out=sc[:, BETA, :], in_=ps[:, 0:NH], func=AF.Sigmoid), reads=[pk], writes=[sk[BETA]])
            kb.op("vector", lambda e, ps=ps, sc=sc: e.tensor_tensor(out=sc[:, T0, :], in0=ps[:, NH:2 * NH], in1=dtb_t[:, :], op=ALU.add), reads=[pk, dtb_k], writes=[sk[T0]])
            kb.op("vector", lambda e, sc=sc: e.scalar_tensor_tensor(out=sc[:, T1, :], in0=sc[:, T0, :], scalar=-1.0, in1=sc[:, T0, :], op0=ALU.mult, op1=ALU.max), reads=[sk[T0]], writes=[sk[T1]])
            kb.op("scalar", lambda e, sc=sc: e.activation(out=sc[:, T1, :], in_=sc[:, T1, :], func=AF.Exp, scale=-1.0), reads=[sk[T1]], writes=[sk[T1]])
            kb.op("scalar", lambda e, sc=sc: e.activation(out=sc[:, T1, :], in_=sc[:, T1, :], func=AF.Ln, bias=ONEC, scale=1.0), reads=[sk[T1], cst_k], writes=[sk[T1]])
            kb.op("vector", lambda e, sc=sc: e.scalar_tensor_tensor(out=sc[:, T0, :], in0=sc[:, T0, :], scalar=0.0, in1=sc[:, T1, :], op0=ALU.max, op1=ALU.add),
                  reads=[sk[T1]], writes=[sk[T0]])
            kb.op("vector", lambda e, sc=sc: e.scalar_tensor_tensor(out=sc[:, GG, :], in0=sc[:, T0, :], scalar=-1.0, in1=ea_t[:, :], op0=ALU.mult, op1=ALU.mult),
                  reads=[sk[T0], ea_k], writes=[sk[GG]])
            ps2, pk2 = pp.get()
            mm(ps2, pk2, 128, NH, C["tri"][:, :], sc[:, GG, :], [CK["tri"], sk[GG]])
            kb.op("scalar", lambda e, ps2=ps2, sc=sc: e.activation(out=sc[:, GC, :], in_=ps2[:, 0:NH], func=AF.Copy), reads=[pk2], writes=[sk[GC]])
            kb.op("scalar", lambda e, ps2=ps2, sc=sc: e.activation(out=sc[:, EG, :], in_=ps2[:, 0:NH], func=AF.Exp), reads=[pk2], writes=[sk[EG]])
            ps3, pk3 = pp.get()
            mm(ps3, pk3, 128, NH, C["selblk"][:, :], sc[:, GC, :], [CK["selblk"], sk[GC]])
            kb.op("vector", lambda e, ps3=ps3, sc=sc: e.tensor_tensor(out=sc[:, EDL, :], in0=ps3[:, 0:NH], in1=sc[:, GC, :], op=ALU.subtract), reads=[pk3, sk[GC]], writes=[sk[EDL]])
            kb.op("scalar", lambda e, sc=sc: e.activation(out=sc[:, EDL, :], in_=sc[:, EDL, :], func=AF.Exp), reads=[sk[EDL]], writes=[sk[EDL]])
            ps4, pk4 = pp.get()
            mm(ps4, pk4, 128, NH, C["selA"][:, :], sc[:, GC, :], [CK["selA"], sk[GC]])
            kb.op("scalar", lambda e, ps4=ps4, sc=sc: e.activation(out=sc[:, EGLA, :], in_=ps4[:, 0:NH], func=AF.Exp), reads=[pk4], writes=[sk[EGLA]])
            ps5, pk5 = pp.get()
            mm(ps5, pk5, 128, NH, C["selB"][:, :], sc[:, GC, :], [CK["selB"], sk[GC]])
            kb.op("scalar", lambda e, ps5=ps5, sc=sc: e.activation(out=sc[:, EGLB, :], in_=ps5[:, 0:NH], func=AF.Exp), reads=[pk5], writes=[sk[EGLB]])
            kb.op("vector", lambda e, sc=sc: e.tensor_tensor(out=sc[:, BEG, :], in0=sc[:, BETA, :], in1=sc[:, EG, :], op=ALU.mult), reads=[sk[BETA], sk[EG]], writes=[sk[BEG]])
        for ty in range(3):
            for h in range(NH):
                a = (ty * NH + h) % 2
                xe, xk = x_ext[ty][h], x_k[ty][h]
                cb = (ty * NH + h) * 4
                kb.op("vector", lambda e, xe=xe, a=a, cb=cb: e.tensor_scalar(out=acc_s[a][:, :], in0=xe[:, 0:TT], scalar1=cw_t[:, cb:cb + 1], scalar2=None, op0=ALU.mult),
                      reads=[xk, cw_k], writes=[acc_k[a]])
                for tap in range(1, 4):
                    kb.op("vector", lambda e, xe=xe, a=a, cb=cb, tap=tap: e.scalar_tensor_tensor(out=acc_s[a][:, :], in0=xe[:, tap:tap + TT], scalar=cw_t[:, cb + tap:cb + tap + 1],
                                                                                                in1=acc_s[a][:, :], op0=ALU.mult, op1=ALU.add),
                          reads=[xk, cw_k], writes=[acc_k[a]])
                kb.op("scalar", lambda e, a=a, ty=ty, h=h: e.activation(out=xc[ty][h][:, :], in_=acc_s[a][:, :], func=AF.Silu), reads=[acc_k[a]], writes=[xc_k[ty][h]])
                if ty < 2:
                    kb.op("scalar", lambda e, ty=ty, h=h: e.activation(out=sq_s[:, :], in_=xc[ty][h][:, :], func=AF.Square), reads=[xc_k[ty][h]], writes=[sq_k])
                    ps, pk = pp.get()
                    mm(ps, pk, 128, TT, ones_bf[:, :], sq_s[:, :], [ones_bfk, sq_k])
                    kb.op("scalar", lambda e, ps=ps: e.activation(out=rn_s[:, :], in_=ps[:, :TT], func=AF.Sqrt, bias=EPSC, scale=1.0), reads=[pk, cst_k], writes=[rn_k])
                    kb.op("vector", lambda e: e.reciprocal(out=rn_s[:, :], in_=rn_s[:, :]), reads=[rn_k], writes=[rn_k])
                    scl = float(128 ** -0.5) if ty == 0 else 1.0
                    kb.op("vector", lambda e, ty=ty, h=h, scl=scl: e.scalar_tensor_tensor(out=xc[ty][h][:, :], in0=xc[ty][h][:, :], scalar=scl, in1=rn_s[:, :], op0=ALU.mult, op1=ALU.mult),
                          reads=[rn_k], writes=[xc_k[ty][h]])
        for j in range(4):
            sc, sk = sc_s[j], sc_k[j]
            cols = slice(j * 128, (j + 1) * 128)
            ob = (i * 4 + j) % 2
            for h in range(NH):
                s = unit % NSET
                unit += 1
                t, tk = tmp[s], tmk[s]
                qn, kn, vc = xc[0][h], xc[1][h], xc[2][h]
                qk_, kk_, vk_ = xc_k[0][h], xc_k[1][h], xc_k[2][h]
                hs = slice(h, h + 1)
                ps, pk = pp.get()
                kb.op("tensor", lambda e, ps=ps, kn=kn: e.transpose(out=ps[:, 0:128], in_=kn[:, cols], identity=C["ident"][:, :]), reads=[kk_, CK["ident"]], writes=[pk])
                kb.op("scalar", lambda e, ps=ps, t=t: e.activation(out=t["kbe"][:, :], in_=ps[:, 0:128], func=AF.Copy, scale=sc[:, BEG, hs]), reads=[pk, sk[BEG]], writes=[tk["kbe"]])
                kb.op("vector", lambda e, ps=ps, t=t: e.tensor_scalar(out=t["kdec"][:, :], in0=ps[:, 0:128], scalar1=sc[:, EDL, hs], scalar2=None, op0=ALU.mult), reads=[pk, sk[EDL]], writes=[tk["kdec"]])
                ps, pk = pp.get()
                kb.op("tensor", lambda e, ps=ps, vc=vc: e.transpose(out=ps[:, 0:128], in_=vc[:, cols], identity=C["ident"][:, :]), reads=[vk_, CK["ident"]], writes=[pk])
                kb.op("scalar", lambda e, ps=ps, t=t: e.activation(out=t["vb"][:, :], in_=ps[:, 0:128], func=AF.Copy, scale=sc[:, BETA, hs]), reads=[pk, sk[BETA]], writes=[tk["vb"]])
                kb.op("gpsimd", lambda e, t=t: e.tensor_scalar(out=t["dg"][:, 0:128], in0=C["ident"][:, :], scalar1=sc[:, GC, hs], scalar2=None, op0=ALU.mult), reads=[CK["ident"], sk[GC]], writes=[tk["dg"]])
                kb.op("gpsimd", lambda e, t=t: e.tensor_scalar(out=t["dg"][:, 128:256], in0=C["ident"][:, :], scalar1=sc[:, BETA, hs], scalar2=None, op0=ALU.mult), reads=[CK["ident"], sk[BETA]], writes=[tk["dg"]])
                psr, pkr = pp.get()
                mm(psr, pkr, 128, 256, C["ones"][:, :], t["dg"][:, :], [CK["ones"], tk["dg"]])
                kb.op("scalar", lambda e, t=t, psr=psr: e.activation(out=t["egf"][:, :], in_=psr[:, 0:128], func=AF.Exp), reads=[pkr], writes=[tk["egf"]])
                kb.op("vector", lambda e, t=t, psr=psr: e.scalar_tensor_tensor(out=t["decT"][:, :], in0=psr[:, 0:128], scalar=sc[:, GC, hs], in1=C["mTfull"][:, :], op0=ALU.subtract, op1=ALU.add),
                      reads=[pkr, sk[GC], CK["mTfull"]], writes=[tk["decT"]])
                kb.op("scalar", lambda e, t=t: e.activation(out=t["decT"][:, :], in_=t["decT"][:, :], func=AF.Exp), reads=[tk["decT"]], writes=[tk["decT"]])
                kb.op("vector", lambda e, t=t, psr=psr: e.scalar_tensor_tensor(out=t["decS"][:, :], in0=psr[:, 0:128], scalar=sc[:, GC, hs], in1=C["mstrict"][:, :], op0=ALU.subtract, op1=ALU.subtract),
                      reads=[pkr, sk[GC], CK["mstrict"]], writes=[tk["decS"]])
                kb.op("scalar", lambda e, t=t: e.activation(out=t["decS"][:, :], in_=t["decS"][:, :], func=AF.Exp, scale=-1.0), reads=[tk["decS"]], writes=[tk["decS"]])
                kb.op("vector", lambda e, t=t, psr=psr: e.tensor_tensor(out=t["WT"][:, :], in0=psr[:, 128:256], in1=t["decT"][:, :], op=ALU.mult), reads=[pkr, tk["decT"]], writes=[tk["WT"]])
                psk, pkk = pp.get()
                mm(psk, pkk, 128, 128, kn[:, cols], kn[:, cols], [kk_])
                kb.op("vector", lambda e, t=t, psk=psk: e.scalar_tensor_tensor(out=t["nkk"][:, :], in0=psk[:, 0:128], scalar=-1.0, in1=C["sT01"][:, :], op0=ALU.mult, op1=ALU.mult),
                      reads=[pkk, CK["sT01"]], writes=[tk["nkk"]])
                kb.op("vector", lambda e, t=t, psk=psk: e.scalar_tensor_tensor(out=t["M"][:, :], in0=psk[:, 0:128], scalar=sc[:, BETA, hs], in1=t["decS"][:, :], op0=ALU.mult, op1=ALU.mult),
                      reads=[pkk, sk[BETA], tk["decS"]], writes=[tk["M"]])
                kb.op("gpsimd", lambda e, t=t: e.tensor_scalar(out=t["M"][:, :], in0=t["M"][:, :], scalar1=-1.0, scalar2=None, op0=ALU.mult), reads=[tk["M"]], writes=[tk["M"]])
                kb.op("gpsimd", lambda e, t=t: e.tensor_tensor(out=t["MT"][:, :], in0=t["nkk"][:, :], in1=t["WT"][:, :], op=ALU.mult), reads=[tk["nkk"], tk["WT"]], writes=[tk["MT"]])
                kb.op("gpsimd", lambda e, t=t: e.tensor_tensor(out=t["TT"][:, :], in0=t["MT"][:, :], in1=C["ident"][:, :], op=ALU.add), reads=[tk["MT"], CK["ident"]], writes=[tk["TT"]])
                psa, pka = pp.get()
                mm(psa, pka, 128, 128, kn[:, cols], qn[:, cols], [kk_, qk_])
                kb.op("vector", lambda e, t=t, psa=psa: e.tensor_tensor(out=t["attnT"][:, :], in0=psa[:, 0:128], in1=t["decT"][:, :], op=ALU.mult), reads=[pka, tk["decT"]], writes=[tk["attnT"]])
                kb.op("gpsimd", lambda e, t=t, qn=qn: e.tensor_tensor(out=t["qdecT"][:, :], in0=qn[:, cols], in1=t["egf"][:, :], op=ALU.mult), reads=[qk_, tk["egf"]], writes=[tk["qdecT"]])
                cur, curT = "M", "MT"
                nxt, nxtT = "M2", "M2T"
                for lev in range(1, 6):
                    ps1, pk1 = pp.get()
                    mm(ps1, pk1, 128, 128, t[curT][:, :], t[cur][:, :], [tk[curT], tk[cur]])
                    kb.op("scalar", lambda e, t=t, ps1=ps1, nxt=nxt: e.activation(out=t[nxt][:, :], in_=ps1[:, 0:128], func=AF.Copy), reads=[pk1], writes=[tk[nxt]])
                    if lev < 5:
                        ps2, pk2 = pp.get()
                        mm(ps2, pk2, 128, 128, t[cur][:, :], t[curT][:, :], [tk[curT], tk[cur]])
                        kb.op("vector", lambda e, t=t, ps2=ps2, nxtT=nxtT: e.tensor_copy(out=t[nxtT][:, :], in_=ps2[:, 0:128]), reads=[pk2], writes=[tk[nxtT]])
                    ps3, pk3 = pp.get()
                    mm(ps3, pk3, 128, 128, t[nxt][:, :], t["TT"][:, :], [tk[nxt], tk["TT"]])
                    kb.op("vector", lambda e, t=t, ps3=ps3: e.tensor_tensor(out=t["TT"][:, :], in0=t["TT"][:, :], in1=ps3[:, 0:128], op=ALU.add), reads=[pk3], writes=[tk["TT"]])
                    cur, curT, nxt, nxtT = nxt, nxtT, cur, curT
                psv, pkv = pp.get()
                mm(psv, pkv, 128, 128, t["TT"][:, :], t["vb"][:, :], [tk["TT"], tk["vb"]])
                kb.op("scalar", lambda e, t=t, psv=psv: e.activation(out=t["val"][:, :], in_=psv[:, 0:128], func=AF.Copy), reads=[pkv], writes=[tk["val"]])
                psc, pkc = pp.get()
                mm(psc, pkc, 128, 128, t["kbe"][:, :], t["TT"][:, :], [tk["TT"], tk["kbe"]])
                kb.op("scalar", lambda e, t=t, psc=psc: e.activation(out=t["kcdT"][:, :], in_=psc[:, 0:128], func=AF.Copy), reads=[pkc], writes=[tk["kcdT"]])
                pso, pko = pp.get()
                for c in range(2):
                    r0 = c * 64
                    rows = slice(r0, r0 + 64)
                    psn, pkn = pp.get()
                    mm(psn, pkn, 64, 128, t["kcdT"][:, rows], S_s[h][:, :], [tk["kcdT"], S_k[h]], p0=r0)
                    kb.op("vector", lambda e, t=t, psn=psn, rows=rows: e.tensor_tensor(out=t["vn"][rows, :], in0=t["val"][rows, :], in1=psn[rows, 0:128], op=ALU.subtract),
                          reads=[pkn, tk["val"]], writes=[tk["vn"]])
                    mm(pso, pko, 64, 128, t["qdecT"][:, rows], S_s[h][:, :], [tk["qdecT"], S_k[h]], True, False, p0=r0)
                    mm(pso, pko, 64, 128, t["attnT"][rows, rows], t["vn"][rows, :], [tk["attnT"], tk["vn"]], False, True, p0=r0)
                    pss, pks = pp.get()
                    mm(pss, pks, 128, 128, t["kdec"][rows, :], t["vn"][rows, :], [tk["kdec"], tk["vn"]])
                    egl = EGLA if c == 0 else EGLB
                    kb.op("vector", lambda e, pss=pss, egl=egl: e.scalar_tensor_tensor(out=S_s[h][:, :], in0=S_s[h][:, :], scalar=sc[:, egl, hs], in1=pss[:, 0:128], op0=ALU.mult, op1=ALU.add),
                          reads=[pks, sk[egl]], writes=[S_k[h]])
                kb.op("scalar", lambda e, pso=pso, h=h: e.activation(out=junk[:, :], in_=pso[:, 0:128], func=AF.Square, accum_out=ss_s[:, h:h + 1]), reads=[pko], writes=[junk_k, ss_k])
                kb.op("scalar", lambda e, h=h: e.activation(out=ss_s[:, h:h + 1], in_=ss_s[:, h:h + 1], func=AF.Sqrt, bias=EPSC, scale=1.0 / 128), reads=[ss_k, cst_k], writes=[ss_k])
                kb.op("vector", lambda e, h=h: e.reciprocal(out=ss_s[:, h:h + 1], in_=ss_s[:, h:h + 1]), reads=[ss_k], writes=[ss_k])
                kb.op("vector", lambda e, pso=pso, h=h: e.scalar_tensor_tensor(out=o_s[ob][:, h * 128:(h + 1) * 128], in0=pso[:, 0:128], scalar=ss_s[:, h:h + 1],
                                                                               in1=gz_s[j][:, h * 128:(h + 1) * 128], op0=ALU.mult, op1=ALU.mult),
                      reads=[pko, ss_k, gz_k[j]], writes=[o_k[ob]])
            kb.dma("sync", o_out[:, i * 4 + j, :], o_s[ob][:, :], reads=[o_k[ob]], is_output=True)
    return kb.finish(), kb


def build_norm0(T):
    kb = KB()
    TT = 512
    NT = T // TT
    hT = kb.dram("hT", [D, T], F32, "ExternalInput")
    g_in = kb.dram("g", [D], F32, "ExternalInput")
    hn_out = kb.dram("hn_out", [D, T], BF16, "ExternalOutput")
    g_t, g_k = load_vec_col(kb, "g_s", g_in, D)
    ones = kb.sbuf("ones", [128, 128], BF16)
    ones_trk = Trk()
    kb.op("vector", lambda h: h.memset(ones[:], 1.0), writes=[ones_trk])
    eps_t = kb.sbuf("eps", [128, 1], F32)
    eps_trk = Trk()
    kb.op("vector", lambda h: h.memset(eps_t[:], EPS), writes=[eps_trk])
    pp = PsumPool(kb)
    h_s = [kb.sbuf("h%d" % b, [128, 8, TT], F32) for b in range(2)]
    h_k = [[Trk() for _ in range(8)] for _ in range(2)]
    hn_s = [kb.sbuf("hn%d" % b, [128, 8, TT], BF16) for b in range(2)]
    hn_k = [[Trk() for _ in range(8)] for _ in range(2)]
    sq_s = kb.sbuf("sq", [128, 8, TT], BF16)
    sq_k = [Trk() for _ in range(8)]
    rstd_s = kb.sbuf("rstd", [128, TT], F32)
    rstd_k = Trk()
    scr = {"sq": [(sq_s[:, c, :], sq_k[c]) for c in range(8)], "rstd": (rstd_s[:, :], rstd_k), "eps": (eps_t[:, 0:1], eps_trk)}
    hT_v = hT.rearrange("(c p) t -> p c t", p=128)
    hno_v = hn_out.rearrange("(c p) t -> p c t", p=128)
    for i in range(NT):
        b = i % 2
        t0 = i * TT
        kb.dma("sync", h_s[b][:, :, :], hT_v[:, :, t0:t0 + TT], writes=h_k[b])
        rmsnorm_fm(kb, pp, [h_s[b][:, c, :] for c in range(8)], h_k[b], g_t, g_k, ones[:, :], ones_trk,
                   [hn_s[b][:, c, :] for c in range(8)], hn_k[b], scr, 8, TT, D)
        kb.dma("sync", hno_v[:, :, t0:t0 + TT], hn_s[b][:, :, :], reads=hn_k[b], is_output=True)
    return kb.finish(), kb


BF = ml_dtypes.bfloat16
B_, L_, T_ = 4, 8192, 4096
NQB_ = 32
_PROGS = {}


def _prog(name):
    if name not in _PROGS:
        if name == "n0":
            _PROGS[name] = build_norm0(T_)[0]
        elif name == "e1":
            _PROGS[name] = build_e1(T_)[0]
        elif name == "e2":
            _PROGS[name] = build_e2(NQB_)[0]
        elif name == "o1":
            _PROGS[name] = build_o1(L_, 4)[0]
        elif name == "p":
            _PROGS[name] = build_post(T_, False)[0]
        elif name == "pf":
            _PROGS[name] = build_post(T_, True)[0]
    return _PROGS[name]


def _run(name, in_maps):
    res = run_bass_kernel_spmd(_prog(name), in_maps, core_ids=list(range(NCORES)))
    return res.results


def _rot_cols(w):
    n = w.shape[1] // 64
    w4 = w.reshape(w.shape[0], n, 2, 32)
    return np.ascontiguousarray(w4[:, :, ::-1, :]).reshape(w.shape[0], n * 64)


def _rope_tabs(pos):
    inv = (np.float32(10000.0) ** (-np.arange(0, 64, 2, dtype=np.float32) / np.float32(64))).astype(np.float32)
    ang = (pos.astype(np.float32)[:, None] * inv[None, :]).astype(np.float32)
    c = np.cos(ang).astype(np.float32).T
    s = np.sin(ang).astype(np.float32).T
    return np.ascontiguousarray(np.concatenate([c, c, c, c], 0)), np.ascontiguousarray(np.concatenate([-s, s, -s, s], 0))


def _c(a):
    return np.ascontiguousarray(a)


def kernel(x, mix_norm, mlp_norm, w_ff1, w_ff2, ev_w_in, ev_kv_norm, ev_w_uk, ev_w_uv, ev_pool_w, ev_pool_scale, ev_w_out,
           od_w_in, od_conv_w, od_a_log, od_dt_bias, od_o_norm, od_w_out, final_norm):
    f32 = np.float32
    x = np.asarray(x, f32)
    cores = [(c // 2, c % 2) for c in range(NCORES)]
    hT = [_c(x[b].T) for b in range(B_)]
    res = _run("n0", [{"hT": _c(hT[b][:, hf * T_:(hf + 1) * T_]), "g": _c(np.asarray(mix_norm[0], f32))} for (b, hf) in cores])
    hn = [np.concatenate([res[2 * b]["hn_out"], res[2 * b + 1]["hn_out"]], 1) for b in range(B_)]
    gconst = gdn_consts()
    iota = np.tile(np.arange(256, dtype=f32), (128, 1))
    ident_bf = np.eye(128, dtype=f32).astype(BF)
    out = None
    for layer in range(4):
        j = layer // 2
        if layer % 2 == 0:
            w_in = np.asarray(ev_w_in[j], f32)
            w_rot = _c(np.concatenate([_rot_cols(w_in[:, 0:512]), _rot_cols(w_in[:, 640:1152]), _rot_cols(w_in[:, 1152:1216])], 1))
            w_uk = np.asarray(ev_w_uk[j], f32)
            ims = []
            for (b, hf) in cores:
                pos = hf * T_ + np.arange(T_)
                cosT, sinT = _rope_tabs(pos)
                invc0 = np.zeros((128, 4, 512), f32)
                for g in range(4):
                    invc0[:, g, :] = (1.0 / np.minimum(pos[:512] + 1, 2 ** (g + 1))).astype(f32)
                halo = np.zeros((D, HALO), BF) if hf == 0 else hn[b][:, T_ - HALO:T_]
                ims.append({"hnT": _c(np.concatenate([halo, hn[b][:, hf * T_:(hf + 1) * T_]], 1)), "w_in": _c(w_in), "w_rot": w_rot,
                            "kvn": _c(np.asarray(ev_kv_norm[j], f32)), "w_uk": _c(w_uk), "w_ukr": _rot_cols(w_uk), "w_uv": _c(np.asarray(ev_w_uv[j], f32)),
                            "pool_w": _c(np.asarray(ev_pool_w[j], f32)), "pool_sc": _c(np.asarray(ev_pool_scale[j], f32)),
                            "cosT": cosT, "sinT": sinT, "invc0": invc0})
            r1 = _run("e1", ims)
            cat = lambda k, ax: [np.concatenate([r1[2 * b][k], r1[2 * b + 1][k]], ax) for b in range(B_)]
            qT, qiT, kiT, kT, vv, wiT, ybT = cat("qT", 1), cat("qiT", 1), cat("kiT", 1), cat("kT", 1), cat("v", 0), cat("wiT", 1), cat("ybT", 1)
            ims = []
            qposs = []
            for (b, par) in cores:
                blocks = [2 * i + ((i % 2) ^ par) for i in range(NQB_)]
                qpos = np.concatenate([np.arange(g * 128, (g + 1) * 128) for g in blocks])
                qposs.append(qpos)
                ims.append({"qh": _c(qT[b].reshape(8, 64, L_)[:, :, qpos].transpose(1, 0, 2)),
                            "qih": _c(qiT[b].reshape(8, 64, L_)[:, :, qpos].transpose(1, 0, 2)),
                            "wi_tok": _c(wiT[b][:, qpos].T.reshape(NQB_, 128, 8).transpose(1, 0, 2)),
                            "qrel": _c((qpos.reshape(NQB_, 128) - 256 * np.arange(NQB_)[:, None]).T.astype(f32)),
                            "kiT": _c(kiT[b]), "kT": _c(kT[b]), "v": _c(vv[b].reshape(L_ // 128, 128, 64).transpose(1, 0, 2)),
                            "iota": iota, "ident": ident_bf})
            r2 = _run("e2", ims)
            yT = []
            for b in range(B_):
                ya = np.zeros((L_, 512), BF)
                for par in range(2):
                    ya[qposs[2 * b + par]] = r2[2 * b + par]["ya"].transpose(1, 0, 2).reshape(NQB_ * 128, 512)
                yT.append(np.concatenate([_c(ya.T), ybT[b]], 0))
            w_out = np.asarray(ev_w_out[j], f32)
        else:
            w_in = np.asarray(od_w_in[j], f32)
            conv_w = np.asarray(od_conv_w[j], f32)
            ims = []
            for (b, hg) in cores:
                heads = list(range(hg * 4, hg * 4 + 4))
                cols = lambda base: np.concatenate([np.arange(base + h * 128, base + (h + 1) * 128) for h in heads])
                cw = np.zeros((128, 3, 4, 4), f32)
                for ty in range(3):
                    for hi, h in enumerate(heads):
                        cw[:, ty, hi, :] = conv_w[:, ty * 1024 + h * 128: ty * 1024 + (h + 1) * 128].T
                im = {"hnT": _c(hn[b]), "w_qkv": _c(np.concatenate([w_in[:, cols(0)], w_in[:, cols(1024)], w_in[:, cols(2048)]], 1)),
                      "w_z": _c(w_in[:, cols(3072)]),
                      "w_ba": _c(np.concatenate([w_in[:, [4096 + h for h in heads]], w_in[:, [4104 + h for h in heads]]], 1)),
                      "cw": _c(cw.reshape(128, -1)), "alog_b": _c(np.tile(np.asarray(od_a_log[j], f32)[heads][None, :], (128, 1))),
                      "dtb_b": _c(np.tile(np.asarray(od_dt_bias[j], f32)[heads][None, :], (128, 1))),
                      "onorm_b": _c(np.tile(np.asarray(od_o_norm[j], f32)[None, :], (128, 1)))}
                for n, a in gconst.items():
                    im["c_" + n] = a
                ims.append(im)
            r1 = _run("o1", ims)
            yT = []
            for b in range(B_):
                o = np.concatenate([r1[2 * b + hg]["o_tok"].transpose(1, 0, 2).reshape(L_, 512) for hg in range(2)], 1)
                yT.append(_c(o.T))
            w_out = np.asarray(od_w_out[j], f32)
        final = layer == 3
        g_next = np.asarray(final_norm if final else mix_norm[layer + 1], f32)
        ims = [{"hT": _c(hT[b][:, hf * T_:(hf + 1) * T_]), "yT": _c(yT[b][:, hf * T_:(hf + 1) * T_]), "w_out": _c(w_out),
                "w1": _c(np.asarray(w_ff1[layer], f32)), "w2": _c(np.asarray(w_ff2[layer], f32)),
                "g_mlp": _c(np.asarray(mlp_norm[layer], f32)), "g_next": _c(g_next)} for (b, hf) in cores]
        rp = _run("pf" if final else "p", ims)
        hT = [np.concatenate([rp[2 * b]["hT_out"], rp[2 * b + 1]["hT_out"]], 1) for b in range(B_)]
        hn = [np.concatenate([rp[2 * b]["hn_out"], rp[2 * b + 1]["hn_out"]], 1) for b in range(B_)]
    out = np.stack([_c(hn[b].T) for b in range(B_)], 0).astype(np.float32)
    return out
```
